# Optimizing a Trainium2 kernel written in Bass

```python
import jax, jax.numpy as jnp
from jax import lax
import numpy as np

D_MODEL = 1024
BATCH = 8
SEQ = 8192
DEPTH = 1

N_HEADS_A = 8
N_KV_HEADS_A = 2
HEAD_DIM_A = 64
IDX_HEADS = 8
IDX_DIM = 64
TOPK_MAX = 256
Q_BLOCK = 128
CONV_CH = 512
CONV_WIDTH = 31
N_MEM = 256
MEM_HEADS = 4
MEM_HEAD_DIM = 128
N_BRANCHES = 3
N_GROUPS = 4
EXPERTS_PER_GROUP = 8
N_EXPERTS = N_GROUPS * EXPERTS_PER_GROUP
TOP_K_INNER = 2
D_FF_EXPERT = 256

ROPE_THETA = 10000.0
EPS = 1e-6

SPLIT_SIZES = (
    N_HEADS_A * HEAD_DIM_A,
    N_KV_HEADS_A * HEAD_DIM_A,
    N_KV_HEADS_A * HEAD_DIM_A,
    IDX_HEADS * IDX_DIM,
    IDX_DIM,
    IDX_HEADS,
    2 * CONV_CH,
    MEM_HEADS * MEM_HEAD_DIM,
    N_BRANCHES * D_MODEL,
)
IN_COLS = sum(SPLIT_SIZES)

kernel_name = "hybrid_dsa_conformer_memory_hiermoe"


def rmsnorm(x, g):
    xf = x.astype(jnp.float32)
    y = xf * lax.rsqrt(jnp.mean(xf * xf, axis=-1, keepdims=True) + EPS)
    return (y * g.astype(jnp.float32)).astype(x.dtype)


def layernorm(x, g, b):
    xf = x.astype(jnp.float32)
    mu = jnp.mean(xf, axis=-1, keepdims=True)
    var = jnp.mean(jnp.square(xf - mu), axis=-1, keepdims=True)
    y = (xf - mu) * lax.rsqrt(var + EPS)
    return (y * g.astype(jnp.float32) + b.astype(jnp.float32)).astype(x.dtype)


def rope(x, pos):
    d = x.shape[-1]
    half = d // 2
    inv = ROPE_THETA ** (-jnp.arange(half, dtype=jnp.float32) / half)
    ang = pos.astype(jnp.float32)[:, None] * inv[None, :]
    cos = jnp.cos(ang)[None, :, None, :]
    sin = jnp.sin(ang)[None, :, None, :]
    xf = x.astype(jnp.float32)
    x1, x2 = xf[..., :half], xf[..., half:]
    out = jnp.concatenate([x1 * cos - x2 * sin, x2 * cos + x1 * sin], axis=-1)
    return out.astype(x.dtype)


def dsa_attention(q, k, v, q_idx, k_idx, w_idx, topk):
    B, S = q.shape[0], q.shape[1]
    G = N_HEADS_A // N_KV_HEADS_A
    n_blocks = S // Q_BLOCK
    key_pos = jnp.arange(S)
    w_idx = w_idx.astype(jnp.float32) * (IDX_HEADS ** -0.5)
    scale = HEAD_DIM_A ** -0.5

    def block(i):
        start = i * Q_BLOCK
        qb = lax.dynamic_slice_in_dim(q, start, Q_BLOCK, axis=1)
        qib = lax.dynamic_slice_in_dim(q_idx, start, Q_BLOCK, axis=1)
        wib = lax.dynamic_slice_in_dim(w_idx, start, Q_BLOCK, axis=1)
        tpos = start + jnp.arange(Q_BLOCK)
        logits = jnp.einsum('bqhd,bsd->bqhs', qib, k_idx,
                            preferred_element_type=jnp.float32) * (IDX_DIM ** -0.5)
        score = jnp.einsum('bqh,bqhs->bqs', wib, jax.nn.relu(logits))
        causal = key_pos[None, :] <= tpos[:, None]
        score = jnp.where(causal[None], score, -jnp.inf)
        _, sel = lax.top_k(score, topk)
        valid = sel <= tpos[None, :, None]
        kg = jax.vmap(lambda kk, ii: kk[ii])(k, sel)
        vg = jax.vmap(lambda vv, ii: vv[ii])(v, sel)
        qg = qb.reshape(B, Q_BLOCK, N_KV_HEADS_A, G, HEAD_DIM_A)
        s = jnp.einsum('bqhgd,bqkhd->bqhgk', qg, kg,
                       preferred_element_type=jnp.float32) * scale
        s = jnp.where(valid[:, :, None, None, :], s, -jnp.inf)
        p = jax.nn.softmax(s, axis=-1)
        o = jnp.einsum('bqhgk,bqkhd->bqhgd', p.astype(vg.dtype), vg)
        return o.reshape(B, Q_BLOCK, N_HEADS_A * HEAD_DIM_A)

    out = lax.map(block, jnp.arange(n_blocks))
    return jnp.moveaxis(out, 0, 1).reshape(B, S, N_HEADS_A * HEAD_DIM_A)


def conformer_conv(conv_in, conv_w, conv_b, ln_g, ln_b):
    a, gte = jnp.split(conv_in, 2, axis=-1)
    u = a * jax.nn.sigmoid(gte)
    y = lax.conv_general_dilated(
        u, conv_w, window_strides=(1,), padding=[(CONV_WIDTH - 1, 0)],
        dimension_numbers=('NWC', 'WIO', 'NWC'), feature_group_count=CONV_CH)
    y = y + conv_b
    return jax.nn.silu(layernorm(y, ln_g, ln_b))


def memory_attention(q_m, mem, g_mem, w_mem_kv, g_qm, g_km):
    B, S = q_m.shape[0], q_m.shape[1]
    M = mem.shape[1]
    kv = jnp.einsum('bmd,dc->bmc', rmsnorm(mem, g_mem), w_mem_kv)
    k_m, v_m = jnp.split(kv, 2, axis=-1)
    k_m = rmsnorm(k_m.reshape(B, M, MEM_HEADS, MEM_HEAD_DIM), g_km)
    v_m = v_m.reshape(B, M, MEM_HEADS, MEM_HEAD_DIM)
    q = rmsnorm(q_m.reshape(B, S, MEM_HEADS, MEM_HEAD_DIM), g_qm)
    s = jnp.einsum('bshd,bmhd->bhsm', q, k_m,
                   preferred_element_type=jnp.float32) * (MEM_HEAD_DIM ** -0.5)
    p = jax.nn.softmax(s, axis=-1)
    o = jnp.einsum('bhsm,bmhd->bshd', p.astype(v_m.dtype), v_m)
    return o.reshape(B, S, MEM_HEADS * MEM_HEAD_DIM)


def hier_moe(h, w_rg, b_rg, w_re, b_re, w_up, w_down):
    T = h.shape[0]
    g_logits = (h @ w_rg + b_rg).astype(jnp.float32)
    p_g = jax.nn.softmax(g_logits, axis=-1)
    g_sel = jnp.argmax(g_logits, axis=-1)
    p_sel = jnp.take_along_axis(p_g, g_sel[:, None], axis=1)
    e_logits = (h @ w_re + b_re).astype(jnp.float32).reshape(T, N_GROUPS, EXPERTS_PER_GROUP)
    e_in = jnp.take_along_axis(e_logits, g_sel[:, None, None], axis=1)[:, 0]
    top_v, top_i = lax.top_k(e_in, TOP_K_INNER)
    w2 = jax.nn.softmax(top_v, axis=-1) * p_sel
    expert_id = g_sel[:, None] * EXPERTS_PER_GROUP + top_i
    combine = jnp.sum(jax.nn.one_hot(expert_id, N_EXPERTS, dtype=jnp.float32)
                      * w2[..., None], axis=1).astype(h.dtype)
    out = jnp.zeros_like(h)
    for n in range(N_EXPERTS):
        a, b = jnp.split(h @ w_up[n], 2, axis=-1)
        out = out + combine[:, n:n + 1] * ((jax.nn.silu(a) * b) @ w_down[n])
    return out


def setup_inputs(seed: int = 0) -> dict:
    key = jax.random.key(seed)
    ks = jax.random.split(key, 28)
    f32 = jnp.float32
    L = DEPTH

    def nrm(k, shape, fan_in):
        return jax.random.normal(k, shape, f32) * (fan_in ** -0.5)

    def gain(k, shape):
        return 1.0 + 0.05 * jax.random.normal(k, shape, f32)

    def small(k, shape, s):
        return s * jax.random.normal(k, shape, f32)

    return {
        "x": jax.random.normal(ks[0], (BATCH, SEQ, D_MODEL), f32),
        "mem": jax.random.normal(ks[1], (BATCH, N_MEM, D_MODEL), f32),
        "g_mix": gain(ks[2], (L, D_MODEL)),
        "w_in": nrm(ks[3], (L, D_MODEL, IN_COLS), D_MODEL),
        "b_gate": small(ks[4], (L, N_BRANCHES * D_MODEL), 0.02),
        "g_qa": gain(ks[5], (L, HEAD_DIM_A)),
        "g_ka": gain(ks[6], (L, HEAD_DIM_A)),
        "g_idx_k": gain(ks[7], (L, IDX_DIM)),
        "conv_w": nrm(ks[8], (L, CONV_WIDTH, 1, CONV_CH), CONV_WIDTH),
        "conv_b": small(ks[9], (L, CONV_CH), 0.02),
        "ln_g": gain(ks[10], (L, CONV_CH)),
        "ln_b": small(ks[11], (L, CONV_CH), 0.02),
        "g_mem": gain(ks[12], (L, D_MODEL)),
        "w_mem_kv": nrm(ks[13], (L, D_MODEL, 2 * MEM_HEADS * MEM_HEAD_DIM), D_MODEL),
        "g_qm": gain(ks[14], (L, MEM_HEAD_DIM)),
        "g_km": gain(ks[15], (L, MEM_HEAD_DIM)),
        "w_br_a": nrm(ks[16], (L, N_HEADS_A * HEAD_DIM_A, D_MODEL), N_HEADS_A * HEAD_DIM_A),
        "w_br_b": nrm(ks[17], (L, CONV_CH, D_MODEL), CONV_CH),
        "w_br_m": nrm(ks[18], (L, MEM_HEADS * MEM_HEAD_DIM, D_MODEL), MEM_HEADS * MEM_HEAD_DIM),
        "w_o": nrm(ks[19], (L, D_MODEL, D_MODEL), D_MODEL),
        "g_ffn": gain(ks[20], (L, D_MODEL)),
        "w_rg": nrm(ks[21], (L, D_MODEL, N_GROUPS), D_MODEL),
        "b_rg": small(ks[22], (L, N_GROUPS), 0.01),
        "w_re": nrm(ks[23], (L, D_MODEL, N_EXPERTS), D_MODEL),
        "b_re": small(ks[24], (L, N_EXPERTS), 0.01),
        "w_up": nrm(ks[25], (L, N_EXPERTS, D_MODEL, 2 * D_FF_EXPERT), D_MODEL),
        "w_down": nrm(ks[26], (L, N_EXPERTS, D_FF_EXPERT, D_MODEL), D_FF_EXPERT),
    }


def reference(x, mem, g_mix, w_in, b_gate, g_qa, g_ka, g_idx_k, conv_w, conv_b, ln_g, ln_b,
              g_mem, w_mem_kv, g_qm, g_km, w_br_a, w_br_b, w_br_m, w_o, g_ffn,
              w_rg, b_rg, w_re, b_re, w_up, w_down):
    B, S, D = x.shape
    topk = min(TOPK_MAX, S // 4)
    pos = jnp.arange(S, dtype=jnp.int32)
    split_points = []
    acc = 0
    for sz in SPLIT_SIZES[:-1]:
        acc += sz
        split_points.append(acc)

    for l in range(DEPTH):
        h = rmsnorm(x, g_mix[l])
        proj = jnp.einsum('bsd,dc->bsc', h, w_in[l])
        q_a, k_a, v_a, q_i, k_i, w_i, conv_in, q_m, gate_logits = jnp.split(
            proj, split_points, axis=-1)

        q_a = rope(rmsnorm(q_a.reshape(B, S, N_HEADS_A, HEAD_DIM_A), g_qa[l]), pos)
        k_a = rope(rmsnorm(k_a.reshape(B, S, N_KV_HEADS_A, HEAD_DIM_A), g_ka[l]), pos)
        v_a = v_a.reshape(B, S, N_KV_HEADS_A, HEAD_DIM_A)
        q_i = rope(q_i.reshape(B, S, IDX_HEADS, IDX_DIM), pos)
        k_i = rope(rmsnorm(k_i, g_idx_k[l])[:, :, None, :], pos)[:, :, 0, :]
        o_a = dsa_attention(q_a, k_a, v_a, q_i, k_i, w_i, topk)

        o_b = conformer_conv(conv_in, conv_w[l], conv_b[l], ln_g[l], ln_b[l])

        o_m = memory_attention(q_m, mem, g_mem[l], w_mem_kv[l], g_qm[l], g_km[l])

        gates = jax.nn.sigmoid((gate_logits + b_gate[l]).astype(jnp.float32)).astype(x.dtype)
        gates = gates.reshape(B, S, N_BRANCHES, D)
        merged = (gates[:, :, 0] * (o_a @ w_br_a[l])
                  + gates[:, :, 1] * (o_b @ w_br_b[l])
                  + gates[:, :, 2] * (o_m @ w_br_m[l]))
        x = x + merged @ w_o[l]

        h2 = rmsnorm(x, g_ffn[l]).reshape(B * S, D)
        moe = hier_moe(h2, w_rg[l], b_rg[l], w_re[l], b_re[l], w_up[l], w_down[l])
        x = x + moe.reshape(B, S, D)
    return x
```

```python
import numpy as np
import ml_dtypes
from contextlib import ExitStack
import concourse.bass as bass
import concourse.mybir as mybir
from concourse.bass_utils import run_bass_kernel_spmd

F32 = mybir.dt.float32
BF16 = mybir.dt.bfloat16
ALU = mybir.AluOpType
AF = mybir.ActivationFunctionType
AX = mybir.AxisListType

D = 1024
EPS = 1e-6
C_QA, C_KA, C_VA, C_QI, C_KI, C_WI, C_CV, C_QM, C_GT = 0, 512, 640, 768, 1280, 1344, 1352, 2376, 2888
N_TM = 1352
N_FM = 5960 - 1352
KITER = 14
TOPK = 256
NEG = -30000.0

ENGS = ("pe", "act", "dve", "pool", "sp")
SEM_LIMIT = 30000
N_DMA_SEMS = 32


class _Rec:
    def __init__(self):
        self.calls = []

    def __getattr__(self, name):
        def f(*a, **kw):
            self.calls.append((name, a, kw))
            return self
        return f


def _replayer(calls):
    def run(e):
        r = None
        for (name, a, kw) in calls:
            r = getattr(e, name)(*a, **kw)
        return r
    return run


class Prog:
    def __init__(self, nc, stack):
        self.nc = nc
        self.stack = stack
        self.q = {e: [] for e in ENGS}
        self.nsem = 0
        self.cur = {}
        self.cnt = {}
        for e in ENGS:
            self.cur[e] = self._new_sem(e)
            self.cnt[e] = 0
        self.dma_sems = [self._new_sem("dma%d" % i) for i in range(N_DMA_SEMS)]
        self.dma_cnt = [0] * N_DMA_SEMS
        self.n_hw = 16
        self.dma_next_hw = 0
        self.dma_next_sw = 0
        self.last_w = {}
        self.readers = {}
        self.seen = {e: {} for e in ENGS}
        self.n_ops = 0

    def _new_sem(self, name):
        self.nsem += 1
        return self.stack.enter_context(self.nc.semaphore("s_%s_%d" % (name, self.nsem)))

    def _deps(self, reads, writes, skip_src=None):
        ev = []
        for k in reads:
            w = self.last_w.get(k)
            if w is not None and w[0] != skip_src:
                ev.append(w[1])
        for k in writes:
            w = self.last_w.get(k)
            if w is not None and w[0] != skip_src:
                ev.append(w[1])
            r = self.readers.get(k)
            if r:
                for src, e in r.items():
                    if src != skip_src:
                        ev.append(e)
        return ev

    def _waits(self, eng, evs):
        best = {}
        for (sem, val) in evs:
            i = id(sem)
            if self.seen[eng].get(i, 0) < val:
                if i not in best or best[i][1] < val:
                    best[i] = (sem, val)
        out = []
        for i, (sem, val) in best.items():
            self.seen[eng][i] = val
            out.append((sem, val))
        return out

    def _record(self, ev, reads, writes, src):
        for k in writes:
            self.last_w[k] = (src, ev)
            self.readers[k] = {}
        for k in reads:
            self.readers.setdefault(k, {})[src] = ev

    def op(self, eng, fn, reads=(), writes=()):
        waits = self._waits(eng, self._deps(reads, writes, "pe" if eng == "pe" else None))
        if self.cnt[eng] >= SEM_LIMIT:
            self.cur[eng] = self._new_sem(eng)
            self.cnt[eng] = 0
        self.cnt[eng] += 1
        sem = self.cur[eng]
        ev = (sem, self.cnt[eng])
        rec = _Rec()
        fn(rec)
        assert rec.calls
        self.q[eng].append((_replayer(rec.calls), waits, sem, 1))
        self._record(ev, reads, writes, eng)
        self.n_ops += 1
        return ev

    def _slot(self, eng):
        if eng == "pool":
            s = self.n_hw + self.dma_next_sw
            self.dma_next_sw = (self.dma_next_sw + 1) % (N_DMA_SEMS - self.n_hw)
        else:
            s = self.dma_next_hw
            self.dma_next_hw = (self.dma_next_hw + 1) % self.n_hw
        return s

    def dma(self, eng, out, in_, reads=(), writes=(), **kw):
        s = self._slot(eng)
        sem = self.dma_sems[s]
        evs = self._deps(reads, writes)
        if self.dma_cnt[s] > 0:
            evs.append((sem, self.dma_cnt[s]))
        waits = self._waits(eng, evs)
        self.dma_cnt[s] += 16
        ev = (sem, self.dma_cnt[s])

        def fn(e, out=out, in_=in_, kw=kw):
            return e.dma_start(out=out, in_=in_, **kw)

        self.q[eng].append((fn, waits, sem, 16))
        self._record(ev, reads, writes, ("dma", s))
        self.n_ops += 1
        return ev

    def idma(self, out, in_, idx_ap, gather, reads=(), writes=(), bound=None):
        eng = "pool"
        s = self._slot(eng)
        sem = self.dma_sems[s]
        evs = self._deps(reads, writes)
        if self.dma_cnt[s] > 0:
            evs.append((sem, self.dma_cnt[s]))
        waits = self._waits(eng, evs)
        self.dma_cnt[s] += 16
        ev = (sem, self.dma_cnt[s])
        off = bass.IndirectOffsetOnAxis(ap=idx_ap, axis=0)

        def fn(e):
            kw = {}
            if bound is not None:
                kw = dict(bounds_check=bound, oob_is_err=False)
            if gather:
                return e.indirect_dma_start(out=out, out_offset=None, in_=in_, in_offset=off, **kw)
            return e.indirect_dma_start(out=out, out_offset=off, in_=in_, in_offset=None, **kw)

        self.q[eng].append((fn, waits, sem, 16))
        self._record(ev, reads, writes, ("dma", s))
        self.n_ops += 1
        return ev

    def barrier(self):
        evs = [(self.cur[e], self.cnt[e]) for e in ENGS if self.cnt[e] > 0]
        evs += [(self.dma_sems[s], self.dma_cnt[s]) for s in range(N_DMA_SEMS) if self.dma_cnt[s] > 0]
        for e in ENGS:
            w = self._waits(e, evs)
            if w:
                self.q[e].append((None, w, None, 0))

    def final_wait(self, eng, evs):
        waits = self._waits(eng, evs)
        self.q[eng].append((None, waits, None, 0))

    def emit(self):
        nc = self.nc
        with nc.Block() as block:
            def mk(q):
                def body(e):
                    for (fn, waits, sem, inc) in q:
                        for (ws, wv) in waits:
                            e.wait_ge(ws, wv)
                        if fn is not None:
                            fn(e).then_inc(sem, inc)
                return body
            block.tensor(mk(self.q["pe"]))
            block.scalar(mk(self.q["act"]))
            block.vector(mk(self.q["dve"]))
            block.gpsimd(mk(self.q["pool"]))
            block.sync(mk(self.q["sp"]))


def build(S, dbg=False):
    NT = S // 128
    NBLK = S // 512
    SB = min(2048, S)
    NSB = S // SB
    nc = bass.Bass("TRN2", target_bir_lowering=False)

    def din(name, shape, dt=F32):
        return nc.dram_tensor(name, list(shape), dt, kind="ExternalInput").ap()

    def dscr(name, shape, dt):
        kind = "ExternalOutput" if dbg else "Internal"
        return nc.dram_tensor(name, list(shape), dt, kind=kind).ap()

    x = din("x", [S, D]); mem = din("mem", [256, D])
    g_mix = din("g_mix", [1, D]); w_in = din("w_in", [D, 5960]); b_gate = din("b_gate", [24, 128])
    g_qa = din("g_qa", [1, 64]); g_ka = din("g_ka", [1, 64]); g_idx_k = din("g_idx_k", [1, 64])
    conv_w = din("conv_w", [31, 512]); conv_b = din("conv_b", [4, 128])
    ln_g = din("ln_g", [4, 128]); ln_b = din("ln_b", [4, 128])
    g_mem = din("g_mem", [1, D]); w_mem_kv = din("w_mem_kv", [D, 1024])
    g_qm = din("g_qm", [1, 128]); g_km = din("g_km", [1, 128])
    w_br_a = din("w_br_a", [512, D]); w_br_b = din("w_br_b", [512, D]); w_br_m = din("w_br_m", [512, D])
    w_o = din("w_o", [D, D]); g_ffn = din("g_ffn", [1, D])
    w_rg = din("w_rg", [D, 4]); b_rg = din("b_rg", [1, 4]); w_re = din("w_re", [D, 32]); b_re = din("b_re", [1, 32])
    w_up = din("w_up", [32, D, 512]); w_down = din("w_down", [32, 256, D])
    c_identb = din("c_identb", [128, 128], BF16); c_identf = din("c_identf", [128, 128])
    c_i4 = din("c_i4", [128, 512], BF16); c_cb = din("c_cb", [128, 128])
    c_cos = din("c_cos", [128, NT, 32]); c_sin = din("c_sin", [128, NT, 32])
    c_pow2 = din("c_pow2", [128, KITER + 1])
    NTS_ = (2 * S + 32 * 511 + 511) // 512
    c_ltri = din("c_ltri", [128, 128], BF16); c_thr = din("c_thr", [128, 32, 2 * S // 512]); c_tm = din("c_tm", [128, 32, 32])
    c_kc = din("c_kc", [128, NTS_, 32]); c_io8 = din("c_io8", [128, 10])
    out = nc.dram_tensor("out", [S, D], F32, kind="ExternalOutput").ap()

    qT_s = dscr("qT_s", [NT, 64, 1024], BF16)
    qiT_s = dscr("qiT_s", [NT, 64, 1024], BF16)
    kT_s = dscr("kT_s", [2, 64, S], BF16)
    kiT_s = dscr("kiT_s", [64, S], BF16)
    v_s = dscr("v_s", [S, 130], BF16)
    wi_s = dscr("wi_s", [S, 8], F32)
    mg_s = dscr("mg_s", [NBLK, 128, 8, 2, 512], BF16)
    oaT_s = dscr("oaT_s", [128, 4, S], BF16)
    NTS = (2 * S + 32 * 511 + 511) // 512
    x1_s = dscr("x1_s", [S, D], F32)
    h2_s = dscr("h2_s", [S, D], BF16)
    Hs = dscr("Hs", [NTS * 512, D], BF16)
    Ysc = dscr("Ysc", [NTS * 512, D], F32)
    wub_s = dscr("wub_s", [32 * 128, 8 * 512], BF16)
    wdb_s = dscr("wdb_s", [32 * 128, 2 * D], BF16)

    st = ExitStack()
    with st:
        P = Prog(nc, st)

        ARENA_F32 = 50688
        arena = st.enter_context(nc.sbuf_tensor("arena", [128, ARENA_F32], F32))
        top = {"v": 0, "max": 0}

        class _Scope:
            def __init__(self, s):
                self.s = s
            def __enter__(self):
                self.s.__enter__()
                self.mark = top["v"]
                return self.s
            def __exit__(self, *a):
                top["v"] = self.mark
                return self.s.__exit__(*a)

        def scope():
            return _Scope(ExitStack())

        def sb(stack, name, shape, dt):
            esz = 4 if dt in (F32, mybir.dt.int32) else 2
            n = 1
            for d_ in shape[1:]:
                n *= d_
            nwords = (n * esz + 3) // 4
            nwords = (nwords + 7) // 8 * 8
            off = top["v"]
            top["v"] = off + nwords
            top["max"] = max(top["max"], top["v"])
            assert top["v"] <= ARENA_F32, ("SBUF arena overflow", name, top["v"])
            v = arena[0:shape[0], off:off + nwords]
            if dt != F32:
                v = v.bitcast(dt)
            v = v[:, 0:n]
            if len(shape) == 3:
                v = v.rearrange("p (a b) -> p a b", a=shape[1])
            elif len(shape) == 4:
                v = v.rearrange("p (a b c) -> p a b c", a=shape[1], b=shape[2])
            return v

        def pe(fn, r, w): return P.op("pe", fn, r, w)
        def act(fn, r, w): return P.op("act", fn, r, w)
        def dve(fn, r, w): return P.op("dve", fn, r, w)
        def pool(fn, r, w): return P.op("pool", fn, r, w)

        BK = [st.enter_context(nc.psum_tensor("bk%d" % i, [128, 512], F32)) for i in range(8)]
        rr = {"i": 0}

        def nbank(n=6):
            i = rr["i"] % n
            rr["i"] += 1
            return BK[i], "bk%d" % i

        identb = sb(st, "identb", [128, 128], BF16)
        identf = sb(st, "identf", [128, 128], F32)
        onesf = sb(st, "onesf", [128, 128], F32)
        onesb = sb(st, "onesb", [128, 128], BF16)
        epsb = sb(st, "epsb", [128, 1], F32)
        P.dma("sp", identb[:], c_identb, writes=["identb"])
        P.dma("sp", identf[:], c_identf, writes=["identf"])
        pool(lambda e: e.memset(onesf[:], 1.0), [], ["onesf"])
        pool(lambda e: e.memset(onesb[:], 1.0), [], ["onesb"])
        pool(lambda e: e.memset(epsb[:], EPS), [], ["epsb"])

        joinsc = sb(st, "joinsc", [128, 1], F32)

        def bcast_load(stack, name, ap_row, n):
            t = sb(stack, name, [128, n], F32)
            P.dma("sp", t[:], ap_row.to_broadcast([128, n]), writes=[name])
            return t

        def load_T(stack, name, src, R, C):
            t = sb(stack, name, [128, C, R], F32)
            with scope() as tmp:
                raw = sb(tmp, name + "_raw", [R, C * 128], F32)
                P.dma("sp", raw[:], src, writes=[name + "_raw"])
                for c in range(C):
                    bk, bkk = nbank()
                    pe(lambda e, bk=bk, c=c: e.transpose(bk[:, 0:R], raw[:, c * 128:(c + 1) * 128], identf[0:R, 0:R]),
                       [name + "_raw", "identf"], [bkk])
                    dve(lambda e, bk=bk, c=c: e.tensor_copy(out=t[:, c, :], in_=bk[:, 0:R]), [bkk], [name])
                P.barrier()
            return t

        def rstd_from_ss(ss, rs, n, scale, keys_r, key_w):
            act(lambda e: e.activation(out=rs, in_=ss, func=AF.Sqrt, bias=epsb[:], scale=scale), keys_r + ["epsb"], [key_w])
            dve(lambda e: e.reciprocal(out=rs, in_=rs), [key_w], [key_w])

        def wload_bf16(dst3, src2, K, N, key, n0=0):
            subs = []
            c = 0
            while c < N:
                w = min(2048, N - c)
                for k in range(K):
                    sk = "%s#%d_%d" % (key, c, k)
                    subs.append(sk)
                    P.dma("pool", dst3[:, k, c:c + w], src2[k * 128:(k + 1) * 128, n0 + c:n0 + c + w], writes=[sk])
                c += w
            P.op("pool", lambda e: e.memset(joinsc[:], 0.0), subs, [key, "joinsc"])

        with scope() as p1:
            w_tm = sb(p1, "w_tm", [128, 8, N_TM], BF16)
            w_fm = sb(p1, "w_fm", [128, 8, N_FM], BF16)
            wbb = sb(p1, "wbb", [128, 4, D], BF16)
            wbm = sb(p1, "wbm", [128, 4, D], BF16)
            kmT = sb(p1, "kmT", [128, 4, 256], BF16)
            vm = sb(p1, "vm", [128, 2, 512], BF16)
            wload_bf16(w_tm, w_in, 8, N_TM, "w_tm", 0)
            wload_bf16(w_fm, w_in, 8, N_FM, "w_fm", N_TM)
            wload_bf16(wbb, w_br_b, 4, D, "wbb")
            wload_bf16(wbm, w_br_m, 4, D, "wbm")
            gmix_bc = bcast_load(p1, "gmix_bc", g_mix, D)
            gqa_bc = bcast_load(p1, "gqa_bc", g_qa, 64)
            gka_bc = bcast_load(p1, "gka_bc", g_ka, 64)
            gki_bc = bcast_load(p1, "gki_bc", g_idx_k, 64)
            bgT = load_T(p1, "bgT", b_gate, 24, 1)
            cbT = load_T(p1, "cbT", conv_b, 4, 1)
            lgT = load_T(p1, "lgT", ln_g, 4, 1)
            lbT = load_T(p1, "lbT", ln_b, 4, 1)
            cwT = load_T(p1, "cwT", conv_w, 31, 4)
            uu = sb(p1, "uu", [128, 4, 30 + 512], BF16)
            pool(lambda e: e.memset(uu[:], 0.0), [], ["uu"])

            with scope() as p0:
                wkv = sb(p0, "wkv", [128, 8, 1024], BF16)
                wload_bf16(wkv, w_mem_kv, 8, 1024, "wkv")
                gmem_bc = bcast_load(p0, "gmem_bc", g_mem, D)
                gkm_bc = bcast_load(p0, "gkm_bc", g_km, 128)
                gqm_bc = bcast_load(p0, "gqm_bc", g_qm, 128)
                dve(lambda e: e.scalar_tensor_tensor(out=gkm_bc[:], in0=gkm_bc[:], scalar=128.0 ** -0.5, in1=gqm_bc[:],
                                                     op0=ALU.mult, op1=ALU.mult), ["gkm_bc", "gqm_bc"], ["gkm_bc"])
                mt_ = sb(p0, "mt_", [128, D], F32)
                junk = sb(p0, "junk0", [128, D], BF16)
                hm = sb(p0, "hm", [128, D], BF16)
                memT = sb(p0, "memT", [128, 8, 256], BF16)
                ss = sb(p0, "ss0", [128, 4], F32)
                rs = sb(p0, "rs0", [128, 4], F32)
                kx = sb(p0, "kx", [128, 512], F32)
                kn = sb(p0, "kn", [128, 512], BF16)
                for m in range(2):
                    P.dma("sp", mt_[:], mem[m * 128:(m + 1) * 128, :], writes=["mt_"])
                    act(lambda e: e.activation(out=junk[:], in_=mt_[:], func=AF.Square, accum_out=ss[:, 0:1]), ["mt_"], ["junk0", "ss0"])
                    rstd_from_ss(ss[:, 0:1], rs[:, 0:1], 1, 1.0 / D, ["ss0"], "rs0")
                    dve(lambda e: e.scalar_tensor_tensor(out=hm[:], in0=mt_[:], scalar=rs[:, 0:1], in1=gmem_bc[:],
                                                         op0=ALU.mult, op1=ALU.mult), ["mt_", "rs0", "gmem_bc"], ["hm"])
                    tb = BK[6][:].bitcast(BF16)
                    pe(lambda e: [e.transpose(tb[:, k * 128:(k + 1) * 128], hm[:, k * 128:(k + 1) * 128], identb[:]) for k in range(8)][-1],
                       ["hm", "identb"], ["bk6"])
                    dve(lambda e, m=m: e.tensor_copy(out=memT[:, :, m * 128:(m + 1) * 128],
                                                     in_=tb.rearrange("p (k t) -> p k t", k=8)), ["bk6"], ["memT"])
                for m in range(2):
                    for half in range(2):
                        bk, bkk = nbank()
                        pe(lambda e, bk=bk, m=m, half=half: [e.matmul(bk[:], lhsT=memT[:, k, m * 128:(m + 1) * 128],
                                                                       rhs=wkv[:, k, half * 512:(half + 1) * 512],
                                                                       start=(k == 0), stop=(k == 7)) for k in range(8)][-1],
                           ["memT", "wkv"], [bkk])
                        if half == 1:
                            act(lambda e, bk=bk, m=m: e.copy(out=vm[:, m, :], in_=bk[:]), [bkk], ["vm"])
                        else:
                            act(lambda e, bk=bk: e.copy(out=kx[:], in_=bk[:]), [bkk], ["kx"])
                            dve(lambda e: e.tensor_tensor(out=junk[:, 0:512], in0=kx[:], in1=kx[:], op=ALU.mult), ["kx"], ["junk0"])
                            dve(lambda e: e.tensor_reduce(out=ss[:], in_=junk[:, 0:512].rearrange("p (h d) -> p h d", h=4),
                                                          axis=AX.X, op=ALU.add), ["junk0"], ["ss0"])
                            rstd_from_ss(ss[:], rs[:], 4, 1.0 / 128, ["ss0"], "rs0")
                            for h in range(4):
                                dve(lambda e, h=h: e.scalar_tensor_tensor(out=kn[:, h * 128:(h + 1) * 128], in0=kx[:, h * 128:(h + 1) * 128],
                                                                          scalar=rs[:, h:h + 1], in1=gkm_bc[:], op0=ALU.mult, op1=ALU.mult),
                                    ["kx", "rs0", "gkm_bc"], ["kn"])
                            tb = BK[6][:].bitcast(BF16)
                            pe(lambda e: [e.transpose(tb[:, h * 128:(h + 1) * 128], kn[:, h * 128:(h + 1) * 128], identb[:]) for h in range(4)][-1],
                               ["kn", "identb"], ["bk6"])
                            dve(lambda e, m=m: e.tensor_copy(out=kmT[:, :, m * 128:(m + 1) * 128],
                                                             in_=tb[:, 0:512].rearrange("p (h t) -> p h t", h=4)), ["bk6"], ["kmT"])
                P.barrier()

            hT = sb(p1, "hT", [128, 8, 512], BF16)
            obT = sb(p1, "obT", [128, 4, 512], BF16)
            omT = sb(p1, "omT", [128, 4, 512], BF16)
            xt = [sb(p1, "xt%d" % i, [128, D], F32) for i in range(2)]
            junk = sb(p1, "junkA", [128, D], BF16)
            hh = sb(p1, "hh", [128, D], BF16)
            ss = sb(p1, "ssA", [128, 16], F32)
            rs = sb(p1, "rsA", [128, 16], F32)
            cs = [sb(p1, "cs%d" % i, [128, 2, 32], F32) for i in range(4)]
            qx = sb(p1, "qx", [128, 512], F32)
            sq = sb(p1, "sqA", [128, 512], F32)
            qn = sb(p1, "qn", [128, 512], F32)
            t1 = sb(p1, "t1", [128, 256], F32); t2 = sb(p1, "t2", [128, 256], F32)
            t3 = sb(p1, "t3", [128, 256], F32); t4 = sb(p1, "t4", [128, 256], F32)
            qr2 = [sb(p1, "qr%d" % i, [128, 512], BF16) for i in range(2)]
            stg = [sb(p1, "stg%d" % i, [64, 1024], BF16) for i in range(2)]
            vst = sb(p1, "vst", [128, 2, 65], BF16)
            wst = sb(p1, "wst", [128, 8], F32)
            pool(lambda e: e.memset(vst[:], 1.0), [], ["vst"])
            sgB = sb(p1, "sgB", [128, 512], F32)
            yy = sb(p1, "yy", [128, 4, 512], F32)
            sq2 = [sb(p1, "sq2_%d" % i, [128, 512], F32) for i in range(2)]
            rb = sb(p1, "rbB", [128, 512], F32)
            qmn = sb(p1, "qmn", [128, 512], BF16)
            pT = [sb(p1, "pTm%d" % i, [128, 512], BF16) for i in range(2)]
            rden = sb(p1, "rden", [128, 512], F32)
            mgst = [sb(p1, "mgst%d" % i, [128, 2, 512], BF16) for i in range(2)]
            print("phase1 arena words", top["v"], "of", ARENA_F32)
            sgi = {"i": 0}
            ab = {"i": 0}
            dgi = {"i": 0}
            dg = [sb(p1, "dg%d" % i, [128, 128], BF16) for i in range(8)]

            def norm_rope(src_ap, src_keys, nh, gain_bc, gkey, ck, csk, out_dram_fn):
                n = nh * 64
                qr = qr2[sgi["i"] % 2]; qrk = "qr%d" % (sgi["i"] % 2)
                if gain_bc is not None:
                    act(lambda e: e.copy(out=qx[:, 0:n], in_=src_ap), src_keys, ["qx"])
                    dve(lambda e: e.tensor_tensor(out=sq[:, 0:n], in0=qx[:, 0:n], in1=qx[:, 0:n], op=ALU.mult), ["qx"], ["sqA"])
                    dve(lambda e: e.tensor_reduce(out=ss[:, 0:nh], in_=sq[:, 0:n].rearrange("p (h d) -> p h d", h=nh),
                                                  axis=AX.X, op=ALU.add), ["sqA"], ["ssA"])
                    rstd_from_ss(ss[:, 0:nh], rs[:, 0:nh], nh, 1.0 / 64, ["ssA"], "rsA")
                    dve(lambda e: e.tensor_tensor(out=qn[:, 0:n].rearrange("p (h d) -> p h d", h=nh),
                                                  in0=qx[:, 0:n].rearrange("p (h d) -> p h d", h=nh),
                                                  in1=rs[:, 0:nh].unsqueeze(2).to_broadcast([128, nh, 64]), op=ALU.mult),
                        ["qx", "rsA"], ["qn"])
                    dve(lambda e: e.tensor_tensor(out=qn[:, 0:n].rearrange("p (h d) -> p h d", h=nh),
                                                  in0=qn[:, 0:n].rearrange("p (h d) -> p h d", h=nh),
                                                  in1=gain_bc[:].unsqueeze(1).to_broadcast([128, nh, 64]), op=ALU.mult),
                        ["qn", gkey], ["qn"])
                else:
                    act(lambda e: e.copy(out=qn[:, 0:n], in_=src_ap), src_keys, ["qn"])
                q3 = qn[:, 0:n].rearrange("p (h d) -> p h d", h=nh)
                o3 = qr[:, 0:n].rearrange("p (h d) -> p h d", h=nh)
                cosb = ck[:, 0:1, :].to_broadcast([128, nh, 32])
                sinb = ck[:, 1:2, :].to_broadcast([128, nh, 32])
                m = nh * 32
                v1 = t1[:, 0:m].rearrange("p (h d) -> p h d", h=nh); v2 = t2[:, 0:m].rearrange("p (h d) -> p h d", h=nh)
                v3 = t3[:, 0:m].rearrange("p (h d) -> p h d", h=nh); v4 = t4[:, 0:m].rearrange("p (h d) -> p h d", h=nh)
                dve(lambda e: e.tensor_tensor(out=v1, in0=q3[:, :, 0:32], in1=cosb, op=ALU.mult), ["qn", csk], ["t1"])
                dve(lambda e: e.tensor_tensor(out=v2, in0=q3[:, :, 32:64], in1=sinb, op=ALU.mult), ["qn", csk], ["t2"])
                dve(lambda e: e.tensor_tensor(out=o3[:, :, 0:32], in0=v1, in1=v2, op=ALU.subtract), ["t1", "t2"], [qrk])
                pool(lambda e: e.tensor_tensor(out=v3, in0=q3[:, :, 32:64], in1=cosb, op=ALU.mult), ["qn", csk], ["t3"])
                pool(lambda e: e.tensor_tensor(out=v4, in0=q3[:, :, 0:32], in1=sinb, op=ALU.mult), ["qn", csk], ["t4"])
                pool(lambda e: e.tensor_tensor(out=o3[:, :, 32:64], in0=v3, in1=v4, op=ALU.add), ["t3", "t4"], [qrk])
                sg = stg[sgi["i"] % 2]; sgk = "stg%d" % (sgi["i"] % 2); sgi["i"] += 1

                def part2():
                    tb = BK[7][:].bitcast(BF16)
                    pe(lambda e: [e.transpose(tb[0:64, h * 128:(h + 1) * 128], qr[:, h * 64:(h + 1) * 64], identb[:]) for h in range(nh)][-1],
                       [qrk, "identb"], ["bk7"])
                    act(lambda e: e.copy(out=sg[:, 0:nh * 128], in_=tb[0:64, 0:nh * 128]), ["bk7"], [sgk])
                    out_dram_fn(sg, sgk)
                return part2

            for b in range(NBLK):
                for tl in range(4):
                    ti = b * 4 + tl
                    r0 = ti * 128
                    xb = xt[tl % 2]; xk = "xt%d" % (tl % 2)
                    ck = cs[tl]; csk = "cs%d" % tl
                    P.dma("sp", xb[:], x[r0:r0 + 128, :], writes=[xk])
                    P.dma("sp", ck[:, 0, :], c_cos[:, ti, :], writes=[csk])
                    P.dma("sp", ck[:, 1, :], c_sin[:, ti, :], writes=[csk])
                    act(lambda e: e.activation(out=junk[:], in_=xb[:], func=AF.Square, accum_out=ss[:, 15:16]), [xk], ["junkA", "ss15"])
                    act(lambda e: e.activation(out=rs[:, 15:16], in_=ss[:, 15:16], func=AF.Sqrt, bias=epsb[:], scale=1.0 / D), ["ss15", "epsb"], ["rs15"])
                    dve(lambda e: e.reciprocal(out=rs[:, 15:16], in_=rs[:, 15:16]), ["rs15"], ["rs15"])
                    dve(lambda e: e.scalar_tensor_tensor(out=hh[:], in0=xb[:], scalar=rs[:, 15:16], in1=gmix_bc[:],
                                                         op0=ALU.mult, op1=ALU.mult), [xk, "rs15", "gmix_bc"], ["hh"])
                    tb6 = BK[6][:].bitcast(BF16)
                    pe(lambda e: [e.transpose(tb6[:, k * 128:(k + 1) * 128], hh[:, k * 128:(k + 1) * 128], identb[:]) for k in range(8)][-1],
                       ["hh", "identb"], ["bk6"])
                    act(lambda e: e.copy(out=hT[:, :, tl * 128:(tl + 1) * 128], in_=tb6.rearrange("p (k t) -> p k t", k=8)), ["bk6"], ["hT"])

                def tm_proj(tl, c0, n):
                    ab["i"] += 1
                    bk, bkk = BK[4 + ab["i"] % 2], "bk%d" % (4 + ab["i"] % 2)
                    pe(lambda e: [e.matmul(bk[:, 0:n], lhsT=hT[:, k, tl * 128:(tl + 1) * 128], rhs=w_tm[:, k, c0:c0 + n],
                                           start=(k == 0), stop=(k == 7)) for k in range(8)][-1], ["hT", "w_tm"], [bkk])
                    return bk, bkk

                def fm_proj(cc):
                    bk, bkk = nbank(4)
                    pe(lambda e: [e.matmul(bk[:], lhsT=w_fm[:, k, cc * 128:(cc + 1) * 128], rhs=hT[:, k, :],
                                           start=(k == 0), stop=(k == 7)) for k in range(8)][-1], ["hT", "w_fm"], [bkk])
                    return bk, bkk

                A_items = []
                X_items = []

                def mk_A(tl):
                    ti = b * 4 + tl
                    r0 = ti * 128
                    ck = cs[tl]; csk = "cs%d" % tl

                    def it_q():
                        bk, bkk = tm_proj(tl, C_QA, 512)
                        return norm_rope(bk[:, 0:512], [bkk], 8, gqa_bc, "gqa_bc", ck, csk,
                                         lambda sg, sgk: P.dma("sp", qT_s[ti], sg[:], reads=[sgk], writes=["qT_s%d" % ti]))

                    def it_kv():
                        bk, bkk = tm_proj(tl, C_KA, 256)
                        act(lambda e: e.copy(out=vst[:, :, 0:64], in_=bk[:, 128:256].rearrange("p (g d) -> p g d", g=2)), [bkk], ["vst"])
                        P.dma("sp", v_s[r0:r0 + 128, :], vst[:].rearrange("p g d -> p (g d)"), reads=["vst"], writes=["v_s%d" % ti])

                        def kout(sg, sgk):
                            for g in range(2):
                                P.dma("sp", kT_s[g, :, r0:r0 + 128], sg[:, g * 128:(g + 1) * 128], reads=[sgk], writes=["kT_s%d" % ti])
                        return norm_rope(bk[:, 0:128], [bkk], 2, gka_bc, "gka_bc", ck, csk, kout)

                    def it_qi():
                        bk, bkk = tm_proj(tl, C_QI, 512)
                        return norm_rope(bk[:, 0:512], [bkk], 8, None, None, ck, csk,
                                         lambda sg, sgk: P.dma("sp", qiT_s[ti], sg[:], reads=[sgk], writes=["qiT_s%d" % ti]))

                    def it_ki():
                        bk, bkk = tm_proj(tl, C_KI, 72)
                        act(lambda e: e.activation(out=wst[:], in_=bk[:, 64:72], func=AF.Copy, scale=float(8 ** -0.5 * 64 ** -0.5)), [bkk], ["wst"])
                        P.dma("sp", wi_s[r0:r0 + 128, :], wst[:], reads=["wst"], writes=["wi_s%d" % ti])
                        return norm_rope(bk[:, 0:64], [bkk], 1, gki_bc, "gki_bc", ck, csk,
                                         lambda sg, sgk: P.dma("sp", kiT_s[:, r0:r0 + 128], sg[:, 0:128], reads=[sgk], writes=["kiT_s%d" % ti]))
                    return [it_q, it_kv, it_qi, it_ki]

                for tl in range(4):
                    A_items.extend(mk_A(tl))

                def mk_conv(cp):
                    def it():
                        for c in cp:
                            bka, bkak = fm_proj(c)
                            bkg, bkgk = fm_proj(4 + c)
                            act(lambda e: e.activation(out=sgB[:], in_=bkg[:], func=AF.Sigmoid), [bkgk], ["sgB"])
                            dve(lambda e: e.tensor_tensor(out=uu[:, c, 30:542], in0=bka[:], in1=sgB[:], op=ALU.mult), [bkak, "sgB"], ["uu%d" % c])
                            cbk, cbkk = nbank(4)
                            for j in range(31):
                                d_ = dg[dgi["i"] % 8]; dk = "dg%d" % (dgi["i"] % 8); dgi["i"] += 1
                                pool(lambda e: e.tensor_scalar(out=d_[:], in0=identb[:], scalar1=cwT[:, c, j:j + 1], scalar2=0.0, op0=ALU.mult, op1=ALU.add),
                                     ["identb", "cwT"], [dk])
                                pe(lambda e: e.matmul(cbk[:], lhsT=d_[:], rhs=uu[:, c, j:j + 512], start=(j == 0), stop=(j == 30)),
                                   [dk, "uu%d" % c], [cbkk])
                            act(lambda e: e.activation(out=yy[:, c, :], in_=cbk[:], func=AF.Identity, bias=cbT[:, 0, c:c + 1]), [cbkk, "cbT"], ["yy%d" % c])
                            pool(lambda e: e.tensor_copy(out=uu[:, c, 0:30], in_=uu[:, c, 512:542]), ["uu%d" % c], ["uu%d" % c])
                    return it

                def it_ln():
                    yk = ["yy%d" % c for c in range(4)]
                    mbk, mbkk = nbank()
                    pe(lambda e: [e.matmul(mbk[:], lhsT=onesf[:], rhs=yy[:, c, :], start=(c == 0), stop=(c == 3)) for c in range(4)][-1],
                       ["onesf"] + yk, [mbkk])
                    for c in range(4):
                        dve(lambda e: e.scalar_tensor_tensor(out=yy[:, c, :], in0=mbk[:], scalar=-1.0 / 512, in1=yy[:, c, :],
                                                             op0=ALU.mult, op1=ALU.add), [mbkk, "yy%d" % c], ["yy%d" % c])
                    vbk, vbkk = nbank()
                    for c in range(4):
                        s_ = sq2[c % 2]; sk = "sq2_%d" % (c % 2)
                        act(lambda e: e.activation(out=s_[:], in_=yy[:, c, :], func=AF.Square), ["yy%d" % c], [sk])
                        pe(lambda e: e.matmul(vbk[:], lhsT=onesf[:], rhs=s_[:], start=(c == 0), stop=(c == 3)), ["onesf", sk], [vbkk])
                    act(lambda e: e.activation(out=rb[:], in_=vbk[:], func=AF.Sqrt, bias=epsb[:], scale=1.0 / 512), [vbkk, "epsb"], ["rbB"])
                    dve(lambda e: e.reciprocal(out=rb[:], in_=rb[:]), ["rbB"], ["rbB"])
                    for c in range(4):
                        dve(lambda e: e.tensor_tensor(out=yy[:, c, :], in0=yy[:, c, :], in1=rb[:], op=ALU.mult), ["yy%d" % c, "rbB"], ["yy%d" % c])
                        act(lambda e: e.activation(out=obT[:, c, :], in_=yy[:, c, :], func=AF.Silu, bias=lbT[:, 0, c:c + 1],
                                                   scale=lgT[:, 0, c:c + 1]), ["yy%d" % c, "lbT", "lgT"], ["obT"])

                def mk_mem(h):
                    def it():
                        bq, bqk = fm_proj(8 + h)
                        act(lambda e: e.activation(out=sgB[:], in_=bq[:], func=AF.Square), [bqk], ["sgB"])
                        b2, b2k = nbank()
                        pe(lambda e: e.matmul(b2[:], lhsT=onesf[:], rhs=sgB[:], start=True, stop=True), ["onesf", "sgB"], [b2k])
                        act(lambda e: e.activation(out=rb[:], in_=b2[:], func=AF.Sqrt, bias=epsb[:], scale=1.0 / 128), [b2k, "epsb"], ["rbB"])
                        dve(lambda e: e.reciprocal(out=rb[:], in_=rb[:]), ["rbB"], ["rbB"])
                        dve(lambda e: e.tensor_tensor(out=qmn[:], in0=bq[:], in1=rb[:], op=ALU.mult), [bqk, "rbB"], ["qmn"])
                        for mc in range(2):
                            b3, b3k = nbank()
                            pe(lambda e: e.matmul(b3[:], lhsT=kmT[:, h, mc * 128:(mc + 1) * 128], rhs=qmn[:], start=True, stop=True),
                               ["kmT", "qmn"], [b3k])
                            act(lambda e: e.activation(out=pT[mc][:], in_=b3[:], func=AF.Exp), [b3k], ["pTm%d" % mc])
                        bo, bok = nbank()
                        pe(lambda e: [e.matmul(bo[:], lhsT=vm[:, mc, h * 128:(h + 1) * 128], rhs=pT[mc][:], start=(mc == 0), stop=(mc == 1))
                                      for mc in range(2)][-1], ["vm", "pTm0", "pTm1"], [bok])
                        bd, bdk = nbank()
                        pe(lambda e: [e.matmul(bd[:], lhsT=onesb[:], rhs=pT[mc][:], start=(mc == 0), stop=(mc == 1)) for mc in range(2)][-1],
                           ["onesb", "pTm0", "pTm1"], [bdk])
                        dve(lambda e: e.reciprocal(out=rden[:], in_=bd[:]), [bdk], ["rden"])
                        dve(lambda e: e.tensor_tensor(out=omT[:, h, :], in0=bo[:], in1=rden[:], op=ALU.mult), [bok, "rden"], ["omT"])
                    return it

                def mk_gate(oc):
                    def it():
                        g1 = sq2[0]; g2 = sq2[1]; t2_ = rden
                        ms = mgst[oc % 2]; msk = "mgst%d" % (oc % 2)
                        bg0, bg0k = fm_proj(12 + oc)
                        act(lambda e: e.activation(out=ms[:, 0, :], in_=bg0[:], func=AF.Sigmoid, bias=bgT[:, 0, oc:oc + 1]), [bg0k, "bgT"], [msk])
                        bg1, bg1k = fm_proj(20 + oc)
                        act(lambda e: e.activation(out=g1[:], in_=bg1[:], func=AF.Sigmoid, bias=bgT[:, 0, 8 + oc:9 + oc]), [bg1k, "bgT"], ["sq2_0"])
                        bg2, bg2k = fm_proj(28 + oc)
                        act(lambda e: e.activation(out=g2[:], in_=bg2[:], func=AF.Sigmoid, bias=bgT[:, 0, 16 + oc:17 + oc]), [bg2k, "bgT"], ["sq2_1"])
                        pb, pbk = nbank()
                        pe(lambda e: [e.matmul(pb[:], lhsT=wbb[:, k, oc * 128:(oc + 1) * 128], rhs=obT[:, k, :], start=(k == 0), stop=(k == 3))
                                      for k in range(4)][-1], ["wbb", "obT"], [pbk])
                        dve(lambda e: e.tensor_tensor(out=g1[:], in0=pb[:], in1=g1[:], op=ALU.mult), [pbk, "sq2_0"], ["sq2_0"])
                        pm, pmk = nbank()
                        pe(lambda e: [e.matmul(pm[:], lhsT=wbm[:, k, oc * 128:(oc + 1) * 128], rhs=omT[:, k, :], start=(k == 0), stop=(k == 3))
                                      for k in range(4)][-1], ["wbm", "omT"], [pmk])
                        dve(lambda e: e.tensor_tensor(out=t2_[:], in0=pm[:], in1=g2[:], op=ALU.mult), [pmk, "sq2_1"], ["rden"])
                        pool(lambda e: e.tensor_tensor(out=ms[:, 1, :], in0=g1[:], in1=t2_[:], op=ALU.add), ["sq2_0", "rden"], [msk])
                        P.dma("sp", mg_s[b, :, oc], ms[:], reads=[msk], writes=["mg_s%d" % b])
                    return it

                X_items = [mk_conv((0, 1)), mk_conv((2, 3)), it_ln] + [mk_mem(h) for h in range(4)] + [mk_gate(oc) for oc in range(8)]
                na, nx = len(A_items), len(X_items)
                prev2 = None
                for n_ in range(na):
                    if n_ < nx:
                        X_items[n_]()
                    p2 = A_items[n_]()
                    if prev2 is not None:
                        prev2()
                    prev2 = p2
                for n_ in range(na, nx):
                    X_items[n_]()
                prev2()
            P.barrier()

        with scope() as p2:
            KT = sb(p2, "KT", [128, S], BF16)
            kiT = sb(p2, "kiT", [128, S], BF16)
            V = sb(p2, "V", [128, NT, 130], BF16)
            widx = sb(p2, "widx", [128, NT, 8], F32)
            score = sb(p2, "score", [128, S], F32)
            MB = sb(p2, "MB", [128, S], BF16)
            i4 = sb(p2, "i4", [128, 512], BF16)
            cb = sb(p2, "cb", [128, 128], F32)
            pow2 = sb(p2, "pow2", [128, KITER + 1], F32)
            allk = ["kT_s%d" % t for t in range(NT)]
            for g in range(2):
                P.dma("sp", KT[g * 64:(g + 1) * 64, :], kT_s[g], reads=allk, writes=["KT"])
            pool(lambda e: e.memset(kiT[64:128, :], 0.0), [], ["kiT"])
            P.dma("sp", kiT[0:64, :], kiT_s, reads=["kiT_s%d" % t for t in range(NT)], writes=["kiT"])
            for t0 in range(0, NT, 16):
                t1_ = min(NT, t0 + 16)
                P.dma("sp", V[:, t0:t1_, :], v_s[t0 * 128:t1_ * 128, :].rearrange("(n p) c -> p n c", p=128),
                      reads=["v_s%d" % t for t in range(t0, t1_)], writes=["V"])
            for t0 in range(0, NT, 8):
                t1_ = min(NT, t0 + 8)
                P.dma("sp", widx[:, t0:t1_, :], wi_s[t0 * 128:t1_ * 128, :].rearrange("(n p) c -> p n c", p=128),
                      reads=["wi_s%d" % t for t in range(t0, t1_)], writes=["widx"])
            P.dma("sp", i4[:], c_i4, writes=["i4"])
            conv_jobs = []
            for ex_ in range(32):
                for k8 in range(8):
                    conv_jobs.append((wub_s[ex_ * 128:(ex_ + 1) * 128, k8 * 512:(k8 + 1) * 512], w_up[ex_][k8 * 128:(k8 + 1) * 128, :], "wub_s"))
                for a_ in range(2):
                    conv_jobs.append((wdb_s[ex_ * 128:(ex_ + 1) * 128, a_ * D:(a_ + 1) * D], w_down[ex_][a_ * 128:(a_ + 1) * 128, :], "wdb_s"))

            def issue_conv(n_):
                for _ in range(n_):
                    if conv_jobs:
                        o_, i_, k_ = conv_jobs.pop(0)
                        P.dma("pool", o_, i_, writes=[])

            P.dma("sp", cb[:], c_cb, writes=["cb"])
            P.dma("sp", pow2[:], c_pow2, writes=["pow2"])
            qT = [sb(p2, "qT%d" % i, [128, 1024], BF16) for i in range(2)]
            qiT = [sb(p2, "qiT%d" % i, [128, 1024], BF16) for i in range(2)]
            for i_ in range(2):
                pool(lambda e: e.memset(qT[i_][:], 0.0), [], ["qT%d" % i_])
                pool(lambda e: e.memset(qiT[i_][:], 0.0), [], ["qiT%d" % i_])
            MB2 = [MB, sb(p2, "MBb", [128, S], BF16)]
            SC2 = [score, sb(p2, "scoreb", [128, S], F32)]
            diagw = sb(p2, "diagw", [128, 8, 128], BF16)
            RR = [sb(p2, "RR%d" % i, [128, 512], BF16) for i in range(4)]
            PT = [sb(p2, "PT%d" % i, [128, 512], BF16) for i in range(4)]
            sm2 = [sb(p2, "sm%d" % i, [128, 8], F32) for i in range(2)]
            wk2 = [sb(p2, "wk%d" % i, [128, KITER + 1], F32) for i in range(2)]
            jk = sb(p2, "jk", [128, 8], BF16)
            rdn = sb(p2, "rdn", [128, 8], F32)
            oa = sb(p2, "oa", [128, 512], BF16)
            oaT = [sb(p2, "oaT%d" % i, [128, 4, 128], BF16) for i in range(2)]
            OB = [BK[4], BK[5]]
            SC = BK[3]
            print("phase2 arena words", top["v"], "of", ARENA_F32)

            def stage_A(i):
                n = (i + 1) * 128
                qib = qiT[i % 2]; qik = "qiT%d" % (i % 2)
                MBi = MB2[i % 2]; MBk = "MB%d" % (i % 2)
                score = SC2[i % 2]; sck = "score%d" % (i % 2)
                sm = sm2[i % 2]; smk = "sm%d" % (i % 2)
                wk = wk2[i % 2]; wkk = "wk%d" % (i % 2)
                P.dma("sp", qib[0:64, :], qiT_s[i], reads=["qiT_s%d" % i], writes=[qik])
                for h in range(8):
                    pool(lambda e, h=h: e.tensor_scalar(out=diagw[:, h, :], in0=identb[:], scalar1=widx[:, i, h:h + 1], scalar2=None, op0=ALU.mult),
                         ["identb", "widx"], ["diagw"])
                nch = (n + 511) // 512
                units = [(c, h) for c in range(nch) for h in range(8)]
                Lb = {}

                def emit_L(u):
                    c, h = units[u]
                    k0 = c * 512
                    ncol = min(512, n - k0)
                    bk, bkk = nbank(3)
                    pe(lambda e: e.matmul(bk[:, 0:ncol], lhsT=qib[:, h * 128:(h + 1) * 128], rhs=kiT[:, k0:k0 + ncol], start=True, stop=True),
                       [qik, "kiT"], [bkk])
                    Lb[u] = (bk, bkk)

                for u in range(min(2, len(units))):
                    emit_L(u)
                for u in range(len(units)):
                    c, h = units[u]
                    k0 = c * 512
                    ncol = min(512, n - k0)
                    bk, bkk = Lb.pop(u)
                    R = RR[u % 4]; Rk = "RR%d" % (u % 4)
                    act(lambda e: e.activation(out=R[:, 0:ncol], in_=bk[:, 0:ncol], func=AF.Relu), [bkk], [Rk])
                    if u + 2 < len(units):
                        emit_L(u + 2)
                    scb = (BK[3], "bk3") if c % 2 == 0 else (BK[7], "bk7")
                    pe(lambda e: e.matmul(scb[0][:, 0:ncol], lhsT=diagw[:, h, :], rhs=R[:, 0:ncol], start=(h == 0), stop=(h == 7)),
                       ["diagw", Rk], [scb[1]])
                    if h == 7:
                        act(lambda e: e.copy(out=score[:, k0:k0 + ncol], in_=scb[0][:, 0:ncol]), [scb[1]], [sck])
                dve(lambda e: e.tensor_tensor(out=score[:, i * 128:(i + 1) * 128], in0=score[:, i * 128:(i + 1) * 128], in1=cb[:], op=ALU.add),
                    [sck, "cb"], [sck])
                if i >= 2:
                    dve(lambda e: e.tensor_reduce(out=sm[:, 0:1], in_=score[:, 0:n], axis=AX.X, op=ALU.max), [sck], [smk])
                    dve(lambda e: e.tensor_reduce(out=sm[:, 1:2], in_=score[:, 0:256], axis=AX.X, op=ALU.min), [sck], [smk])
                    dve(lambda e: e.tensor_tensor(out=sm[:, 2:3], in0=sm[:, 0:1], in1=sm[:, 1:2], op=ALU.subtract), [smk], [smk])
                    dve(lambda e: e.tensor_scalar(out=wk[:], in0=pow2[:], scalar1=sm[:, 2:3], scalar2=None, op0=ALU.mult), [smk, "pow2"], [wkk])
                    dve(lambda e: e.tensor_tensor(out=sm[:, 3:4], in0=sm[:, 1:2], in1=wk[:, 0:1], op=ALU.add), [smk, wkk], [smk])
                    for k in range(KITER):
                        dve(lambda e: e.tensor_scalar(out=jk[:, 0:1].to_broadcast([128, n]), in0=score[:, 0:n], scalar1=sm[:, 3:4], scalar2=None,
                                                      op0=ALU.is_ge, op1=ALU.add, accum_out=sm[:, 4:5]), [sck, smk], ["jk", smk])
                        dve(lambda e: e.tensor_scalar(out=sm[:, 5:6], in0=sm[:, 4:5], scalar1=TOPK - 0.5, scalar2=0.5, op0=ALU.is_ge, op1=ALU.subtract),
                            [smk], [smk])
                        dve(lambda e: e.scalar_tensor_tensor(out=sm[:, 3:4], in0=sm[:, 5:6], scalar=wk[:, k:k + 1], in1=sm[:, 3:4],
                                                             op0=ALU.mult, op1=ALU.add), [smk, wkk], [smk])
                    dve(lambda e: e.tensor_tensor(out=sm[:, 6:7], in0=sm[:, 3:4], in1=wk[:, KITER:KITER + 1], op=ALU.subtract), [smk, wkk], [smk])

            def stage_A2(i):
                n = (i + 1) * 128
                MBi = MB2[i % 2]; MBk = "MB%d" % (i % 2)
                score = SC2[i % 2]; sck = "score%d" % (i % 2)
                sm = sm2[i % 2]; smk = "sm%d" % (i % 2)
                if i >= 2:
                    dve(lambda e: e.tensor_scalar(out=MBi[:, 0:n], in0=score[:, 0:n], scalar1=sm[:, 6:7], scalar2=NEG, op0=ALU.is_lt, op1=ALU.mult),
                        [sck, smk], [MBk])
                else:
                    dve(lambda e: e.tensor_scalar(out=MBi[:, 0:n], in0=score[:, 0:n], scalar1=-1e29, scalar2=NEG, op0=ALU.is_lt, op1=ALU.mult),
                        [sck], [MBk])

            def stage_B(i):
                qb = qT[i % 2]; qk = "qT%d" % (i % 2)
                MBi = MB2[i % 2]; MBk = "MB%d" % (i % 2)
                P.dma("sp", qb[0:64, 0:512], qT_s[i][:, 0:512], reads=["qT_s%d" % i], writes=[qk])
                P.dma("sp", qb[64:128, 512:1024], qT_s[i][:, 512:1024], reads=["qT_s%d" % i], writes=[qk])
                units = [(j, g) for j in range(i + 1) for g in range(2)]
                Sb = {}

                def emit_S(u):
                    j, g = units[u]
                    bk, bkk = nbank(3)
                    pe(lambda e: [e.matmul(bk[:], lhsT=KT[:, j * 128:(j + 1) * 128], rhs=qb[:, g * 512:(g + 1) * 512], start=True, stop=False),
                                  e.matmul(bk[:], lhsT=MBi[:, j * 128:(j + 1) * 128], rhs=i4[:], start=False, stop=True)][-1],
                       ["KT", qk, MBk, "i4"], [bkk])
                    Sb[u] = (bk, bkk)

                for u in range(min(2, len(units))):
                    emit_S(u)
                for u in range(len(units)):
                    j, g = units[u]
                    bk, bkk = Sb.pop(u)
                    pt = PT[u % 4]; ptk = "PT%d" % (u % 4)
                    act(lambda e: e.activation(out=pt[:], in_=bk[:], func=AF.Exp, scale=0.125), [bkk], [ptk])
                    if u + 2 < len(units):
                        emit_S(u + 2)
                    ob = OB[g]
                    pe(lambda e: [e.matmul(ob[:, hh * 65:(hh + 1) * 65], lhsT=pt[:, hh * 128:(hh + 1) * 128], rhs=V[:, j, g * 65:(g + 1) * 65],
                                           start=(j == 0 and hh == 0), stop=(j == i and hh == 3), skip_group_check=True) for hh in range(4)][-1],
                       [ptk, "V"], ["bk%d" % (4 + g)])
                for g in range(2):
                    ob = OB[g]
                    o3 = ob[:, 0:260].rearrange("p (h d) -> p h d", h=4)
                    act(lambda e: e.activation(out=rdn[:, g * 4:(g + 1) * 4].unsqueeze(2), in_=o3[:, :, 64:65], func=AF.Ln), ["bk%d" % (4 + g)], ["rdn"])
                    act(lambda e: e.activation(out=rdn[:, g * 4:(g + 1) * 4], in_=rdn[:, g * 4:(g + 1) * 4], func=AF.Exp, scale=-1.0), ["rdn"], ["rdn"])
                    for hh in range(4):
                        h_ = g * 4 + hh
                        act(lambda e: e.activation(out=oa[:, h_ * 64:(h_ + 1) * 64], in_=o3[:, hh, 0:64], func=AF.Copy, scale=rdn[:, h_:h_ + 1]),
                            ["bk%d" % (4 + g), "rdn"], ["oa"])
                tb = BK[6][:].bitcast(BF16)
                pe(lambda e: [e.transpose(tb[:, k * 128:(k + 1) * 128], oa[:, k * 128:(k + 1) * 128], identb[:]) for k in range(4)][-1],
                   ["oa", "identb"], ["bk6"])
                ot = oaT[i % 2]; otk = "oaT%d" % (i % 2)
                act(lambda e: e.copy(out=ot[:], in_=tb[:, 0:512].rearrange("p (k t) -> p k t", k=4)), ["bk6"], [otk])
                P.dma("sp", oaT_s[:, :, i * 128:(i + 1) * 128], ot[:], reads=[otk], writes=["oaT_s%d" % i])

            stage_A(0); stage_A2(0)
            if NT > 1:
                stage_A(1); stage_A2(1)
            per_tile = (len(conv_jobs) + NT - 1) // NT
            for i in range(NT):
                if i + 2 < NT:
                    stage_A(i + 2)
                issue_conv(per_tile)
                stage_B(i)
                if i + 2 < NT:
                    stage_A2(i + 2)
            issue_conv(len(conv_jobs))
            P.barrier()

        out_evs = []
        with scope() as p3:
            I32 = mybir.dt.int32
            NM = 2 * S // 512
            gffn_bc = bcast_load(p3, "gffn_bc", g_ffn, D)
            wr = sb(p3, "wr", [128, 8, 36], BF16)
            wba = sb(p3, "wba", [128, 4, D], BF16)
            wo = sb(p3, "wo", [128, 8, D], BF16)
            wload_bf16(wba, w_br_a, 4, D, "wba")
            wload_bf16(wo, w_o, 8, D, "wo")
            brt = sb(p3, "brt", [128, 36], F32)
            P.dma("sp", brt[:, 0:4], b_rg.to_broadcast([128, 4]), writes=["brt"])
            P.dma("sp", brt[:, 4:36], b_re.to_broadcast([128, 32]), writes=["brt"])
            for k in range(8):
                P.dma("pool", wr[:, k, 0:4], w_rg[k * 128:(k + 1) * 128, :], writes=["wr"])
                P.dma("pool", wr[:, k, 4:36], w_re[k * 128:(k + 1) * 128, :], writes=["wr"])
            ltri = sb(p3, "ltri", [128, 128], BF16)
            P.dma("sp", ltri[:], c_ltri, writes=["ltri"])
            thrC = sb(p3, "thrC", [128, 32, NM], F32)
            P.dma("sp", thrC[:], c_thr, writes=["thrC"])
            tmC = sb(p3, "tmC", [128, 32, 32], F32)
            P.dma("sp", tmC[:], c_tm, writes=["tmC"])
            kC = sb(p3, "kC", [128, NTS, 32], F32)
            P.dma("sp", kC[:], c_kc, writes=["kC"])
            io8 = sb(p3, "io8", [128, 10], F32)
            P.dma("sp", io8[:], c_io8, writes=["io8"])
            M1s = sb(p3, "M1s", [128, NT, 32], F32)
            M2s = sb(p3, "M2s", [128, NT, 32], F32)
            rank1 = sb(p3, "rank1", [128, NT], F32)
            rank2 = sb(p3, "rank2", [128, NT], F32)
            W1s = sb(p3, "W1s", [128, NT], F32)
            W2s = sb(p3, "W2s", [128, NT], F32)
            cum = sb(p3, "cum", [128, 32], F32)
            pool(lambda e: e.memset(cum[:], 0.0), [], ["cum"])
            pos1 = sb(p3, "pos1", [128, NT], I32)
            pos2 = sb(p3, "pos2", [128, NT], I32)

            with scope() as pa:
                oab = sb(pa, "oab", [128, 4, 512], BF16)
                mgo = [sb(pa, "mgo%d" % i, [128, 2, 512], BF16) for i in range(2)]
                mTb = sb(pa, "mTb", [128, 8, 512], BF16)
                tt0 = sb(pa, "tt0", [128, 512], F32)
                x1b = [sb(pa, "x1b%d" % i, [128, D], F32) for i in range(4)]
                junk = sb(pa, "junk3", [128, D], BF16)
                h2 = [sb(pa, "h2_%d" % i, [128, D], BF16) for i in range(4)]
                h2Tt = [sb(pa, "h2Tt%d" % i, [128, 8, 128], BF16) for i in range(2)]
                ss4 = sb(pa, "ss4", [128, 4], F32)
                rs4 = sb(pa, "rs4", [128, 4], F32)
                lg4 = sb(pa, "lg4", [128, 4, 36], F32)
                gmx = sb(pa, "gmx", [128, 4], F32)
                oh4 = sb(pa, "oh4", [128, 4, 4], F32)
                eg4 = sb(pa, "eg4", [128, 4, 4], F32)
                se4 = sb(pa, "se4", [128, 4], F32)
                ps4 = sb(pa, "ps4", [128, 4], F32)
                em4 = sb(pa, "em4", [128, 4, 32], F32)
                em24 = sb(pa, "em24", [128, 4, 32], F32)
                m14 = sb(pa, "m14", [128, 4], F32)
                m24 = sb(pa, "m24", [128, 4], F32)
                mk14 = sb(pa, "mk14", [128, 4, 32], F32)
                mk24 = sb(pa, "mk24", [128, 4, 32], F32)
                dm4 = sb(pa, "dm4", [128, 4], F32)
                Mb4 = sb(pa, "Mb4", [128, 4, 32], BF16)
                cumT = sb(pa, "cumT", [128, 4, 32], F32)
                rk4 = sb(pa, "rk4", [128, 4, 32], F32)
                tmp4 = sb(pa, "tmp4", [128, 4, 32], F32)
                brt4 = brt[:].unsqueeze(1).to_broadcast([128, 4, 36])
                for blk in range(NBLK):
                    c0 = blk * 512
                    tb0 = blk * 4
                    P.dma("sp", oab[:], oaT_s[:, :, c0:c0 + 512], reads=["oaT_s%d" % (c0 // 128 + q_) for q_ in range(4)], writes=["oab"])
                    for oc in range(8):
                        mo = mgo[oc % 2]; mok = "mgo%d" % (oc % 2)
                        P.dma("sp", mo[:], mg_s[blk, :, oc], reads=["mg_s%d" % blk], writes=[mok])
                        bk, bkk = nbank()
                        pe(lambda e: [e.matmul(bk[:], lhsT=wba[:, k, oc * 128:(oc + 1) * 128], rhs=oab[:, k, :], start=(k == 0), stop=(k == 3))
                                      for k in range(4)][-1], ["wba", "oab"], [bkk])
                        dve(lambda e: e.tensor_tensor(out=tt0[:], in0=bk[:], in1=mo[:, 0, :], op=ALU.mult), [bkk, mok], ["tt0"])
                        pool(lambda e: e.tensor_tensor(out=mTb[:, oc, :], in0=tt0[:], in1=mo[:, 1, :], op=ALU.add), ["tt0", mok], ["mTb"])
                    for tl in range(4):
                        t = tb0 + tl
                        xb = x1b[tl]; xk = "x1b%d" % tl
                        P.dma("sp", xb[:], x[t * 128:(t + 1) * 128, :], writes=[xk])
                        for half in range(2):
                            bk, bkk = nbank()
                            pe(lambda e: [e.matmul(bk[:], lhsT=mTb[:, k, tl * 128:(tl + 1) * 128], rhs=wo[:, k, half * 512:(half + 1) * 512],
                                                   start=(k == 0), stop=(k == 7)) for k in range(8)][-1], ["mTb", "wo"], [bkk])
                            dve(lambda e: e.tensor_tensor(out=xb[:, half * 512:(half + 1) * 512], in0=bk[:],
                                                          in1=xb[:, half * 512:(half + 1) * 512], op=ALU.add), [bkk, xk], [xk])
                        P.dma("sp", x1_s[t * 128:(t + 1) * 128, :], xb[:], reads=[xk], writes=["x1_s%d" % t])
                        act(lambda e: e.activation(out=junk[:], in_=xb[:], func=AF.Square, accum_out=ss4[:, tl:tl + 1]), [xk], ["junk3", "ss4"])
                    act(lambda e: e.activation(out=rs4[:], in_=ss4[:], func=AF.Sqrt, bias=epsb[:], scale=1.0 / D), ["ss4", "epsb"], ["rs4"])
                    dve(lambda e: e.reciprocal(out=rs4[:], in_=rs4[:]), ["rs4"], ["rs4"])
                    for tl in range(4):
                        t = tb0 + tl
                        xb = x1b[tl]; xk = "x1b%d" % tl
                        hb = h2[tl]; hk = "h2_%d" % tl
                        hT_ = h2Tt[tl % 2]; hTk = "h2Tt%d" % (tl % 2)
                        dve(lambda e: e.scalar_tensor_tensor(out=hb[:], in0=xb[:], scalar=rs4[:, tl:tl + 1], in1=gffn_bc[:], op0=ALU.mult, op1=ALU.mult),
                            [xk, "rs4", "gffn_bc"], [hk])
                        P.dma("sp", h2_s[t * 128:(t + 1) * 128, :], hb[:], reads=[hk], writes=["h2_s%d" % t])
                        tb = BK[6][:].bitcast(BF16)
                        pe(lambda e: [e.transpose(tb[:, k * 128:(k + 1) * 128], hb[:, k * 128:(k + 1) * 128], identb[:]) for k in range(8)][-1],
                           [hk, "identb"], ["bk6"])
                        act(lambda e: e.copy(out=hT_[:], in_=tb.rearrange("p (k t) -> p k t", k=8)), ["bk6"], [hTk])
                        pe(lambda e: [e.matmul(BK[7][:, tl * 64:tl * 64 + 36], lhsT=hT_[:, k, :], rhs=wr[:, k, :], start=(k == 0), stop=(k == 7))
                                      for k in range(8)][-1], [hTk, "wr"], ["bk7"])
                    lgv = BK[7][:, 0:256].rearrange("p (t c) -> p t c", t=4)[:, :, 0:36]
                    dve(lambda e: e.tensor_tensor(out=lg4[:], in0=lgv, in1=brt4, op=ALU.add), ["bk7", "brt"], ["lg4"])
                    gl = lg4[:, :, 0:4]
                    el = lg4[:, :, 4:36]
                    dve(lambda e: e.tensor_reduce(out=gmx[:], in_=gl, axis=AX.X, op=ALU.max), ["lg4"], ["gmx"])
                    dve(lambda e: e.tensor_tensor(out=oh4[:], in0=gl, in1=gmx[:].unsqueeze(2).to_broadcast([128, 4, 4]), op=ALU.is_ge), ["lg4", "gmx"], ["oh4"])
                    dve(lambda e: e.tensor_tensor(out=eg4[:], in0=gl, in1=gmx[:].unsqueeze(2).to_broadcast([128, 4, 4]), op=ALU.subtract), ["lg4", "gmx"], ["eg4"])
                    act(lambda e: e.activation(out=eg4[:], in_=eg4[:], func=AF.Exp), ["eg4"], ["eg4"])
                    dve(lambda e: e.tensor_reduce(out=se4[:], in_=eg4[:], axis=AX.X, op=ALU.add), ["eg4"], ["se4"])
                    dve(lambda e: e.reciprocal(out=ps4[:], in_=se4[:]), ["se4"], ["ps4"])
                    dve(lambda e: e.tensor_scalar(out=oh4[:], in0=oh4[:], scalar1=1.0, scalar2=1e9, op0=ALU.subtract, op1=ALU.mult), ["oh4"], ["oh4"])
                    dve(lambda e: e.tensor_tensor(out=em4[:].rearrange("p t (g x) -> p t g x", g=4), in0=el.rearrange("p t (g x) -> p t g x", g=4),
                                                  in1=oh4[:].unsqueeze(3).to_broadcast([128, 4, 4, 8]), op=ALU.add), ["lg4", "oh4"], ["em4"])
                    dve(lambda e: e.tensor_reduce(out=m14[:], in_=em4[:], axis=AX.X, op=ALU.max), ["em4"], ["m14"])
                    dve(lambda e: e.tensor_tensor(out=mk14[:], in0=em4[:], in1=m14[:].unsqueeze(2).to_broadcast([128, 4, 32]), op=ALU.is_ge), ["em4", "m14"], ["mk14"])
                    dve(lambda e: e.scalar_tensor_tensor(out=em24[:].rearrange("p t e -> p (t e)"), in0=mk14[:].rearrange("p t e -> p (t e)"), scalar=-1e9,
                                                         in1=em4[:].rearrange("p t e -> p (t e)"), op0=ALU.mult, op1=ALU.add), ["mk14", "em4"], ["em24"])
                    dve(lambda e: e.tensor_reduce(out=m24[:], in_=em24[:], axis=AX.X, op=ALU.max), ["em24"], ["m24"])
                    dve(lambda e: e.tensor_tensor(out=mk24[:], in0=em24[:], in1=m24[:].unsqueeze(2).to_broadcast([128, 4, 32]), op=ALU.is_ge), ["em24", "m24"], ["mk24"])
                    dve(lambda e: e.tensor_tensor(out=dm4[:], in0=m24[:], in1=m14[:], op=ALU.subtract), ["m24", "m14"], ["dm4"])
                    act(lambda e: e.activation(out=dm4[:], in_=dm4[:], func=AF.Exp), ["dm4"], ["dm4"])
                    dve(lambda e: e.tensor_scalar(out=dm4[:], in0=dm4[:], scalar1=1.0, scalar2=None, op0=ALU.add), ["dm4"], ["dm4"])
                    dve(lambda e: e.reciprocal(out=dm4[:], in_=dm4[:]), ["dm4"], ["dm4"])
                    dve(lambda e: e.tensor_tensor(out=W1s[:, tb0:tb0 + 4], in0=dm4[:], in1=ps4[:], op=ALU.mult), ["dm4", "ps4"], ["W1s"])
                    dve(lambda e: e.tensor_tensor(out=W2s[:, tb0:tb0 + 4], in0=ps4[:], in1=W1s[:, tb0:tb0 + 4], op=ALU.subtract), ["ps4", "W1s"], ["W2s"])
                    dve(lambda e: e.tensor_tensor(out=Mb4[:], in0=mk14[:], in1=mk24[:], op=ALU.add), ["mk14", "mk24"], ["Mb4"])
                    pe(lambda e: [[e.matmul(BK[7][:, 256 + tl * 32:256 + (tl + 1) * 32], lhsT=ltri[:], rhs=Mb4[:, tl, :], start=True, stop=True),
                                   e.matmul(BK[7][:, 384 + tl * 32:384 + (tl + 1) * 32], lhsT=onesb[:], rhs=Mb4[:, tl, :], start=True, stop=True)][-1]
                                  for tl in range(4)][-1], ["ltri", "onesb", "Mb4"], ["bk7"])
                    pool(lambda e: e.tensor_copy(out=cumT[:, 0, :], in_=cum[:]), ["cum"], ["cumT"])
                    for tl in range(1, 4):
                        dve(lambda e: e.tensor_tensor(out=cumT[:, tl, :], in0=BK[7][:, 384 + (tl - 1) * 32:384 + tl * 32], in1=cumT[:, tl - 1, :], op=ALU.add),
                            ["bk7", "cumT"], ["cumT"])
                    dve(lambda e: e.tensor_tensor(out=cum[:], in0=BK[7][:, 480:512], in1=cumT[:, 3, :], op=ALU.add), ["bk7", "cumT"], ["cum"])
                    dve(lambda e: e.tensor_tensor(out=rk4[:], in0=BK[7][:, 256:384].rearrange("p (t e) -> p t e", t=4), in1=cumT[:], op=ALU.add),
                        ["bk7", "cumT"], ["rk4"])
                    dve(lambda e: e.tensor_tensor(out=tmp4[:], in0=mk14[:], in1=rk4[:], op=ALU.mult), ["mk14", "rk4"], ["tmp4"])
                    dve(lambda e: e.tensor_reduce(out=rank1[:, tb0:tb0 + 4], in_=tmp4[:], axis=AX.X, op=ALU.add), ["tmp4"], ["rank1"])
                    dve(lambda e: e.tensor_tensor(out=tmp4[:], in0=mk24[:], in1=rk4[:], op=ALU.mult), ["mk24", "rk4"], ["tmp4"])
                    dve(lambda e: e.tensor_reduce(out=rank2[:, tb0:tb0 + 4], in_=tmp4[:], axis=AX.X, op=ALU.add), ["tmp4"], ["rank2"])
                    pool(lambda e: e.tensor_copy(out=M1s[:, tb0:tb0 + 4, :], in_=mk14[:]), ["mk14"], ["M1s"])
                    pool(lambda e: e.tensor_copy(out=M2s[:, tb0:tb0 + 4, :], in_=mk24[:]), ["mk24"], ["M2s"])
                P.barrier()

            Ek = sb(p3, "Ek", [128, NTS], F32)
            idxu = sb(p3, "idxu", [128, NTS, 8], I32)
            idxd = sb(p3, "idxd", [128, NTS, 2], I32)
            with scope() as pb:
                big = sb(pb, "bigtmp", [128, max(NT, NTS, 32) * 32], F32)
                ntl = sb(pb, "ntl", [128, 32], F32)
                offt = sb(pb, "offt", [128, 32], F32)
                offs = sb(pb, "offs", [128, 32], F32)
                pf = sb(pb, "pf", [128, NT], F32)
                ef = sb(pb, "ef", [128, NTS, 8], F32)
                b3 = big[:, 0:32 * NM].rearrange("p (e m) -> p e m", e=32)
                dve(lambda e: e.tensor_tensor(out=b3, in0=cum[:].unsqueeze(2).to_broadcast([128, 32, NM]), in1=thrC[:], op=ALU.is_gt),
                    ["cum", "thrC"], ["bigtmp"])
                dve(lambda e: e.tensor_reduce(out=ntl[:], in_=b3, axis=AX.X, op=ALU.add), ["bigtmp"], ["ntl"])
                b4 = big[:, 0:1024].rearrange("p (e f) -> p e f", e=32)
                dve(lambda e: e.tensor_tensor(out=b4, in0=ntl[:].unsqueeze(1).to_broadcast([128, 32, 32]), in1=tmC[:], op=ALU.mult),
                    ["ntl", "tmC"], ["bigtmp"])
                dve(lambda e: e.tensor_reduce(out=offt[:], in_=b4, axis=AX.X, op=ALU.add), ["bigtmp"], ["offt"])
                dve(lambda e: e.tensor_scalar(out=offs[:], in0=offt[:], scalar1=512.0, scalar2=None, op0=ALU.mult), ["offt"], ["offs"])
                b5 = big[:, 0:NTS * 32].rearrange("p (k e) -> p k e", k=NTS)
                dve(lambda e: e.tensor_tensor(out=b5, in0=offt[:].unsqueeze(1).to_broadcast([128, NTS, 32]), in1=kC[:], op=ALU.is_le),
                    ["offt", "kC"], ["bigtmp"])
                dve(lambda e: e.tensor_reduce(out=Ek[:], in_=b5, axis=AX.X, op=ALU.add), ["bigtmp"], ["Ek"])
                dve(lambda e: e.tensor_scalar(out=Ek[:], in0=Ek[:], scalar1=-1.0, scalar2=None, op0=ALU.add), ["Ek"], ["Ek"])
                dve(lambda e: e.scalar_tensor_tensor(out=ef[:, :, 0], in0=Ek[:], scalar=128.0,
                                                     in1=io8[:, 8:9].to_broadcast([128, NTS]), op0=ALU.mult, op1=ALU.add),
                    ["Ek", "io8"], ["ef"])
                dve(lambda e: e.tensor_copy(out=idxu[:, :, 0], in_=ef[:, :, 0]), ["ef"], ["idxu"])
                for (Ms, Mk, rnk, rkk, ps, pk) in ((M1s, "M1s", rank1, "rank1", pos1, "pos1"), (M2s, "M2s", rank2, "rank2", pos2, "pos2")):
                    b6 = big[:, 0:NT * 32].rearrange("p (t e) -> p t e", t=NT)
                    dve(lambda e: e.tensor_tensor(out=b6, in0=Ms[:], in1=offs[:].unsqueeze(1).to_broadcast([128, NT, 32]), op=ALU.mult),
                        [Mk, "offs"], ["bigtmp"])
                    dve(lambda e: e.tensor_reduce(out=pf[:], in_=b6, axis=AX.X, op=ALU.add), ["bigtmp"], ["pf"])
                    dve(lambda e: e.tensor_tensor(out=pf[:], in0=pf[:], in1=rnk[:], op=ALU.add), ["pf", rkk], ["pf"])
                    dve(lambda e: e.tensor_copy(out=ps[:], in_=pf[:]), ["pf"], [pk])
                hrow = [sb(pb, "hrow%d" % i, [128, D], BF16) for i in range(3)]
                for t in range(NT):
                    hb = hrow[t % 3]; hk = "hrow%d" % (t % 3)
                    P.dma("sp", hb[:], h2_s[t * 128:(t + 1) * 128, :], reads=["h2_s%d" % t], writes=[hk])
                    P.idma(Hs[:, :], hb[:, :], pos1[:, t:t + 1], False, reads=[hk, "pos1"], writes=[])
                    P.idma(Hs[:, :], hb[:, :], pos2[:, t:t + 1], False, reads=[hk, "pos2"], writes=[])
                P.barrier()

            with scope() as pc:
                wu = [sb(pc, "wu%d" % i, [128, 8, 512], BF16) for i in range(3)]
                wd = [sb(pc, "wd%d" % i, [128, 2, D], BF16) for i in range(3)]
                hs = [sb(pc, "hs%d" % i, [128, D], BF16) for i in range(4)]
                HT = [sb(pc, "HT%d" % i, [128, 8, 512], BF16) for i in range(2)]
                sa = [sb(pc, "sa%d" % i, [128, 512], F32) for i in range(2)]
                gg = [sb(pc, "gg%d" % i, [128, 2, 512], BF16) for i in range(2)]
                ysb = [sb(pc, "ysb%d" % i, [128, D], F32) for i in range(2)]
                wub_v = wub_s
                wdb_v = wdb_s

                def load_w(k):
                    P.idma(wu[k % 3][:].rearrange("p k c -> p (k c)"), wub_v[:, :], idxu[:, k, 0:1], True, reads=["idxu"], writes=["wu%d" % (k % 3)])
                    P.idma(wd[k % 3][:].rearrange("p a c -> p (a c)"), wdb_v[:, :], idxu[:, k, 0:1], True, reads=["idxu"], writes=["wd%d" % (k % 3)])

                def load_h(k):
                    HTk = HT[k % 2]; HTkk = "HT%d" % (k % 2)
                    for sub in range(4):
                        hb = hs[sub]; hk = "hs%d" % sub
                        r0 = k * 512 + sub * 128
                        P.dma("sp", hb[:], Hs[r0:r0 + 128, :], writes=[hk])
                        tbk = "bk%d" % (6 + sub % 2)
                        tb = BK[6 + sub % 2][:].bitcast(BF16)
                        pe(lambda e: [e.transpose(tb[:, kk * 128:(kk + 1) * 128], hb[:, kk * 128:(kk + 1) * 128], identb[:]) for kk in range(8)][-1],
                           [hk, "identb"], [tbk])
                        if sub % 2 == 0:
                            act(lambda e: e.copy(out=HTk[:, :, sub * 128:(sub + 1) * 128], in_=tb.rearrange("p (k t) -> p k t", k=8)), [tbk], [HTkk])
                        else:
                            dve(lambda e: e.tensor_copy(out=HTk[:, :, sub * 128:(sub + 1) * 128], in_=tb.rearrange("p (k t) -> p k t", k=8)), [tbk], [HTkk])

                pend = None

                def emit_down(pd):
                    k_, ci_ = pd
                    wdb_ = wd[k_ % 3]; wdk_ = "wd%d" % (k_ % 3)
                    for sub in range(4):
                        yb = ysb[sub % 2]; yk = "ysb%d" % (sub % 2)
                        for half in range(2):
                            di = 4 + ((sub * 2 + half) % 2)
                            pe(lambda e: [e.matmul(BK[di][:], lhsT=gg[ci_][:, a, sub * 128:(sub + 1) * 128], rhs=wdb_[:, a, half * 512:(half + 1) * 512],
                                                   start=(a == 0), stop=(a == 1)) for a in range(2)][-1], ["gg%d" % ci_, wdk_], ["bk%d" % di])
                            if half == 0:
                                act(lambda e: e.copy(out=yb[:, 0:512], in_=BK[di][:]), ["bk%d" % di], [yk])
                            else:
                                dve(lambda e: e.tensor_copy(out=yb[:, 512:1024], in_=BK[di][:]), ["bk%d" % di], [yk])
                        r0 = k_ * 512 + sub * 128
                        P.dma("sp", Ysc[r0:r0 + 128, :], yb[:], reads=[yk], writes=[])

                load_w(0)
                if NTS > 1:
                    load_w(1)
                load_h(0)
                for k in range(NTS):
                    ci = k % 2
                    wub = wu[k % 3]; wuk = "wu%d" % (k % 3)
                    HTk = HT[k % 2]; HTkk = "HT%d" % (k % 2)
                    for a in range(2):
                        for fc in (a, 2 + a):
                            pe(lambda e: [e.matmul(BK[fc][:], lhsT=wub[:, kk, fc * 128:(fc + 1) * 128], rhs=HTk[:, kk, :],
                                                   start=(kk == 0), stop=(kk == 7)) for kk in range(8)][-1], [wuk, HTkk], ["bk%d" % fc])
                        act(lambda e: e.activation(out=sa[a][:], in_=BK[a][:], func=AF.Silu), ["bk%d" % a], ["sa%d" % a])
                        dve(lambda e: e.tensor_tensor(out=gg[ci][:, a, :], in0=BK[2 + a][:], in1=sa[a][:], op=ALU.mult),
                            ["bk%d" % (2 + a), "sa%d" % a], ["gg%d" % ci])
                    if k + 1 < NTS:
                        load_h(k + 1)
                    if pend is not None:
                        emit_down(pend)
                    pend = (k, ci)
                    if k + 2 < NTS:
                        load_w(k + 2)
                emit_down(pend)
                P.barrier()

            with scope() as pd_:
                xf = [sb(pd_, "xf%d" % i, [128, D], F32) for i in range(2)]
                y1 = [sb(pd_, "y1_%d" % i, [128, D], F32) for i in range(2)]
                y2 = [sb(pd_, "y2_%d" % i, [128, D], F32) for i in range(2)]
                for t in range(NT):
                    xb = xf[t % 2]; xk = "xf%d" % (t % 2)
                    a1 = y1[t % 2]; a1k = "y1_%d" % (t % 2)
                    a2 = y2[t % 2]; a2k = "y2_%d" % (t % 2)
                    P.dma("sp", xb[:], x1_s[t * 128:(t + 1) * 128, :], reads=["x1_s%d" % t], writes=[xk])
                    P.idma(a1[:, :], Ysc[:, :], pos1[:, t:t + 1], True, reads=["pos1"], writes=[a1k])
                    P.idma(a2[:, :], Ysc[:, :], pos2[:, t:t + 1], True, reads=["pos2"], writes=[a2k])
                    dve(lambda e: e.scalar_tensor_tensor(out=xb[:], in0=a1[:], scalar=W1s[:, t:t + 1], in1=xb[:], op0=ALU.mult, op1=ALU.add),
                        [a1k, "W1s", xk], [xk])
                    dve(lambda e: e.scalar_tensor_tensor(out=xb[:], in0=a2[:], scalar=W2s[:, t:t + 1], in1=xb[:], op0=ALU.mult, op1=ALU.add),
                        [a2k, "W2s", xk], [xk])
                    out_evs.append(P.dma("sp", out[t * 128:(t + 1) * 128, :], xb[:], reads=[xk], writes=["out%d" % t]))
            P.final_wait("sp", out_evs)
        P.emit()
    return nc


def _consts(S):
    NT = S // 128
    bf = ml_dtypes.bfloat16
    c = {}
    c["c_identb"] = np.eye(128, dtype=np.float32).astype(bf)
    c["c_identf"] = np.eye(128, dtype=np.float32)
    c["c_i4"] = np.tile(np.eye(128, dtype=np.float32), (1, 4)).astype(bf)
    t = np.arange(128)[:, None]; s = np.arange(128)[None, :]
    c["c_cb"] = np.where(s <= t, 0.0, -1e30).astype(np.float32)
    half = 32
    inv = (10000.0 ** (-np.arange(half, dtype=np.float32) / half)).astype(np.float32)
    pos = np.arange(S, dtype=np.float32)
    ang = (pos[:, None] * inv[None, :]).astype(np.float32)
    cos = np.cos(ang).astype(np.float32).reshape(NT, 128, 32).transpose(1, 0, 2)
    sin = np.sin(ang).astype(np.float32).reshape(NT, 128, 32).transpose(1, 0, 2)
    c["c_cos"] = np.ascontiguousarray(cos); c["c_sin"] = np.ascontiguousarray(sin)
    NTS = (2 * S + 32 * 511 + 511) // 512
    NM = 2 * S // 512
    kk = np.arange(128)
    c["c_ltri"] = (kk[:, None] < kk[None, :]).astype(np.float32).astype(bf)
    c["c_thr"] = np.ascontiguousarray(np.broadcast_to((512.0 * np.arange(NM, dtype=np.float32))[None, None, :], (128, 32, NM))).astype(np.float32)
    ee = np.arange(32)
    c["c_tm"] = np.ascontiguousarray(np.broadcast_to((ee[None, :] < ee[:, None]).astype(np.float32)[None], (128, 32, 32)))
    c["c_kc"] = np.ascontiguousarray(np.broadcast_to(np.arange(NTS, dtype=np.float32)[None, :, None], (128, NTS, 32))).astype(np.float32)
    io = np.zeros((128, 10), np.float32)
    for k8 in range(8):
        io[:, k8] = k8 * 128 + np.arange(128)
    for a in range(2):
        io[:, 8 + a] = a * 128 + np.arange(128)
    c["c_io8"] = io
    c["c_pow2"] = np.tile((2.0 ** -(np.arange(KITER + 1) + 1.0)).astype(np.float32)[None, :], (128, 1))
    return c


def _prep(inp, S):
    f = lambda a: np.ascontiguousarray(np.asarray(a, dtype=np.float32))
    w = {}
    w["g_mix"] = f(inp["g_mix"]).reshape(1, D); w["w_in"] = f(inp["w_in"]).reshape(D, 5960)
    w["b_gate"] = f(inp["b_gate"]).reshape(24, 128)
    w["g_qa"] = f(inp["g_qa"]).reshape(1, 64); w["g_ka"] = f(inp["g_ka"]).reshape(1, 64); w["g_idx_k"] = f(inp["g_idx_k"]).reshape(1, 64)
    w["conv_w"] = f(inp["conv_w"]).reshape(31, 512); w["conv_b"] = f(inp["conv_b"]).reshape(4, 128)
    w["ln_g"] = f(inp["ln_g"]).reshape(4, 128); w["ln_b"] = f(inp["ln_b"]).reshape(4, 128)
    w["g_mem"] = f(inp["g_mem"]).reshape(1, D); w["w_mem_kv"] = f(inp["w_mem_kv"]).reshape(D, 1024)
    w["g_qm"] = f(inp["g_qm"]).reshape(1, 128); w["g_km"] = f(inp["g_km"]).reshape(1, 128)
    w["w_br_a"] = f(inp["w_br_a"]).reshape(512, D); w["w_br_b"] = f(inp["w_br_b"]).reshape(512, D); w["w_br_m"] = f(inp["w_br_m"]).reshape(512, D)
    w["w_o"] = f(inp["w_o"]).reshape(D, D); w["g_ffn"] = f(inp["g_ffn"]).reshape(1, D)
    w["w_rg"] = f(inp["w_rg"]).reshape(D, 4); w["b_rg"] = f(inp["b_rg"]).reshape(1, 4)
    w["w_re"] = f(inp["w_re"]).reshape(D, 32); w["b_re"] = f(inp["b_re"]).reshape(1, 32)
    w["w_up"] = f(inp["w_up"]).reshape(32, D, 512); w["w_down"] = f(inp["w_down"]).reshape(32, 256, D)
    w.update(_consts(S))
    return w


def kernel(**inputs):
    x = np.asarray(inputs["x"], dtype=np.float32)
    mem = np.asarray(inputs["mem"], dtype=np.float32)
    B, S, _ = x.shape
    shared = _prep(inputs, S)
    nc = build(S)
    in_maps = []
    for bi in range(B):
        m = dict(shared)
        m["x"] = np.ascontiguousarray(x[bi])
        m["mem"] = np.ascontiguousarray(mem[bi])
        in_maps.append(m)
    res = run_bass_kernel_spmd(nc, in_maps, core_ids=list(range(B)))
    return np.stack([np.asarray(r["out"]).reshape(S, D) for r in res.results], axis=0).astype(np.float32)
```

```python
import numpy as np
import ml_dtypes
from contextlib import ExitStack
import concourse.bass as bass
import concourse.mybir as mybir
from concourse.bass_utils import run_bass_kernel_spmd

F32 = mybir.dt.float32
BF16 = mybir.dt.bfloat16
ALU = mybir.AluOpType
AF = mybir.ActivationFunctionType
AX = mybir.AxisListType

D = 1024
EPS = 1e-6
C_QA, C_KA, C_VA, C_QI, C_KI, C_WI, C_CV, C_QM, C_GT = 0, 512, 640, 768, 1280, 1344, 1352, 2376, 2888
N_TM = 1352
N_FM = 5960 - 1352
KITER = 14
TOPK = 256
NEG = -30000.0

ENGS = ("pe", "act", "dve", "pool", "sp")
SEM_LIMIT = 30000
N_DMA_SEMS = 32


class _Rec:
    def __init__(self):
        self.calls = []

    def __getattr__(self, name):
        def f(*a, **kw):
            self.calls.append((name, a, kw))
            return self
        return f


def _replayer(calls):
    def run(e):
        r = None
        for (name, a, kw) in calls:
            r = getattr(e, name)(*a, **kw)
        return r
    return run


class Prog:
    def __init__(self, nc, stack):
        self.nc = nc
        self.stack = stack
        self.q = {e: [] for e in ENGS}
        self.nsem = 0
        self.cur = {}
        self.cnt = {}
        for e in ENGS:
            self.cur[e] = self._new_sem(e)
            self.cnt[e] = 0
        self.dma_sems = [self._new_sem("dma%d" % i) for i in range(N_DMA_SEMS)]
        self.dma_cnt = [0] * N_DMA_SEMS
        self.n_hw = 16
        self.dma_next_hw = 0
        self.dma_next_sw = 0
        self.last_w = {}
        self.readers = {}
        self.seen = {e: {} for e in ENGS}
        self.n_ops = 0

    def _new_sem(self, name):
        self.nsem += 1
        return self.stack.enter_context(self.nc.semaphore("s_%s_%d" % (name, self.nsem)))

    def _deps(self, reads, writes, skip_src=None):
        ev = []
        for k in reads:
            w = self.last_w.get(k)
            if w is not None and w[0] != skip_src:
                ev.append(w[1])
        for k in writes:
            w = self.last_w.get(k)
            if w is not None and w[0] != skip_src:
                ev.append(w[1])
            r = self.readers.get(k)
            if r:
                for src, e in r.items():
                    if src != skip_src:
                        ev.append(e)
        return ev

    def _waits(self, eng, evs):
        best = {}
        for (sem, val) in evs:
            i = id(sem)
            if self.seen[eng].get(i, 0) < val:
                if i not in best or best[i][1] < val:
                    best[i] = (sem, val)
        out = []
        for i, (sem, val) in best.items():
            self.seen[eng][i] = val
            out.append((sem, val))
        return out

    def _record(self, ev, reads, writes, src):
        for k in writes:
            self.last_w[k] = (src, ev)
            self.readers[k] = {}
        for k in reads:
            self.readers.setdefault(k, {})[src] = ev

    def op(self, eng, fn, reads=(), writes=()):
        waits = self._waits(eng, self._deps(reads, writes, "pe" if eng == "pe" else None))
        if self.cnt[eng] >= SEM_LIMIT:
            self.cur[eng] = self._new_sem(eng)
            self.cnt[eng] = 0
        self.cnt[eng] += 1
        sem = self.cur[eng]
        ev = (sem, self.cnt[eng])
        rec = _Rec()
        fn(rec)
        assert rec.calls
        self.q[eng].append((_replayer(rec.calls), waits, sem, 1))
        self._record(ev, reads, writes, eng)
        self.n_ops += 1
        return ev

    def _slot(self, eng):
        if eng == "pool":
            s = self.n_hw + self.dma_next_sw
            self.dma_next_sw = (self.dma_next_sw + 1) % (N_DMA_SEMS - self.n_hw)
        else:
            s = self.dma_next_hw
            self.dma_next_hw = (self.dma_next_hw + 1) % self.n_hw
        return s

    def dma(self, eng, out, in_, reads=(), writes=(), **kw):
        s = self._slot(eng)
        sem = self.dma_sems[s]
        evs = self._deps(reads, writes)
        if self.dma_cnt[s] > 0:
            evs.append((sem, self.dma_cnt[s]))
        waits = self._waits(eng, evs)
        self.dma_cnt[s] += 16
        ev = (sem, self.dma_cnt[s])

        def fn(e, out=out, in_=in_, kw=kw):
            return e.dma_start(out=out, in_=in_, **kw)

        self.q[eng].append((fn, waits, sem, 16))
        self._record(ev, reads, writes, ("dma", s))
        self.n_ops += 1
        return ev

    def idma(self, out, in_, idx_ap, gather, reads=(), writes=(), bound=None):
        eng = "pool"
        s = self._slot(eng)
        sem = self.dma_sems[s]
        evs = self._deps(reads, writes)
        if self.dma_cnt[s] > 0:
            evs.append((sem, self.dma_cnt[s]))
        waits = self._waits(eng, evs)
        self.dma_cnt[s] += 16
        ev = (sem, self.dma_cnt[s])
        off = bass.IndirectOffsetOnAxis(ap=idx_ap, axis=0)

        def fn(e):
            kw = {}
            if bound is not None:
                kw = dict(bounds_check=bound, oob_is_err=False)
            if gather:
                return e.indirect_dma_start(out=out, out_offset=None, in_=in_, in_offset=off, **kw)
            return e.indirect_dma_start(out=out, out_offset=off, in_=in_, in_offset=None, **kw)

        self.q[eng].append((fn, waits, sem, 16))
        self._record(ev, reads, writes, ("dma", s))
        self.n_ops += 1
        return ev

    def barrier(self):
        evs = [(self.cur[e], self.cnt[e]) for e in ENGS if self.cnt[e] > 0]
        evs += [(self.dma_sems[s], self.dma_cnt[s]) for s in range(N_DMA_SEMS) if self.dma_cnt[s] > 0]
        for e in ENGS:
            w = self._waits(e, evs)
            if w:
                self.q[e].append((None, w, None, 0))

    def final_wait(self, eng, evs):
        waits = self._waits(eng, evs)
        self.q[eng].append((None, waits, None, 0))

    def emit(self):
        nc = self.nc
        with nc.Block() as block:
            def mk(q):
                def body(e):
                    for (fn, waits, sem, inc) in q:
                        for (ws, wv) in waits:
                            e.wait_ge(ws, wv)
                        if fn is not None:
                            fn(e).then_inc(sem, inc)
                return body
            block.tensor(mk(self.q["pe"]))
            block.scalar(mk(self.q["act"]))
            block.vector(mk(self.q["dve"]))
            block.gpsimd(mk(self.q["pool"]))
            block.sync(mk(self.q["sp"]))


def build(S, dbg=False):
    NT = S // 128
    NBLK = S // 512
    SB = min(2048, S)
    NSB = S // SB
    nc = bass.Bass("TRN2", target_bir_lowering=False)

    def din(name, shape, dt=F32):
        return nc.dram_tensor(name, list(shape), dt, kind="ExternalInput").ap()

    def dscr(name, shape, dt):
        kind = "ExternalOutput" if dbg else "Internal"
        return nc.dram_tensor(name, list(shape), dt, kind=kind).ap()

    x = din("x", [S, D]); mem = din("mem", [256, D])
    g_mix = din("g_mix", [1, D]); w_in = din("w_in", [D, 5960]); b_gate = din("b_gate", [24, 128])
    g_qa = din("g_qa", [1, 64]); g_ka = din("g_ka", [1, 64]); g_idx_k = din("g_idx_k", [1, 64])
    conv_w = din("conv_w", [31, 512]); conv_b = din("conv_b", [4, 128])
    ln_g = din("ln_g", [4, 128]); ln_b = din("ln_b", [4, 128])
    g_mem = din("g_mem", [1, D]); w_mem_kv = din("w_mem_kv", [D, 1024])
    g_qm = din("g_qm", [1, 128]); g_km = din("g_km", [1, 128])
    w_br_a = din("w_br_a", [512, D]); w_br_b = din("w_br_b", [512, D]); w_br_m = din("w_br_m", [512, D])
    w_o = din("w_o", [D, D]); g_ffn = din("g_ffn", [1, D])
    w_rg = din("w_rg", [D, 4]); b_rg = din("b_rg", [1, 4]); w_re = din("w_re", [D, 32]); b_re = din("b_re", [1, 32])
    w_up = din("w_up", [32, D, 512]); w_down = din("w_down", [32, 256, D])
    c_identb = din("c_identb", [128, 128], BF16); c_identf = din("c_identf", [128, 128])
    c_i4 = din("c_i4", [128, 512], BF16); c_cb = din("c_cb", [128, 128])
    c_cos = din("c_cos", [128, NT, 32]); c_sin = din("c_sin", [128, NT, 32])
    c_pow2 = din("c_pow2", [128, KITER + 1])
    NTS_ = (2 * S + 32 * 511 + 511) // 512
    c_ltri = din("c_ltri", [128, 128], BF16); c_thr = din("c_thr", [128, 32, 2 * S // 512]); c_tm = din("c_tm", [128, 32, 32])
    c_kc = din("c_kc", [128, NTS_, 32]); c_io8 = din("c_io8", [128, 10])
    out = nc.dram_tensor("out", [S, D], F32, kind="ExternalOutput").ap()

    qT_s = dscr("qT_s", [NT, 64, 1024], BF16)
    qiT_s = dscr("qiT_s", [NT, 64, 1024], BF16)
    kT_s = dscr("kT_s", [2, 64, S], BF16)
    kiT_s = dscr("kiT_s", [64, S], BF16)
    v_s = dscr("v_s", [S, 130], BF16)
    wi_s = dscr("wi_s", [S, 8], F32)
    mg_s = dscr("mg_s", [NBLK, 128, 8, 2, 512], BF16)
    oaT_s = dscr("oaT_s", [128, 4, S], BF16)
    NTS = (2 * S + 32 * 511 + 511) // 512
    x1_s = dscr("x1_s", [S, D], F32)
    h2_s = dscr("h2_s", [S, D], BF16)
    Hs = dscr("Hs", [NTS * 512, D], BF16)
    Ysc = dscr("Ysc", [NTS * 512, D], F32)
    wub_s = dscr("wub_s", [32 * 128, 8 * 512], BF16)
    wdb_s = dscr("wdb_s", [32 * 128, 2 * D], BF16)

    st = ExitStack()
    with st:
        P = Prog(nc, st)

        ARENA_F32 = 50688
        arena = st.enter_context(nc.sbuf_tensor("arena", [128, ARENA_F32], F32))
        top = {"v": 0, "max": 0}

        class _Scope:
            def __init__(self, s):
                self.s = s
            def __enter__(self):
                self.s.__enter__()
                self.mark = top["v"]
                return self.s
            def __exit__(self, *a):
                top["v"] = self.mark
                return self.s.__exit__(*a)

        def scope():
            return _Scope(ExitStack())

        def sb(stack, name, shape, dt):
            esz = 4 if dt in (F32, mybir.dt.int32) else 2
            n = 1
            for d_ in shape[1:]:
                n *= d_
            nwords = (n * esz + 3) // 4
            nwords = (nwords + 7) // 8 * 8
            off = top["v"]
            top["v"] = off + nwords
            top["max"] = max(top["max"], top["v"])
            assert top["v"] <= ARENA_F32, ("SBUF arena overflow", name, top["v"])
            v = arena[0:shape[0], off:off + nwords]
            if dt != F32:
                v = v.bitcast(dt)
            v = v[:, 0:n]
            if len(shape) == 3:
                v = v.rearrange("p (a b) -> p a b", a=shape[1])
            elif len(shape) == 4:
                v = v.rearrange("p (a b c) -> p a b c", a=shape[1], b=shape[2])
            return v

        def pe(fn, r, w): return P.op("pe", fn, r, w)
        def act(fn, r, w): return P.op("act", fn, r, w)
        def dve(fn, r, w): return P.op("dve", fn, r, w)
        def pool(fn, r, w): return P.op("pool", fn, r, w)

        BK = [st.enter_context(nc.psum_tensor("bk%d" % i, [128, 512], F32)) for i in range(8)]
        rr = {"i": 0}

        def nbank(n=6):
            i = rr["i"] % n
            rr["i"] += 1
            return BK[i], "bk%d" % i

        identb = sb(st, "identb", [128, 128], BF16)
        identf = sb(st, "identf", [128, 128], F32)
        onesf = sb(st, "onesf", [128, 128], F32)
        onesb = sb(st, "onesb", [128, 128], BF16)
        epsb = sb(st, "epsb", [128, 1], F32)
        P.dma("sp", identb[:], c_identb, writes=["identb"])
        P.dma("sp", identf[:], c_identf, writes=["identf"])
        pool(lambda e: e.memset(onesf[:], 1.0), [], ["onesf"])
        pool(lambda e: e.memset(onesb[:], 1.0), [], ["onesb"])
        pool(lambda e: e.memset(epsb[:], EPS), [], ["epsb"])

        joinsc = sb(st, "joinsc", [128, 1], F32)

        def bcast_load(stack, name, ap_row, n):
            t = sb(stack, name, [128, n], F32)
            P.dma("sp", t[:], ap_row.to_broadcast([128, n]), writes=[name])
            return t

        def load_T(stack, name, src, R, C):
            t = sb(stack, name, [128, C, R], F32)
            with scope() as tmp:
                raw = sb(tmp, name + "_raw", [R, C * 128], F32)
                P.dma("sp", raw[:], src, writes=[name + "_raw"])
                for c in range(C):
                    bk, bkk = nbank()
                    pe(lambda e, bk=bk, c=c: e.transpose(bk[:, 0:R], raw[:, c * 128:(c + 1) * 128], identf[0:R, 0:R]),
                       [name + "_raw", "identf"], [bkk])
                    dve(lambda e, bk=bk, c=c: e.tensor_copy(out=t[:, c, :], in_=bk[:, 0:R]), [bkk], [name])
                P.barrier()
            return t

        def rstd_from_ss(ss, rs, n, scale, keys_r, key_w):
            act(lambda e: e.activation(out=rs, in_=ss, func=AF.Sqrt, bias=epsb[:], scale=scale), keys_r + ["epsb"], [key_w])
            dve(lambda e: e.reciprocal(out=rs, in_=rs), [key_w], [key_w])

        def wload_bf16(dst3, src2, K, N, key, n0=0):
            subs = []
            c = 0
            while c < N:
                w = min(2048, N - c)
                for k in range(K):
                    sk = "%s#%d_%d" % (key, c, k)
                    subs.append(sk)
                    P.dma("pool", dst3[:, k, c:c + w], src2[k * 128:(k + 1) * 128, n0 + c:n0 + c + w], writes=[sk])
                c += w
            P.op("pool", lambda e: e.memset(joinsc[:], 0.0), subs, [key, "joinsc"])

        with scope() as p1:
            w_tm = sb(p1, "w_tm", [128, 8, N_TM], BF16)
            w_fm = sb(p1, "w_fm", [128, 8, N_FM], BF16)
            wbb = sb(p1, "wbb", [128, 4, D], BF16)
            wbm = sb(p1, "wbm", [128, 4, D], BF16)
            kmT = sb(p1, "kmT", [128, 4, 256], BF16)
            vm = sb(p1, "vm", [128, 2, 512], BF16)
            wload_bf16(w_tm, w_in, 8, N_TM, "w_tm", 0)
            wload_bf16(w_fm, w_in, 8, N_FM, "w_fm", N_TM)
            wload_bf16(wbb, w_br_b, 4, D, "wbb")
            wload_bf16(wbm, w_br_m, 4, D, "wbm")
            gmix_bc = bcast_load(p1, "gmix_bc", g_mix, D)
            gqa_bc = bcast_load(p1, "gqa_bc", g_qa, 64)
            gka_bc = bcast_load(p1, "gka_bc", g_ka, 64)
            gki_bc = bcast_load(p1, "gki_bc", g_idx_k, 64)
            bgT = load_T(p1, "bgT", b_gate, 24, 1)
            cbT = load_T(p1, "cbT", conv_b, 4, 1)
            lgT = load_T(p1, "lgT", ln_g, 4, 1)
            lbT = load_T(p1, "lbT", ln_b, 4, 1)
            cwT = load_T(p1, "cwT", conv_w, 31, 4)
            uu = sb(p1, "uu", [128, 4, 30 + 512], BF16)
            pool(lambda e: e.memset(uu[:], 0.0), [], ["uu"])

            with scope() as p0:
                wkv = sb(p0, "wkv", [128, 8, 1024], BF16)
                wload_bf16(wkv, w_mem_kv, 8, 1024, "wkv")
                gmem_bc = bcast_load(p0, "gmem_bc", g_mem, D)
                gkm_bc = bcast_load(p0, "gkm_bc", g_km, 128)
                gqm_bc = bcast_load(p0, "gqm_bc", g_qm, 128)
                dve(lambda e: e.scalar_tensor_tensor(out=gkm_bc[:], in0=gkm_bc[:], scalar=128.0 ** -0.5, in1=gqm_bc[:],
                                                     op0=ALU.mult, op1=ALU.mult), ["gkm_bc", "gqm_bc"], ["gkm_bc"])
                mt_ = sb(p0, "mt_", [128, D], F32)
                junk = sb(p0, "junk0", [128, D], BF16)
                hm = sb(p0, "hm", [128, D], BF16)
                memT = sb(p0, "memT", [128, 8, 256], BF16)
                ss = sb(p0, "ss0", [128, 4], F32)
                rs = sb(p0, "rs0", [128, 4], F32)
                kx = sb(p0, "kx", [128, 512], F32)
                kn = sb(p0, "kn", [128, 512], BF16)
                for m in range(2):
                    P.dma("sp", mt_[:], mem[m * 128:(m + 1) * 128, :], writes=["mt_"])
                    act(lambda e: e.activation(out=junk[:], in_=mt_[:], func=AF.Square, accum_out=ss[:, 0:1]), ["mt_"], ["junk0", "ss0"])
                    rstd_from_ss(ss[:, 0:1], rs[:, 0:1], 1, 1.0 / D, ["ss0"], "rs0")
                    dve(lambda e: e.scalar_tensor_tensor(out=hm[:], in0=mt_[:], scalar=rs[:, 0:1], in1=gmem_bc[:],
                                                         op0=ALU.mult, op1=ALU.mult), ["mt_", "rs0", "gmem_bc"], ["hm"])
                    tb = BK[6][:].bitcast(BF16)
                    pe(lambda e: [e.transpose(tb[:, k * 128:(k + 1) * 128], hm[:, k * 128:(k + 1) * 128], identb[:]) for k in range(8)][-1],
                       ["hm", "identb"], ["bk6"])
                    dve(lambda e, m=m: e.tensor_copy(out=memT[:, :, m * 128:(m + 1) * 128],
                                                     in_=tb.rearrange("p (k t) -> p k t", k=8)), ["bk6"], ["memT"])
                for m in range(2):
                    for half in range(2):
                        bk, bkk = nbank()
                        pe(lambda e, bk=bk, m=m, half=half: [e.matmul(bk[:], lhsT=memT[:, k, m * 128:(m + 1) * 128],
                                                                       rhs=wkv[:, k, half * 512:(half + 1) * 512],
                                                                       start=(k == 0), stop=(k == 7)) for k in range(8)][-1],
                           ["memT", "wkv"], [bkk])
                        if half == 1:
                            act(lambda e, bk=bk, m=m: e.copy(out=vm[:, m, :], in_=bk[:]), [bkk], ["vm"])
                        else:
                            act(lambda e, bk=bk: e.copy(out=kx[:], in_=bk[:]), [bkk], ["kx"])
                            dve(lambda e: e.tensor_tensor(out=junk[:, 0:512], in0=kx[:], in1=kx[:], op=ALU.mult), ["kx"], ["junk0"])
                            dve(lambda e: e.tensor_reduce(out=ss[:], in_=junk[:, 0:512].rearrange("p (h d) -> p h d", h=4),
                                                          axis=AX.X, op=ALU.add), ["junk0"], ["ss0"])
                            rstd_from_ss(ss[:], rs[:], 4, 1.0 / 128, ["ss0"], "rs0")
                            for h in range(4):
                                dve(lambda e, h=h: e.scalar_tensor_tensor(out=kn[:, h * 128:(h + 1) * 128], in0=kx[:, h * 128:(h + 1) * 128],
                                                                          scalar=rs[:, h:h + 1], in1=gkm_bc[:], op0=ALU.mult, op1=ALU.mult),
                                    ["kx", "rs0", "gkm_bc"], ["kn"])
                            tb = BK[6][:].bitcast(BF16)
                            pe(lambda e: [e.transpose(tb[:, h * 128:(h + 1) * 128], kn[:, h * 128:(h + 1) * 128], identb[:]) for h in range(4)][-1],
                               ["kn", "identb"], ["bk6"])
                            dve(lambda e, m=m: e.tensor_copy(out=kmT[:, :, m * 128:(m + 1) * 128],
                                                             in_=tb[:, 0:512].rearrange("p (h t) -> p h t", h=4)), ["bk6"], ["kmT"])
                P.barrier()

            hT = sb(p1, "hT", [128, 8, 512], BF16)
            obT = sb(p1, "obT", [128, 4, 512], BF16)
            omT = sb(p1, "omT", [128, 4, 512], BF16)
            xt = [sb(p1, "xt%d" % i, [128, D], F32) for i in range(2)]
            junk = sb(p1, "junkA", [128, 8], BF16)
            hh2 = [sb(p1, "hh%d" % i, [128, D], BF16) for i in range(2)]
            ss = sb(p1, "ssA", [128, 16], F32)
            rs = sb(p1, "rsA", [128, 16], F32)
            cs = [sb(p1, "cs%d" % i, [128, 2, 32], F32) for i in range(4)]
            qx = sb(p1, "qx", [128, 512], F32)
            sq = sb(p1, "sqA", [128, 512], F32)
            qn = sb(p1, "qn", [128, 512], F32)
            t1 = sb(p1, "t1", [128, 256], F32); t2 = sb(p1, "t2", [128, 256], F32)
            t3 = sb(p1, "t3", [128, 256], F32); t4 = sb(p1, "t4", [128, 256], F32)
            qr2 = [sb(p1, "qr%d" % i, [128, 512], BF16) for i in range(2)]
            stg = [sb(p1, "stg%d" % i, [64, 1024], BF16) for i in range(2)]
            vst = sb(p1, "vst", [128, 2, 65], BF16)
            wst = sb(p1, "wst", [128, 8], F32)
            pool(lambda e: e.memset(vst[:], 1.0), [], ["vst"])
            sgB = sb(p1, "sgB", [128, 512], F32)
            yy = sb(p1, "yy", [128, 4, 512], F32)
            sq2 = [sb(p1, "sq2_%d" % i, [128, 512], F32) for i in range(2)]
            rb = sb(p1, "rbB", [128, 512], F32)
            qmn = sb(p1, "qmn", [128, 512], BF16)
            pT = [sb(p1, "pTm%d" % i, [128, 512], BF16) for i in range(2)]
            rden = sb(p1, "rden", [128, 512], F32)
            mgst = [sb(p1, "mgst%d" % i, [128, 2, 512], BF16) for i in range(2)]
            print("phase1 arena words", top["v"], "of", ARENA_F32)
            sgi = {"i": 0}
            ab = {"i": 0}
            dgi = {"i": 0}
            dg = [sb(p1, "dg%d" % i, [128, 128], BF16) for i in range(8)]

            def norm_rope(src_ap, src_keys, nh, gain_bc, gkey, ck, csk, out_dram_fn):
                n = nh * 64
                qr = qr2[sgi["i"] % 2]; qrk = "qr%d" % (sgi["i"] % 2)
                if gain_bc is not None:
                    act(lambda e: e.copy(out=qx[:, 0:n], in_=src_ap), src_keys, ["qx"])
                    dve(lambda e: e.tensor_tensor(out=sq[:, 0:n], in0=qx[:, 0:n], in1=qx[:, 0:n], op=ALU.mult), ["qx"], ["sqA"])
                    dve(lambda e: e.tensor_reduce(out=ss[:, 0:nh], in_=sq[:, 0:n].rearrange("p (h d) -> p h d", h=nh),
                                                  axis=AX.X, op=ALU.add), ["sqA"], ["ssA"])
                    rstd_from_ss(ss[:, 0:nh], rs[:, 0:nh], nh, 1.0 / 64, ["ssA"], "rsA")
                    dve(lambda e: e.tensor_tensor(out=qn[:, 0:n].rearrange("p (h d) -> p h d", h=nh),
                                                  in0=qx[:, 0:n].rearrange("p (h d) -> p h d", h=nh),
                                                  in1=rs[:, 0:nh].unsqueeze(2).to_broadcast([128, nh, 64]), op=ALU.mult),
                        ["qx", "rsA"], ["qn"])
                    dve(lambda e: e.tensor_tensor(out=qn[:, 0:n].rearrange("p (h d) -> p h d", h=nh),
                                                  in0=qn[:, 0:n].rearrange("p (h d) -> p h d", h=nh),
                                                  in1=gain_bc[:].unsqueeze(1).to_broadcast([128, nh, 64]), op=ALU.mult),
                        ["qn", gkey], ["qn"])
                else:
                    act(lambda e: e.copy(out=qn[:, 0:n], in_=src_ap), src_keys, ["qn"])
                q3 = qn[:, 0:n].rearrange("p (h d) -> p h d", h=nh)
                o3 = qr[:, 0:n].rearrange("p (h d) -> p h d", h=nh)
                cosb = ck[:, 0:1, :].to_broadcast([128, nh, 32])
                sinb = ck[:, 1:2, :].to_broadcast([128, nh, 32])
                m = nh * 32
                v1 = t1[:, 0:m].rearrange("p (h d) -> p h d", h=nh); v2 = t2[:, 0:m].rearrange("p (h d) -> p h d", h=nh)
                v3 = t3[:, 0:m].rearrange("p (h d) -> p h d", h=nh); v4 = t4[:, 0:m].rearrange("p (h d) -> p h d", h=nh)
                dve(lambda e: e.tensor_tensor(out=v1, in0=q3[:, :, 0:32], in1=cosb, op=ALU.mult), ["qn", csk], ["t1"])
                dve(lambda e: e.tensor_tensor(out=v2, in0=q3[:, :, 32:64], in1=sinb, op=ALU.mult), ["qn", csk], ["t2"])
                dve(lambda e: e.tensor_tensor(out=o3[:, :, 0:32], in0=v1, in1=v2, op=ALU.subtract), ["t1", "t2"], [qrk])
                pool(lambda e: e.tensor_tensor(out=v3, in0=q3[:, :, 32:64], in1=cosb, op=ALU.mult), ["qn", csk], ["t3"])
                pool(lambda e: e.tensor_tensor(out=v4, in0=q3[:, :, 0:32], in1=sinb, op=ALU.mult), ["qn", csk], ["t4"])
                pool(lambda e: e.tensor_tensor(out=o3[:, :, 32:64], in0=v3, in1=v4, op=ALU.add), ["t3", "t4"], [qrk])
                sg = stg[sgi["i"] % 2]; sgk = "stg%d" % (sgi["i"] % 2); sgi["i"] += 1

                def part2():
                    tb = BK[7][:].bitcast(BF16)
                    pe(lambda e: [e.transpose(tb[0:64, h * 128:(h + 1) * 128], qr[:, h * 64:(h + 1) * 64], identb[:]) for h in range(nh)][-1],
                       [qrk, "identb"], ["bk7"])
                    act(lambda e: e.copy(out=sg[:, 0:nh * 128], in_=tb[0:64, 0:nh * 128]), ["bk7"], [sgk])
                    out_dram_fn(sg, sgk)
                return part2

            for b in range(NBLK):
                for tl in range(4):
                    ti = b * 4 + tl
                    r0 = ti * 128
                    xb = xt[tl % 2]; xk = "xt%d" % (tl % 2)
                    ck = cs[tl]; csk = "cs%d" % tl
                    P.dma("sp", xb[:], x[r0:r0 + 128, :], writes=[xk])
                    P.dma("sp", ck[:, 0, :], c_cos[:, ti, :], writes=[csk])
                    P.dma("sp", ck[:, 1, :], c_sin[:, ti, :], writes=[csk])
                    hh = hh2[tl % 2]; hhk = "hh%d" % (tl % 2)
                    sc_ = 12 + tl; sk_ = "ssh%d" % tl; rk_ = "rsh%d" % tl
                    act(lambda e: e.activation(out=junk[:, 0:1].to_broadcast([128, D]), in_=xb[:], func=AF.Square, accum_out=ss[:, sc_:sc_ + 1]),
                        [xk], ["junkA", sk_])
                    act(lambda e: e.activation(out=rs[:, sc_:sc_ + 1], in_=ss[:, sc_:sc_ + 1], func=AF.Sqrt, bias=epsb[:], scale=1.0 / D), [sk_, "epsb"], [rk_])
                    dve(lambda e: e.reciprocal(out=rs[:, sc_:sc_ + 1], in_=rs[:, sc_:sc_ + 1]), [rk_], [rk_])
                    dve(lambda e: e.scalar_tensor_tensor(out=hh[:], in0=xb[:], scalar=rs[:, sc_:sc_ + 1], in1=gmix_bc[:],
                                                         op0=ALU.mult, op1=ALU.mult), [xk, rk_, "gmix_bc"], [hhk])
                    tb6 = BK[6][:].bitcast(BF16)
                    pe(lambda e: [e.transpose(tb6[:, k * 128:(k + 1) * 128], hh[:, k * 128:(k + 1) * 128], identb[:]) for k in range(8)][-1],
                       [hhk, "identb"], ["bk6"])
                    act(lambda e: e.copy(out=hT[:, :, tl * 128:(tl + 1) * 128], in_=tb6.rearrange("p (k t) -> p k t", k=8)), ["bk6"], ["hT"])

                def tm_proj(tl, c0, n):
                    ab["i"] += 1
                    bk, bkk = BK[4 + ab["i"] % 2], "bk%d" % (4 + ab["i"] % 2)
                    pe(lambda e: [e.matmul(bk[:, 0:n], lhsT=hT[:, k, tl * 128:(tl + 1) * 128], rhs=w_tm[:, k, c0:c0 + n],
                                           start=(k == 0), stop=(k == 7)) for k in range(8)][-1], ["hT", "w_tm"], [bkk])
                    return bk, bkk

                def fm_proj(cc):
                    bk, bkk = nbank(4)
                    pe(lambda e: [e.matmul(bk[:], lhsT=w_fm[:, k, cc * 128:(cc + 1) * 128], rhs=hT[:, k, :],
                                           start=(k == 0), stop=(k == 7)) for k in range(8)][-1], ["hT", "w_fm"], [bkk])
                    return bk, bkk

                A_items = []
                X_items = []

                def mk_A(tl):
                    ti = b * 4 + tl
                    r0 = ti * 128
                    ck = cs[tl]; csk = "cs%d" % tl

                    def it_q():
                        bk, bkk = tm_proj(tl, C_QA, 512)
                        return norm_rope(bk[:, 0:512], [bkk], 8, gqa_bc, "gqa_bc", ck, csk,
                                         lambda sg, sgk: P.dma("sp", qT_s[ti], sg[:], reads=[sgk], writes=["qT_s%d" % ti]))

                    def it_kv():
                        bk, bkk = tm_proj(tl, C_KA, 256)
                        act(lambda e: e.copy(out=vst[:, :, 0:64], in_=bk[:, 128:256].rearrange("p (g d) -> p g d", g=2)), [bkk], ["vst"])
                        P.dma("sp", v_s[r0:r0 + 128, :], vst[:].rearrange("p g d -> p (g d)"), reads=["vst"], writes=["v_s%d" % ti])

                        def kout(sg, sgk):
                            for g in range(2):
                                P.dma("sp", kT_s[g, :, r0:r0 + 128], sg[:, g * 128:(g + 1) * 128], reads=[sgk], writes=["kT_s%d" % ti])
                        return norm_rope(bk[:, 0:128], [bkk], 2, gka_bc, "gka_bc", ck, csk, kout)

                    def it_qi():
                        bk, bkk = tm_proj(tl, C_QI, 512)
                        return norm_rope(bk[:, 0:512], [bkk], 8, None, None, ck, csk,
                                         lambda sg, sgk: P.dma("sp", qiT_s[ti], sg[:], reads=[sgk], writes=["qiT_s%d" % ti]))

                    def it_ki():
                        bk, bkk = tm_proj(tl, C_KI, 72)
                        act(lambda e: e.activation(out=wst[:], in_=bk[:, 64:72], func=AF.Copy, scale=float(8 ** -0.5 * 64 ** -0.5)), [bkk], ["wst"])
                        P.dma("sp", wi_s[r0:r0 + 128, :], wst[:], reads=["wst"], writes=["wi_s%d" % ti])
                        return norm_rope(bk[:, 0:64], [bkk], 1, gki_bc, "gki_bc", ck, csk,
                                         lambda sg, sgk: P.dma("sp", kiT_s[:, r0:r0 + 128], sg[:, 0:128], reads=[sgk], writes=["kiT_s%d" % ti]))
                    return [it_q, it_kv, it_qi, it_ki]

                for tl in range(4):
                    A_items.extend(mk_A(tl))

                def mk_conv(cp):
                    def it():
                        for c in cp:
                            bka, bkak = fm_proj(c)
                            bkg, bkgk = fm_proj(4 + c)
                            act(lambda e: e.activation(out=sgB[:], in_=bkg[:], func=AF.Sigmoid), [bkgk], ["sgB"])
                            dve(lambda e: e.tensor_tensor(out=uu[:, c, 30:542], in0=bka[:], in1=sgB[:], op=ALU.mult), [bkak, "sgB"], ["uu%d" % c])
                            cbk, cbkk = nbank(4)
                            for j in range(31):
                                d_ = dg[dgi["i"] % 8]; dk = "dg%d" % (dgi["i"] % 8); dgi["i"] += 1
                                pool(lambda e: e.tensor_scalar(out=d_[:], in0=identb[:], scalar1=cwT[:, c, j:j + 1], scalar2=0.0, op0=ALU.mult, op1=ALU.add),
                                     ["identb", "cwT"], [dk])
                                pe(lambda e: e.matmul(cbk[:], lhsT=d_[:], rhs=uu[:, c, j:j + 512], start=(j == 0), stop=(j == 30)),
                                   [dk, "uu%d" % c], [cbkk])
                            act(lambda e: e.activation(out=yy[:, c, :], in_=cbk[:], func=AF.Identity, bias=cbT[:, 0, c:c + 1]), [cbkk, "cbT"], ["yy%d" % c])
                            pool(lambda e: e.tensor_copy(out=uu[:, c, 0:30], in_=uu[:, c, 512:542]), ["uu%d" % c], ["uu%d" % c])
                    return it

                def it_ln():
                    yk = ["yy%d" % c for c in range(4)]
                    mbk, mbkk = nbank()
                    pe(lambda e: [e.matmul(mbk[:], lhsT=onesf[:], rhs=yy[:, c, :], start=(c == 0), stop=(c == 3)) for c in range(4)][-1],
                       ["onesf"] + yk, [mbkk])
                    for c in range(4):
                        dve(lambda e: e.scalar_tensor_tensor(out=yy[:, c, :], in0=mbk[:], scalar=-1.0 / 512, in1=yy[:, c, :],
                                                             op0=ALU.mult, op1=ALU.add), [mbkk, "yy%d" % c], ["yy%d" % c])
                    vbk, vbkk = nbank()
                    for c in range(4):
                        s_ = sq2[c % 2]; sk = "sq2_%d" % (c % 2)
                        act(lambda e: e.activation(out=s_[:], in_=yy[:, c, :], func=AF.Square), ["yy%d" % c], [sk])
                        pe(lambda e: e.matmul(vbk[:], lhsT=onesf[:], rhs=s_[:], start=(c == 0), stop=(c == 3)), ["onesf", sk], [vbkk])
                    act(lambda e: e.activation(out=rb[:], in_=vbk[:], func=AF.Sqrt, bias=epsb[:], scale=1.0 / 512), [vbkk, "epsb"], ["rbB"])
                    dve(lambda e: e.reciprocal(out=rb[:], in_=rb[:]), ["rbB"], ["rbB"])
                    for c in range(4):
                        dve(lambda e: e.tensor_tensor(out=yy[:, c, :], in0=yy[:, c, :], in1=rb[:], op=ALU.mult), ["yy%d" % c, "rbB"], ["yy%d" % c])
                        act(lambda e: e.activation(out=obT[:, c, :], in_=yy[:, c, :], func=AF.Silu, bias=lbT[:, 0, c:c + 1],
                                                   scale=lgT[:, 0, c:c + 1]), ["yy%d" % c, "lbT", "lgT"], ["obT"])

                def mk_mem(h):
                    def it():
                        bq, bqk = fm_proj(8 + h)
                        act(lambda e: e.activation(out=sgB[:], in_=bq[:], func=AF.Square), [bqk], ["sgB"])
                        b2, b2k = nbank()
                        pe(lambda e: e.matmul(b2[:], lhsT=onesf[:], rhs=sgB[:], start=True, stop=True), ["onesf", "sgB"], [b2k])
                        act(lambda e: e.activation(out=rb[:], in_=b2[:], func=AF.Sqrt, bias=epsb[:], scale=1.0 / 128), [b2k, "epsb"], ["rbB"])
                        dve(lambda e: e.reciprocal(out=rb[:], in_=rb[:]), ["rbB"], ["rbB"])
                        dve(lambda e: e.tensor_tensor(out=qmn[:], in0=bq[:], in1=rb[:], op=ALU.mult), [bqk, "rbB"], ["qmn"])
                        for mc in range(2):
                            b3, b3k = nbank()
                            pe(lambda e: e.matmul(b3[:], lhsT=kmT[:, h, mc * 128:(mc + 1) * 128], rhs=qmn[:], start=True, stop=True),
                               ["kmT", "qmn"], [b3k])
                            act(lambda e: e.activation(out=pT[mc][:], in_=b3[:], func=AF.Exp), [b3k], ["pTm%d" % mc])
                        bo, bok = nbank()
                        pe(lambda e: [e.matmul(bo[:], lhsT=vm[:, mc, h * 128:(h + 1) * 128], rhs=pT[mc][:], start=(mc == 0), stop=(mc == 1))
                                      for mc in range(2)][-1], ["vm", "pTm0", "pTm1"], [bok])
                        bd, bdk = nbank()
                        pe(lambda e: [e.matmul(bd[:], lhsT=onesb[:], rhs=pT[mc][:], start=(mc == 0), stop=(mc == 1)) for mc in range(2)][-1],
                           ["onesb", "pTm0", "pTm1"], [bdk])
                        dve(lambda e: e.reciprocal(out=rden[:], in_=bd[:]), [bdk], ["rden"])
                        dve(lambda e: e.tensor_tensor(out=omT[:, h, :], in0=bo[:], in1=rden[:], op=ALU.mult), [bok, "rden"], ["omT"])
                    return it

                def mk_gate(oc):
                    def it():
                        g1 = sq2[0]; g2 = sq2[1]; t2_ = rden
                        ms = mgst[oc % 2]; msk = "mgst%d" % (oc % 2)
                        bg0, bg0k = fm_proj(12 + oc)
                        act(lambda e: e.activation(out=ms[:, 0, :], in_=bg0[:], func=AF.Sigmoid, bias=bgT[:, 0, oc:oc + 1]), [bg0k, "bgT"], [msk])
                        bg1, bg1k = fm_proj(20 + oc)
                        act(lambda e: e.activation(out=g1[:], in_=bg1[:], func=AF.Sigmoid, bias=bgT[:, 0, 8 + oc:9 + oc]), [bg1k, "bgT"], ["sq2_0"])
                        bg2, bg2k = fm_proj(28 + oc)
                        act(lambda e: e.activation(out=g2[:], in_=bg2[:], func=AF.Sigmoid, bias=bgT[:, 0, 16 + oc:17 + oc]), [bg2k, "bgT"], ["sq2_1"])
                        pb, pbk = nbank()
                        pe(lambda e: [e.matmul(pb[:], lhsT=wbb[:, k, oc * 128:(oc + 1) * 128], rhs=obT[:, k, :], start=(k == 0), stop=(k == 3))
                                      for k in range(4)][-1], ["wbb", "obT"], [pbk])
                        dve(lambda e: e.tensor_tensor(out=g1[:], in0=pb[:], in1=g1[:], op=ALU.mult), [pbk, "sq2_0"], ["sq2_0"])
                        pm, pmk = nbank()
                        pe(lambda e: [e.matmul(pm[:], lhsT=wbm[:, k, oc * 128:(oc + 1) * 128], rhs=omT[:, k, :], start=(k == 0), stop=(k == 3))
                                      for k in range(4)][-1], ["wbm", "omT"], [pmk])
                        dve(lambda e: e.tensor_tensor(out=t2_[:], in0=pm[:], in1=g2[:], op=ALU.mult), [pmk, "sq2_1"], ["rden"])
                        pool(lambda e: e.tensor_tensor(out=ms[:, 1, :], in0=g1[:], in1=t2_[:], op=ALU.add), ["sq2_0", "rden"], [msk])
                        P.dma("sp", mg_s[b, :, oc], ms[:], reads=[msk], writes=["mg_s%d" % b])
                    return it

                X_items = [mk_conv((0, 1)), mk_conv((2, 3)), it_ln] + [mk_mem(h) for h in range(4)] + [mk_gate(oc) for oc in range(8)]
                na, nx = len(A_items), len(X_items)
                prev2 = None
                for n_ in range(na):
                    if n_ < nx:
                        X_items[n_]()
                    p2 = A_items[n_]()
                    if prev2 is not None:
                        prev2()
                    prev2 = p2
                for n_ in range(na, nx):
                    X_items[n_]()
                prev2()
            P.barrier()

        with scope() as p2:
            KT = sb(p2, "KT", [128, S], BF16)
            kiT = sb(p2, "kiT", [128, S], BF16)
            V = sb(p2, "V", [128, NT, 130], BF16)
            widx = sb(p2, "widx", [128, NT, 8], F32)
            score = sb(p2, "score", [128, S], F32)
            MB = sb(p2, "MB", [128, S], BF16)
            i4 = sb(p2, "i4", [128, 512], BF16)
            cb = sb(p2, "cb", [128, 128], F32)
            pow2 = sb(p2, "pow2", [128, KITER + 1], F32)
            allk = ["kT_s%d" % t for t in range(NT)]
            for g in range(2):
                P.dma("sp", KT[g * 64:(g + 1) * 64, :], kT_s[g], reads=allk, writes=["KT"])
            pool(lambda e: e.memset(kiT[64:128, :], 0.0), [], ["kiT"])
            P.dma("sp", kiT[0:64, :], kiT_s, reads=["kiT_s%d" % t for t in range(NT)], writes=["kiT"])
            for t0 in range(0, NT, 16):
                t1_ = min(NT, t0 + 16)
                P.dma("sp", V[:, t0:t1_, :], v_s[t0 * 128:t1_ * 128, :].rearrange("(n p) c -> p n c", p=128),
                      reads=["v_s%d" % t for t in range(t0, t1_)], writes=["V"])
            for t0 in range(0, NT, 8):
                t1_ = min(NT, t0 + 8)
                P.dma("sp", widx[:, t0:t1_, :], wi_s[t0 * 128:t1_ * 128, :].rearrange("(n p) c -> p n c", p=128),
                      reads=["wi_s%d" % t for t in range(t0, t1_)], writes=["widx"])
            P.dma("sp", i4[:], c_i4, writes=["i4"])
            conv_jobs = []
            for ex_ in range(32):
                for k8 in range(8):
                    conv_jobs.append((wub_s[ex_ * 128:(ex_ + 1) * 128, k8 * 512:(k8 + 1) * 512], w_up[ex_][k8 * 128:(k8 + 1) * 128, :], "wub_s"))
                for a_ in range(2):
                    conv_jobs.append((wdb_s[ex_ * 128:(ex_ + 1) * 128, a_ * D:(a_ + 1) * D], w_down[ex_][a_ * 128:(a_ + 1) * 128, :], "wdb_s"))

            def issue_conv(n_):
                for _ in range(n_):
                    if conv_jobs:
                        o_, i_, k_ = conv_jobs.pop(0)
                        P.dma("pool", o_, i_, writes=[])

            P.dma("sp", cb[:], c_cb, writes=["cb"])
            P.dma("sp", pow2[:], c_pow2, writes=["pow2"])
            qT = [sb(p2, "qT%d" % i, [128, 1024], BF16) for i in range(2)]
            qiT = [sb(p2, "qiT%d" % i, [128, 1024], BF16) for i in range(2)]
            for i_ in range(2):
                pool(lambda e: e.memset(qT[i_][:], 0.0), [], ["qT%d" % i_])
                pool(lambda e: e.memset(qiT[i_][:], 0.0), [], ["qiT%d" % i_])
            MB2 = [MB, sb(p2, "MBb", [128, S], BF16)]
            SC2 = [score, sb(p2, "scoreb", [128, S], F32)]
            diagw = sb(p2, "diagw", [128, 8, 128], BF16)
            RR = [sb(p2, "RR%d" % i, [128, 512], BF16) for i in range(4)]
            PT = [sb(p2, "PT%d" % i, [128, 512], BF16) for i in range(4)]
            sm2 = [sb(p2, "sm%d" % i, [128, 8], F32) for i in range(2)]
            wk2 = [sb(p2, "wk%d" % i, [128, KITER + 1], F32) for i in range(2)]
            jk = sb(p2, "jk", [128, 8], BF16)
            rdn = sb(p2, "rdn", [128, 8], F32)
            oa = sb(p2, "oa", [128, 512], BF16)
            oaT = [sb(p2, "oaT%d" % i, [128, 4, 128], BF16) for i in range(2)]
            OB = [BK[4], BK[5]]
            SC = BK[3]
            print("phase2 arena words", top["v"], "of", ARENA_F32)

            def stage_A(i):
                n = (i + 1) * 128
                qib = qiT[i % 2]; qik = "qiT%d" % (i % 2)
                MBi = MB2[i % 2]; MBk = "MB%d" % (i % 2)
                score = SC2[i % 2]; sck = "score%d" % (i % 2)
                sm = sm2[i % 2]; smk = "sm%d" % (i % 2)
                wk = wk2[i % 2]; wkk = "wk%d" % (i % 2)
                P.dma("sp", qib[0:64, :], qiT_s[i], reads=["qiT_s%d" % i], writes=[qik])
                for h in range(8):
                    pool(lambda e, h=h: e.tensor_scalar(out=diagw[:, h, :], in0=identb[:], scalar1=widx[:, i, h:h + 1], scalar2=None, op0=ALU.mult),
                         ["identb", "widx"], ["diagw"])
                nch = (n + 511) // 512
                units = [(c, h) for c in range(nch) for h in range(8)]
                Lb = {}

                def emit_L(u):
                    c, h = units[u]
                    k0 = c * 512
                    ncol = min(512, n - k0)
                    bk, bkk = nbank(3)
                    pe(lambda e: e.matmul(bk[:, 0:ncol], lhsT=qib[:, h * 128:(h + 1) * 128], rhs=kiT[:, k0:k0 + ncol], start=True, stop=True),
                       [qik, "kiT"], [bkk])
                    Lb[u] = (bk, bkk)

                for u in range(min(2, len(units))):
                    emit_L(u)
                for u in range(len(units)):
                    c, h = units[u]
                    k0 = c * 512
                    ncol = min(512, n - k0)
                    bk, bkk = Lb.pop(u)
                    R = RR[u % 4]; Rk = "RR%d" % (u % 4)
                    act(lambda e: e.activation(out=R[:, 0:ncol], in_=bk[:, 0:ncol], func=AF.Relu), [bkk], [Rk])
                    if u + 2 < len(units):
                        emit_L(u + 2)
                    scb = (BK[3], "bk3") if c % 2 == 0 else (BK[7], "bk7")
                    pe(lambda e: e.matmul(scb[0][:, 0:ncol], lhsT=diagw[:, h, :], rhs=R[:, 0:ncol], start=(h == 0), stop=(h == 7)),
                       ["diagw", Rk], [scb[1]])
                    if h == 7:
                        act(lambda e: e.copy(out=score[:, k0:k0 + ncol], in_=scb[0][:, 0:ncol]), [scb[1]], [sck])
                dve(lambda e: e.tensor_tensor(out=score[:, i * 128:(i + 1) * 128], in0=score[:, i * 128:(i + 1) * 128], in1=cb[:], op=ALU.add),
                    [sck, "cb"], [sck])
                if i >= 2:
                    dve(lambda e: e.tensor_reduce(out=sm[:, 0:1], in_=score[:, 0:n], axis=AX.X, op=ALU.max), [sck], [smk])
                    dve(lambda e: e.tensor_reduce(out=sm[:, 1:2], in_=score[:, 0:256], axis=AX.X, op=ALU.min), [sck], [smk])
                    dve(lambda e: e.tensor_tensor(out=sm[:, 2:3], in0=sm[:, 0:1], in1=sm[:, 1:2], op=ALU.subtract), [smk], [smk])
                    dve(lambda e: e.tensor_scalar(out=wk[:], in0=pow2[:], scalar1=sm[:, 2:3], scalar2=None, op0=ALU.mult), [smk, "pow2"], [wkk])
                    dve(lambda e: e.tensor_tensor(out=sm[:, 3:4], in0=sm[:, 1:2], in1=wk[:, 0:1], op=ALU.add), [smk, wkk], [smk])
                    for k in range(KITER):
                        dve(lambda e: e.tensor_scalar(out=jk[:, 0:1].to_broadcast([128, n]), in0=score[:, 0:n], scalar1=sm[:, 3:4], scalar2=None,
                                                      op0=ALU.is_ge, op1=ALU.add, accum_out=sm[:, 4:5]), [sck, smk], ["jk", smk])
                        dve(lambda e: e.tensor_scalar(out=sm[:, 5:6], in0=sm[:, 4:5], scalar1=TOPK - 0.5, scalar2=0.5, op0=ALU.is_ge, op1=ALU.subtract),
                            [smk], [smk])
                        dve(lambda e: e.scalar_tensor_tensor(out=sm[:, 3:4], in0=sm[:, 5:6], scalar=wk[:, k:k + 1], in1=sm[:, 3:4],
                                                             op0=ALU.mult, op1=ALU.add), [smk, wkk], [smk])
                    dve(lambda e: e.tensor_tensor(out=sm[:, 6:7], in0=sm[:, 3:4], in1=wk[:, KITER:KITER + 1], op=ALU.subtract), [smk, wkk], [smk])

            def stage_A2(i):
                n = (i + 1) * 128
                MBi = MB2[i % 2]; MBk = "MB%d" % (i % 2)
                score = SC2[i % 2]; sck = "score%d" % (i % 2)
                sm = sm2[i % 2]; smk = "sm%d" % (i % 2)
                if i >= 2:
                    dve(lambda e: e.tensor_scalar(out=MBi[:, 0:n], in0=score[:, 0:n], scalar1=sm[:, 6:7], scalar2=NEG, op0=ALU.is_lt, op1=ALU.mult),
                        [sck, smk], [MBk])
                else:
                    dve(lambda e: e.tensor_scalar(out=MBi[:, 0:n], in0=score[:, 0:n], scalar1=-1e29, scalar2=NEG, op0=ALU.is_lt, op1=ALU.mult),
                        [sck], [MBk])

            def stage_B(i):
                qb = qT[i % 2]; qk = "qT%d" % (i % 2)
                MBi = MB2[i % 2]; MBk = "MB%d" % (i % 2)
                P.dma("sp", qb[0:64, 0:512], qT_s[i][:, 0:512], reads=["qT_s%d" % i], writes=[qk])
                P.dma("sp", qb[64:128, 512:1024], qT_s[i][:, 512:1024], reads=["qT_s%d" % i], writes=[qk])
                units = [(j, g) for j in range(i + 1) for g in range(2)]
                Sb = {}

                def emit_S(u):
                    j, g = units[u]
                    bk, bkk = nbank(3)
                    pe(lambda e: [e.matmul(bk[:], lhsT=KT[:, j * 128:(j + 1) * 128], rhs=qb[:, g * 512:(g + 1) * 512], start=True, stop=False),
                                  e.matmul(bk[:], lhsT=MBi[:, j * 128:(j + 1) * 128], rhs=i4[:], start=False, stop=True)][-1],
                       ["KT", qk, MBk, "i4"], [bkk])
                    Sb[u] = (bk, bkk)

                for u in range(min(2, len(units))):
                    emit_S(u)
                for u in range(len(units)):
                    j, g = units[u]
                    bk, bkk = Sb.pop(u)
                    pt = PT[u % 4]; ptk = "PT%d" % (u % 4)
                    act(lambda e: e.activation(out=pt[:], in_=bk[:], func=AF.Exp, scale=0.125), [bkk], [ptk])
                    if u + 2 < len(units):
                        emit_S(u + 2)
                    ob = OB[g]
                    pe(lambda e: [e.matmul(ob[:, hh * 65:(hh + 1) * 65], lhsT=pt[:, hh * 128:(hh + 1) * 128], rhs=V[:, j, g * 65:(g + 1) * 65],
                                           start=(j == 0 and hh == 0), stop=(j == i and hh == 3), skip_group_check=True) for hh in range(4)][-1],
                       [ptk, "V"], ["bk%d" % (4 + g)])
                for g in range(2):
                    ob = OB[g]
                    o3 = ob[:, 0:260].rearrange("p (h d) -> p h d", h=4)
                    act(lambda e: e.activation(out=rdn[:, g * 4:(g + 1) * 4].unsqueeze(2), in_=o3[:, :, 64:65], func=AF.Ln), ["bk%d" % (4 + g)], ["rdn"])
                    act(lambda e: e.activation(out=rdn[:, g * 4:(g + 1) * 4], in_=rdn[:, g * 4:(g + 1) * 4], func=AF.Exp, scale=-1.0), ["rdn"], ["rdn"])
                    for hh in range(4):
                        h_ = g * 4 + hh
                        act(lambda e: e.activation(out=oa[:, h_ * 64:(h_ + 1) * 64], in_=o3[:, hh, 0:64], func=AF.Copy, scale=rdn[:, h_:h_ + 1]),
                            ["bk%d" % (4 + g), "rdn"], ["oa"])
                tb = BK[6][:].bitcast(BF16)
                pe(lambda e: [e.transpose(tb[:, k * 128:(k + 1) * 128], oa[:, k * 128:(k + 1) * 128], identb[:]) for k in range(4)][-1],
                   ["oa", "identb"], ["bk6"])
                ot = oaT[i % 2]; otk = "oaT%d" % (i % 2)
                act(lambda e: e.copy(out=ot[:], in_=tb[:, 0:512].rearrange("p (k t) -> p k t", k=4)), ["bk6"], [otk])
                P.dma("sp", oaT_s[:, :, i * 128:(i + 1) * 128], ot[:], reads=[otk], writes=["oaT_s%d" % i])

            stage_A(0); stage_A2(0)
            if NT > 1:
                stage_A(1); stage_A2(1)
            per_tile = (len(conv_jobs) + NT - 1) // NT
            for i in range(NT):
                if i + 2 < NT:
                    stage_A(i + 2)
                issue_conv(per_tile)
                stage_B(i)
                if i + 2 < NT:
                    stage_A2(i + 2)
            issue_conv(len(conv_jobs))
            P.barrier()

        out_evs = []
        with scope() as p3:
            I32 = mybir.dt.int32
            NM = 2 * S // 512
            gffn_bc = bcast_load(p3, "gffn_bc", g_ffn, D)
            wr = sb(p3, "wr", [128, 8, 36], BF16)
            wba = sb(p3, "wba", [128, 4, D], BF16)
            wo = sb(p3, "wo", [128, 8, D], BF16)
            wload_bf16(wba, w_br_a, 4, D, "wba")
            wload_bf16(wo, w_o, 8, D, "wo")
            brt = sb(p3, "brt", [128, 36], F32)
            P.dma("sp", brt[:, 0:4], b_rg.to_broadcast([128, 4]), writes=["brt"])
            P.dma("sp", brt[:, 4:36], b_re.to_broadcast([128, 32]), writes=["brt"])
            for k in range(8):
                P.dma("pool", wr[:, k, 0:4], w_rg[k * 128:(k + 1) * 128, :], writes=["wr"])
                P.dma("pool", wr[:, k, 4:36], w_re[k * 128:(k + 1) * 128, :], writes=["wr"])
            ltri = sb(p3, "ltri", [128, 128], BF16)
            P.dma("sp", ltri[:], c_ltri, writes=["ltri"])
            thrC = sb(p3, "thrC", [128, 32, NM], F32)
            P.dma("sp", thrC[:], c_thr, writes=["thrC"])
            tmC = sb(p3, "tmC", [128, 32, 32], F32)
            P.dma("sp", tmC[:], c_tm, writes=["tmC"])
            kC = sb(p3, "kC", [128, NTS, 32], F32)
            P.dma("sp", kC[:], c_kc, writes=["kC"])
            io8 = sb(p3, "io8", [128, 10], F32)
            P.dma("sp", io8[:], c_io8, writes=["io8"])
            M1s = sb(p3, "M1s", [128, NT, 32], F32)
            M2s = sb(p3, "M2s", [128, NT, 32], F32)
            rank1 = sb(p3, "rank1", [128, NT], F32)
            rank2 = sb(p3, "rank2", [128, NT], F32)
            W1s = sb(p3, "W1s", [128, NT], F32)
            W2s = sb(p3, "W2s", [128, NT], F32)
            cum = sb(p3, "cum", [128, 32], F32)
            pool(lambda e: e.memset(cum[:], 0.0), [], ["cum"])
            pos1 = sb(p3, "pos1", [128, NT], I32)
            pos2 = sb(p3, "pos2", [128, NT], I32)

            with scope() as pa:
                oab = sb(pa, "oab", [128, 4, 512], BF16)
                mgo = [sb(pa, "mgo%d" % i, [128, 2, 512], BF16) for i in range(2)]
                mTb = sb(pa, "mTb", [128, 8, 512], BF16)
                tt0 = sb(pa, "tt0", [128, 512], F32)
                x1b = [sb(pa, "x1b%d" % i, [128, D], F32) for i in range(4)]
                junk = sb(pa, "junk3", [128, D], BF16)
                h2 = [sb(pa, "h2_%d" % i, [128, D], BF16) for i in range(4)]
                h2Tt = [sb(pa, "h2Tt%d" % i, [128, 8, 128], BF16) for i in range(2)]
                ss4 = sb(pa, "ss4", [128, 4], F32)
                rs4 = sb(pa, "rs4", [128, 4], F32)
                lg4 = sb(pa, "lg4", [128, 4, 36], F32)
                gmx = sb(pa, "gmx", [128, 4], F32)
                oh4 = sb(pa, "oh4", [128, 4, 4], F32)
                eg4 = sb(pa, "eg4", [128, 4, 4], F32)
                se4 = sb(pa, "se4", [128, 4], F32)
                ps4 = sb(pa, "ps4", [128, 4], F32)
                em4 = sb(pa, "em4", [128, 4, 32], F32)
                em24 = sb(pa, "em24", [128, 4, 32], F32)
                m14 = sb(pa, "m14", [128, 4], F32)
                m24 = sb(pa, "m24", [128, 4], F32)
                mk14 = sb(pa, "mk14", [128, 4, 32], F32)
                mk24 = sb(pa, "mk24", [128, 4, 32], F32)
                dm4 = sb(pa, "dm4", [128, 4], F32)
                Mb4 = sb(pa, "Mb4", [128, 4, 32], BF16)
                cumT = sb(pa, "cumT", [128, 4, 32], F32)
                rk4 = sb(pa, "rk4", [128, 4, 32], F32)
                tmp4 = sb(pa, "tmp4", [128, 4, 32], F32)
                brt4 = brt[:].unsqueeze(1).to_broadcast([128, 4, 36])
                for blk in range(NBLK):
                    c0 = blk * 512
                    tb0 = blk * 4
                    P.dma("sp", oab[:], oaT_s[:, :, c0:c0 + 512], reads=["oaT_s%d" % (c0 // 128 + q_) for q_ in range(4)], writes=["oab"])
                    for oc in range(8):
                        mo = mgo[oc % 2]; mok = "mgo%d" % (oc % 2)
                        P.dma("sp", mo[:], mg_s[blk, :, oc], reads=["mg_s%d" % blk], writes=[mok])
                        bk, bkk = nbank()
                        pe(lambda e: [e.matmul(bk[:], lhsT=wba[:, k, oc * 128:(oc + 1) * 128], rhs=oab[:, k, :], start=(k == 0), stop=(k == 3))
                                      for k in range(4)][-1], ["wba", "oab"], [bkk])
                        dve(lambda e: e.tensor_tensor(out=tt0[:], in0=bk[:], in1=mo[:, 0, :], op=ALU.mult), [bkk, mok], ["tt0"])
                        pool(lambda e: e.tensor_tensor(out=mTb[:, oc, :], in0=tt0[:], in1=mo[:, 1, :], op=ALU.add), ["tt0", mok], ["mTb"])
                    for tl in range(4):
                        t = tb0 + tl
                        xb = x1b[tl]; xk = "x1b%d" % tl
                        P.dma("sp", xb[:], x[t * 128:(t + 1) * 128, :], writes=[xk])
                        for half in range(2):
                            bk, bkk = nbank()
                            pe(lambda e: [e.matmul(bk[:], lhsT=mTb[:, k, tl * 128:(tl + 1) * 128], rhs=wo[:, k, half * 512:(half + 1) * 512],
                                                   start=(k == 0), stop=(k == 7)) for k in range(8)][-1], ["mTb", "wo"], [bkk])
                            dve(lambda e: e.tensor_tensor(out=xb[:, half * 512:(half + 1) * 512], in0=bk[:],
                                                          in1=xb[:, half * 512:(half + 1) * 512], op=ALU.add), [bkk, xk], [xk])
                        P.dma("sp", x1_s[t * 128:(t + 1) * 128, :], xb[:], reads=[xk], writes=["x1_s%d" % t])
                        act(lambda e: e.activation(out=junk[:], in_=xb[:], func=AF.Square, accum_out=ss4[:, tl:tl + 1]), [xk], ["junk3", "ss4"])
                    act(lambda e: e.activation(out=rs4[:], in_=ss4[:], func=AF.Sqrt, bias=epsb[:], scale=1.0 / D), ["ss4", "epsb"], ["rs4"])
                    dve(lambda e: e.reciprocal(out=rs4[:], in_=rs4[:]), ["rs4"], ["rs4"])
                    for tl in range(4):
                        t = tb0 + tl
                        xb = x1b[tl]; xk = "x1b%d" % tl
                        hb = h2[tl]; hk = "h2_%d" % tl
                        hT_ = h2Tt[tl % 2]; hTk = "h2Tt%d" % (tl % 2)
                        dve(lambda e: e.scalar_tensor_tensor(out=hb[:], in0=xb[:], scalar=rs4[:, tl:tl + 1], in1=gffn_bc[:], op0=ALU.mult, op1=ALU.mult),
                            [xk, "rs4", "gffn_bc"], [hk])
                        P.dma("sp", h2_s[t * 128:(t + 1) * 128, :], hb[:], reads=[hk], writes=["h2_s%d" % t])
                        tb = BK[6][:].bitcast(BF16)
                        pe(lambda e: [e.transpose(tb[:, k * 128:(k + 1) * 128], hb[:, k * 128:(k + 1) * 128], identb[:]) for k in range(8)][-1],
                           [hk, "identb"], ["bk6"])
                        act(lambda e: e.copy(out=hT_[:], in_=tb.rearrange("p (k t) -> p k t", k=8)), ["bk6"], [hTk])
                        pe(lambda e: [e.matmul(BK[7][:, tl * 64:tl * 64 + 36], lhsT=hT_[:, k, :], rhs=wr[:, k, :], start=(k == 0), stop=(k == 7))
                                      for k in range(8)][-1], [hTk, "wr"], ["bk7"])
                    lgv = BK[7][:, 0:256].rearrange("p (t c) -> p t c", t=4)[:, :, 0:36]
                    dve(lambda e: e.tensor_tensor(out=lg4[:], in0=lgv, in1=brt4, op=ALU.add), ["bk7", "brt"], ["lg4"])
                    gl = lg4[:, :, 0:4]
                    el = lg4[:, :, 4:36]
                    dve(lambda e: e.tensor_reduce(out=gmx[:], in_=gl, axis=AX.X, op=ALU.max), ["lg4"], ["gmx"])
                    dve(lambda e: e.tensor_tensor(out=oh4[:], in0=gl, in1=gmx[:].unsqueeze(2).to_broadcast([128, 4, 4]), op=ALU.is_ge), ["lg4", "gmx"], ["oh4"])
                    dve(lambda e: e.tensor_tensor(out=eg4[:], in0=gl, in1=gmx[:].unsqueeze(2).to_broadcast([128, 4, 4]), op=ALU.subtract), ["lg4", "gmx"], ["eg4"])
                    act(lambda e: e.activation(out=eg4[:], in_=eg4[:], func=AF.Exp), ["eg4"], ["eg4"])
                    dve(lambda e: e.tensor_reduce(out=se4[:], in_=eg4[:], axis=AX.X, op=ALU.add), ["eg4"], ["se4"])
                    dve(lambda e: e.reciprocal(out=ps4[:], in_=se4[:]), ["se4"], ["ps4"])
                    dve(lambda e: e.tensor_scalar(out=oh4[:], in0=oh4[:], scalar1=1.0, scalar2=1e9, op0=ALU.subtract, op1=ALU.mult), ["oh4"], ["oh4"])
                    dve(lambda e: e.tensor_tensor(out=em4[:].rearrange("p t (g x) -> p t g x", g=4), in0=el.rearrange("p t (g x) -> p t g x", g=4),
                                                  in1=oh4[:].unsqueeze(3).to_broadcast([128, 4, 4, 8]), op=ALU.add), ["lg4", "oh4"], ["em4"])
                    dve(lambda e: e.tensor_reduce(out=m14[:], in_=em4[:], axis=AX.X, op=ALU.max), ["em4"], ["m14"])
                    dve(lambda e: e.tensor_tensor(out=mk14[:], in0=em4[:], in1=m14[:].unsqueeze(2).to_broadcast([128, 4, 32]), op=ALU.is_ge), ["em4", "m14"], ["mk14"])
                    dve(lambda e: e.scalar_tensor_tensor(out=em24[:].rearrange("p t e -> p (t e)"), in0=mk14[:].rearrange("p t e -> p (t e)"), scalar=-1e9,
                                                         in1=em4[:].rearrange("p t e -> p (t e)"), op0=ALU.mult, op1=ALU.add), ["mk14", "em4"], ["em24"])
                    dve(lambda e: e.tensor_reduce(out=m24[:], in_=em24[:], axis=AX.X, op=ALU.max), ["em24"], ["m24"])
                    dve(lambda e: e.tensor_tensor(out=mk24[:], in0=em24[:], in1=m24[:].unsqueeze(2).to_broadcast([128, 4, 32]), op=ALU.is_ge), ["em24", "m24"], ["mk24"])
                    dve(lambda e: e.tensor_tensor(out=dm4[:], in0=m24[:], in1=m14[:], op=ALU.subtract), ["m24", "m14"], ["dm4"])
                    act(lambda e: e.activation(out=dm4[:], in_=dm4[:], func=AF.Exp), ["dm4"], ["dm4"])
                    dve(lambda e: e.tensor_scalar(out=dm4[:], in0=dm4[:], scalar1=1.0, scalar2=None, op0=ALU.add), ["dm4"], ["dm4"])
                    dve(lambda e: e.reciprocal(out=dm4[:], in_=dm4[:]), ["dm4"], ["dm4"])
                    dve(lambda e: e.tensor_tensor(out=W1s[:, tb0:tb0 + 4], in0=dm4[:], in1=ps4[:], op=ALU.mult), ["dm4", "ps4"], ["W1s"])
                    dve(lambda e: e.tensor_tensor(out=W2s[:, tb0:tb0 + 4], in0=ps4[:], in1=W1s[:, tb0:tb0 + 4], op=ALU.subtract), ["ps4", "W1s"], ["W2s"])
                    dve(lambda e: e.tensor_tensor(out=Mb4[:], in0=mk14[:], in1=mk24[:], op=ALU.add), ["mk14", "mk24"], ["Mb4"])
                    pe(lambda e: [[e.matmul(BK[7][:, 256 + tl * 32:256 + (tl + 1) * 32], lhsT=ltri[:], rhs=Mb4[:, tl, :], start=True, stop=True),
                                   e.matmul(BK[7][:, 384 + tl * 32:384 + (tl + 1) * 32], lhsT=onesb[:], rhs=Mb4[:, tl, :], start=True, stop=True)][-1]
                                  for tl in range(4)][-1], ["ltri", "onesb", "Mb4"], ["bk7"])
                    pool(lambda e: e.tensor_copy(out=cumT[:, 0, :], in_=cum[:]), ["cum"], ["cumT"])
                    for tl in range(1, 4):
                        dve(lambda e: e.tensor_tensor(out=cumT[:, tl, :], in0=BK[7][:, 384 + (tl - 1) * 32:384 + tl * 32], in1=cumT[:, tl - 1, :], op=ALU.add),
                            ["bk7", "cumT"], ["cumT"])
                    dve(lambda e: e.tensor_tensor(out=cum[:], in0=BK[7][:, 480:512], in1=cumT[:, 3, :], op=ALU.add), ["bk7", "cumT"], ["cum"])
                    dve(lambda e: e.tensor_tensor(out=rk4[:], in0=BK[7][:, 256:384].rearrange("p (t e) -> p t e", t=4), in1=cumT[:], op=ALU.add),
                        ["bk7", "cumT"], ["rk4"])
                    dve(lambda e: e.tensor_tensor(out=tmp4[:], in0=mk14[:], in1=rk4[:], op=ALU.mult), ["mk14", "rk4"], ["tmp4"])
                    dve(lambda e: e.tensor_reduce(out=rank1[:, tb0:tb0 + 4], in_=tmp4[:], axis=AX.X, op=ALU.add), ["tmp4"], ["rank1"])
                    dve(lambda e: e.tensor_tensor(out=tmp4[:], in0=mk24[:], in1=rk4[:], op=ALU.mult), ["mk24", "rk4"], ["tmp4"])
                    dve(lambda e: e.tensor_reduce(out=rank2[:, tb0:tb0 + 4], in_=tmp4[:], axis=AX.X, op=ALU.add), ["tmp4"], ["rank2"])
                    pool(lambda e: e.tensor_copy(out=M1s[:, tb0:tb0 + 4, :], in_=mk14[:]), ["mk14"], ["M1s"])
                    pool(lambda e: e.tensor_copy(out=M2s[:, tb0:tb0 + 4, :], in_=mk24[:]), ["mk24"], ["M2s"])
                P.barrier()

            Ek = sb(p3, "Ek", [128, NTS], F32)
            idxu = sb(p3, "idxu", [128, NTS, 8], I32)
            idxd = sb(p3, "idxd", [128, NTS, 2], I32)
            with scope() as pb:
                big = sb(pb, "bigtmp", [128, max(NT, NTS, 32) * 32], F32)
                ntl = sb(pb, "ntl", [128, 32], F32)
                offt = sb(pb, "offt", [128, 32], F32)
                offs = sb(pb, "offs", [128, 32], F32)
                pf = sb(pb, "pf", [128, NT], F32)
                ef = sb(pb, "ef", [128, NTS, 8], F32)
                b3 = big[:, 0:32 * NM].rearrange("p (e m) -> p e m", e=32)
                dve(lambda e: e.tensor_tensor(out=b3, in0=cum[:].unsqueeze(2).to_broadcast([128, 32, NM]), in1=thrC[:], op=ALU.is_gt),
                    ["cum", "thrC"], ["bigtmp"])
                dve(lambda e: e.tensor_reduce(out=ntl[:], in_=b3, axis=AX.X, op=ALU.add), ["bigtmp"], ["ntl"])
                b4 = big[:, 0:1024].rearrange("p (e f) -> p e f", e=32)
                dve(lambda e: e.tensor_tensor(out=b4, in0=ntl[:].unsqueeze(1).to_broadcast([128, 32, 32]), in1=tmC[:], op=ALU.mult),
                    ["ntl", "tmC"], ["bigtmp"])
                dve(lambda e: e.tensor_reduce(out=offt[:], in_=b4, axis=AX.X, op=ALU.add), ["bigtmp"], ["offt"])
                dve(lambda e: e.tensor_scalar(out=offs[:], in0=offt[:], scalar1=512.0, scalar2=None, op0=ALU.mult), ["offt"], ["offs"])
                b5 = big[:, 0:NTS * 32].rearrange("p (k e) -> p k e", k=NTS)
                dve(lambda e: e.tensor_tensor(out=b5, in0=offt[:].unsqueeze(1).to_broadcast([128, NTS, 32]), in1=kC[:], op=ALU.is_le),
                    ["offt", "kC"], ["bigtmp"])
                dve(lambda e: e.tensor_reduce(out=Ek[:], in_=b5, axis=AX.X, op=ALU.add), ["bigtmp"], ["Ek"])
                dve(lambda e: e.tensor_scalar(out=Ek[:], in0=Ek[:], scalar1=-1.0, scalar2=None, op0=ALU.add), ["Ek"], ["Ek"])
                dve(lambda e: e.scalar_tensor_tensor(out=ef[:, :, 0], in0=Ek[:], scalar=128.0,
                                                     in1=io8[:, 8:9].to_broadcast([128, NTS]), op0=ALU.mult, op1=ALU.add),
                    ["Ek", "io8"], ["ef"])
                dve(lambda e: e.tensor_copy(out=idxu[:, :, 0], in_=ef[:, :, 0]), ["ef"], ["idxu"])
                for (Ms, Mk, rnk, rkk, ps, pk) in ((M1s, "M1s", rank1, "rank1", pos1, "pos1"), (M2s, "M2s", rank2, "rank2", pos2, "pos2")):
                    b6 = big[:, 0:NT * 32].rearrange("p (t e) -> p t e", t=NT)
                    dve(lambda e: e.tensor_tensor(out=b6, in0=Ms[:], in1=offs[:].unsqueeze(1).to_broadcast([128, NT, 32]), op=ALU.mult),
                        [Mk, "offs"], ["bigtmp"])
                    dve(lambda e: e.tensor_reduce(out=pf[:], in_=b6, axis=AX.X, op=ALU.add), ["bigtmp"], ["pf"])
                    dve(lambda e: e.tensor_tensor(out=pf[:], in0=pf[:], in1=rnk[:], op=ALU.add), ["pf", rkk], ["pf"])
                    dve(lambda e: e.tensor_copy(out=ps[:], in_=pf[:]), ["pf"], [pk])
                hrow = [sb(pb, "hrow%d" % i, [128, D], BF16) for i in range(3)]
                for t in range(NT):
                    hb = hrow[t % 3]; hk = "hrow%d" % (t % 3)
                    P.dma("sp", hb[:], h2_s[t * 128:(t + 1) * 128, :], reads=["h2_s%d" % t], writes=[hk])
                    P.idma(Hs[:, :], hb[:, :], pos1[:, t:t + 1], False, reads=[hk, "pos1"], writes=[])
                    P.idma(Hs[:, :], hb[:, :], pos2[:, t:t + 1], False, reads=[hk, "pos2"], writes=[])
                P.barrier()

            with scope() as pc:
                wu = [sb(pc, "wu%d" % i, [128, 8, 512], BF16) for i in range(3)]
                wd = [sb(pc, "wd%d" % i, [128, 2, D], BF16) for i in range(3)]
                hs = [sb(pc, "hs%d" % i, [128, D], BF16) for i in range(4)]
                HT = [sb(pc, "HT%d" % i, [128, 8, 512], BF16) for i in range(2)]
                sa = [sb(pc, "sa%d" % i, [128, 512], F32) for i in range(2)]
                gg = [sb(pc, "gg%d" % i, [128, 2, 512], BF16) for i in range(2)]
                ysb = [sb(pc, "ysb%d" % i, [128, D], F32) for i in range(2)]
                wub_v = wub_s
                wdb_v = wdb_s

                def load_w(k):
                    P.idma(wu[k % 3][:].rearrange("p k c -> p (k c)"), wub_v[:, :], idxu[:, k, 0:1], True, reads=["idxu"], writes=["wu%d" % (k % 3)])
                    P.idma(wd[k % 3][:].rearrange("p a c -> p (a c)"), wdb_v[:, :], idxu[:, k, 0:1], True, reads=["idxu"], writes=["wd%d" % (k % 3)])

                def load_h(k):
                    HTk = HT[k % 2]; HTkk = "HT%d" % (k % 2)
                    for sub in range(4):
                        hb = hs[sub]; hk = "hs%d" % sub
                        r0 = k * 512 + sub * 128
                        P.dma("sp", hb[:], Hs[r0:r0 + 128, :], writes=[hk])
                        tb = BK[6][:].bitcast(BF16)
                        pe(lambda e: [e.transpose(tb[:, kk * 128:(kk + 1) * 128], hb[:, kk * 128:(kk + 1) * 128], identb[:]) for kk in range(8)][-1],
                           [hk, "identb"], ["bk6"])
                        act(lambda e: e.copy(out=HTk[:, :, sub * 128:(sub + 1) * 128], in_=tb.rearrange("p (k t) -> p k t", k=8)), ["bk6"], [HTkk])

                pend = None

                def emit_down(pd):
                    k_, ci_ = pd
                    wdb_ = wd[k_ % 3]; wdk_ = "wd%d" % (k_ % 3)
                    for sub in range(4):
                        yb = ysb[sub % 2]; yk = "ysb%d" % (sub % 2)
                        for half in range(2):
                            di = 4 + ((sub * 2 + half) % 2)
                            pe(lambda e: [e.matmul(BK[di][:], lhsT=gg[ci_][:, a, sub * 128:(sub + 1) * 128], rhs=wdb_[:, a, half * 512:(half + 1) * 512],
                                                   start=(a == 0), stop=(a == 1)) for a in range(2)][-1], ["gg%d" % ci_, wdk_], ["bk%d" % di])
                            if half == 0:
                                act(lambda e: e.copy(out=yb[:, 0:512], in_=BK[di][:]), ["bk%d" % di], [yk])
                            else:
                                dve(lambda e: e.tensor_copy(out=yb[:, 512:1024], in_=BK[di][:]), ["bk%d" % di], [yk])
                        r0 = k_ * 512 + sub * 128
                        P.dma("sp", Ysc[r0:r0 + 128, :], yb[:], reads=[yk], writes=[])

                load_w(0)
                if NTS > 1:
                    load_w(1)
                load_h(0)
                for k in range(NTS):
                    ci = k % 2
                    wub = wu[k % 3]; wuk = "wu%d" % (k % 3)
                    HTk = HT[k % 2]; HTkk = "HT%d" % (k % 2)
                    for a in range(2):
                        for fc in (a, 2 + a):
                            pe(lambda e: [e.matmul(BK[fc][:], lhsT=wub[:, kk, fc * 128:(fc + 1) * 128], rhs=HTk[:, kk, :],
                                                   start=(kk == 0), stop=(kk == 7)) for kk in range(8)][-1], [wuk, HTkk], ["bk%d" % fc])
                        act(lambda e: e.activation(out=sa[a][:], in_=BK[a][:], func=AF.Silu), ["bk%d" % a], ["sa%d" % a])
                        dve(lambda e: e.tensor_tensor(out=gg[ci][:, a, :], in0=BK[2 + a][:], in1=sa[a][:], op=ALU.mult),
                            ["bk%d" % (2 + a), "sa%d" % a], ["gg%d" % ci])
                    if k + 1 < NTS:
                        load_h(k + 1)
                    if pend is not None:
                        emit_down(pend)
                    pend = (k, ci)
                    if k + 2 < NTS:
                        load_w(k + 2)
                emit_down(pend)
                P.barrier()

            with scope() as pd_:
                xf = [sb(pd_, "xf%d" % i, [128, D], F32) for i in range(2)]
                y1 = [sb(pd_, "y1_%d" % i, [128, D], F32) for i in range(2)]
                y2 = [sb(pd_, "y2_%d" % i, [128, D], F32) for i in range(2)]
                for t in range(NT):
                    xb = xf[t % 2]; xk = "xf%d" % (t % 2)
                    a1 = y1[t % 2]; a1k = "y1_%d" % (t % 2)
                    a2 = y2[t % 2]; a2k = "y2_%d" % (t % 2)
                    P.dma("sp", xb[:], x1_s[t * 128:(t + 1) * 128, :], reads=["x1_s%d" % t], writes=[xk])
                    P.idma(a1[:, :], Ysc[:, :], pos1[:, t:t + 1], True, reads=["pos1"], writes=[a1k])
                    P.idma(a2[:, :], Ysc[:, :], pos2[:, t:t + 1], True, reads=["pos2"], writes=[a2k])
                    dve(lambda e: e.scalar_tensor_tensor(out=xb[:], in0=a1[:], scalar=W1s[:, t:t + 1], in1=xb[:], op0=ALU.mult, op1=ALU.add),
                        [a1k, "W1s", xk], [xk])
                    dve(lambda e: e.scalar_tensor_tensor(out=xb[:], in0=a2[:], scalar=W2s[:, t:t + 1], in1=xb[:], op0=ALU.mult, op1=ALU.add),
                        [a2k, "W2s", xk], [xk])
                    out_evs.append(P.dma("sp", out[t * 128:(t + 1) * 128, :], xb[:], reads=[xk], writes=["out%d" % t]))
            P.final_wait("sp", out_evs)
        P.emit()
    return nc


def _consts(S):
    NT = S // 128
    bf = ml_dtypes.bfloat16
    c = {}
    c["c_identb"] = np.eye(128, dtype=np.float32).astype(bf)
    c["c_identf"] = np.eye(128, dtype=np.float32)
    c["c_i4"] = np.tile(np.eye(128, dtype=np.float32), (1, 4)).astype(bf)
    t = np.arange(128)[:, None]; s = np.arange(128)[None, :]
    c["c_cb"] = np.where(s <= t, 0.0, -1e30).astype(np.float32)
    half = 32
    inv = (10000.0 ** (-np.arange(half, dtype=np.float32) / half)).astype(np.float32)
    pos = np.arange(S, dtype=np.float32)
    ang = (pos[:, None] * inv[None, :]).astype(np.float32)
    cos = np.cos(ang).astype(np.float32).reshape(NT, 128, 32).transpose(1, 0, 2)
    sin = np.sin(ang).astype(np.float32).reshape(NT, 128, 32).transpose(1, 0, 2)
    c["c_cos"] = np.ascontiguousarray(cos); c["c_sin"] = np.ascontiguousarray(sin)
    NTS = (2 * S + 32 * 511 + 511) // 512
    NM = 2 * S // 512
    kk = np.arange(128)
    c["c_ltri"] = (kk[:, None] < kk[None, :]).astype(np.float32).astype(bf)
    c["c_thr"] = np.ascontiguousarray(np.broadcast_to((512.0 * np.arange(NM, dtype=np.float32))[None, None, :], (128, 32, NM))).astype(np.float32)
    ee = np.arange(32)
    c["c_tm"] = np.ascontiguousarray(np.broadcast_to((ee[None, :] < ee[:, None]).astype(np.float32)[None], (128, 32, 32)))
    c["c_kc"] = np.ascontiguousarray(np.broadcast_to(np.arange(NTS, dtype=np.float32)[None, :, None], (128, NTS, 32))).astype(np.float32)
    io = np.zeros((128, 10), np.float32)
    for k8 in range(8):
        io[:, k8] = k8 * 128 + np.arange(128)
    for a in range(2):
        io[:, 8 + a] = a * 128 + np.arange(128)
    c["c_io8"] = io
    c["c_pow2"] = np.tile((2.0 ** -(np.arange(KITER + 1) + 1.0)).astype(np.float32)[None, :], (128, 1))
    return c


def _prep(inp, S):
    f = lambda a: np.ascontiguousarray(np.asarray(a, dtype=np.float32))
    w = {}
    w["g_mix"] = f(inp["g_mix"]).reshape(1, D); w["w_in"] = f(inp["w_in"]).reshape(D, 5960)
    w["b_gate"] = f(inp["b_gate"]).reshape(24, 128)
    w["g_qa"] = f(inp["g_qa"]).reshape(1, 64); w["g_ka"] = f(inp["g_ka"]).reshape(1, 64); w["g_idx_k"] = f(inp["g_idx_k"]).reshape(1, 64)
    w["conv_w"] = f(inp["conv_w"]).reshape(31, 512); w["conv_b"] = f(inp["conv_b"]).reshape(4, 128)
    w["ln_g"] = f(inp["ln_g"]).reshape(4, 128); w["ln_b"] = f(inp["ln_b"]).reshape(4, 128)
    w["g_mem"] = f(inp["g_mem"]).reshape(1, D); w["w_mem_kv"] = f(inp["w_mem_kv"]).reshape(D, 1024)
    w["g_qm"] = f(inp["g_qm"]).reshape(1, 128); w["g_km"] = f(inp["g_km"]).reshape(1, 128)
    w["w_br_a"] = f(inp["w_br_a"]).reshape(512, D); w["w_br_b"] = f(inp["w_br_b"]).reshape(512, D); w["w_br_m"] = f(inp["w_br_m"]).reshape(512, D)
    w["w_o"] = f(inp["w_o"]).reshape(D, D); w["g_ffn"] = f(inp["g_ffn"]).reshape(1, D)
    w["w_rg"] = f(inp["w_rg"]).reshape(D, 4); w["b_rg"] = f(inp["b_rg"]).reshape(1, 4)
    w["w_re"] = f(inp["w_re"]).reshape(D, 32); w["b_re"] = f(inp["b_re"]).reshape(1, 32)
    w["w_up"] = f(inp["w_up"]).reshape(32, D, 512); w["w_down"] = f(inp["w_down"]).reshape(32, 256, D)
    w.update(_consts(S))
    return w


def kernel(**inputs):
    x = np.asarray(inputs["x"], dtype=np.float32)
    mem = np.asarray(inputs["mem"], dtype=np.float32)
    B, S, _ = x.shape
    shared = _prep(inputs, S)
    nc = build(S)
    in_maps = []
    for bi in range(B):
        m = dict(shared)
        m["x"] = np.ascontiguousarray(x[bi])
        m["mem"] = np.ascontiguousarray(mem[bi])
        in_maps.append(m)
    res = run_bass_kernel_spmd(nc, in_maps, core_ids=list(range(B)))
    return np.stack([np.asarray(r["out"]).reshape(S, D) for r in res.results], axis=0).astype(np.float32)
```

```python
import numpy as np
import ml_dtypes
from contextlib import ExitStack
import concourse.bass as bass
import concourse.mybir as mybir
from concourse.bass_utils import run_bass_kernel_spmd

F32 = mybir.dt.float32
BF16 = mybir.dt.bfloat16
ALU = mybir.AluOpType
AF = mybir.ActivationFunctionType
AX = mybir.AxisListType

D = 1024
EPS = 1e-6
C_QA, C_KA, C_VA, C_QI, C_KI, C_WI, C_CV, C_QM, C_GT = 0, 512, 640, 768, 1280, 1344, 1352, 2376, 2888
N_TM = 1352
N_FM = 5960 - 1352
KITER = 14
TOPK = 256
NEG = -30000.0

ENGS = ("pe", "act", "dve", "pool", "sp")
SEM_LIMIT = 30000
N_DMA_SEMS = 32


class _Rec:
    def __init__(self):
        self.calls = []

    def __getattr__(self, name):
        def f(*a, **kw):
            self.calls.append((name, a, kw))
            return self
        return f


def _replayer(calls):
    def run(e):
        r = None
        for (name, a, kw) in calls:
            r = getattr(e, name)(*a, **kw)
        return r
    return run


class Prog:
    def __init__(self, nc, stack):
        self.nc = nc
        self.stack = stack
        self.q = {e: [] for e in ENGS}
        self.nsem = 0
        self.cur = {}
        self.cnt = {}
        for e in ENGS:
            self.cur[e] = self._new_sem(e)
            self.cnt[e] = 0
        self.dma_sems = [self._new_sem("dma%d" % i) for i in range(N_DMA_SEMS)]
        self.dma_cnt = [0] * N_DMA_SEMS
        self.n_hw = 16
        self.dma_next_hw = 0
        self.dma_next_sw = 0
        self.last_w = {}
        self.readers = {}
        self.seen = {e: {} for e in ENGS}
        self.n_ops = 0

    def _new_sem(self, name):
        self.nsem += 1
        return self.stack.enter_context(self.nc.semaphore("s_%s_%d" % (name, self.nsem)))

    def _deps(self, reads, writes, skip_src=None):
        ev = []
        for k in reads:
            w = self.last_w.get(k)
            if w is not None and w[0] != skip_src:
                ev.append(w[1])
        for k in writes:
            w = self.last_w.get(k)
            if w is not None and w[0] != skip_src:
                ev.append(w[1])
            r = self.readers.get(k)
            if r:
                for src, e in r.items():
                    if src != skip_src:
                        ev.append(e)
        return ev

    def _waits(self, eng, evs):
        best = {}
        for (sem, val) in evs:
            i = id(sem)
            if self.seen[eng].get(i, 0) < val:
                if i not in best or best[i][1] < val:
                    best[i] = (sem, val)
        out = []
        for i, (sem, val) in best.items():
            self.seen[eng][i] = val
            out.append((sem, val))
        return out

    def _record(self, ev, reads, writes, src):
        for k in writes:
            self.last_w[k] = (src, ev)
            self.readers[k] = {}
        for k in reads:
            self.readers.setdefault(k, {})[src] = ev

    def op(self, eng, fn, reads=(), writes=()):
        waits = self._waits(eng, self._deps(reads, writes, "pe" if eng == "pe" else None))
        if self.cnt[eng] >= SEM_LIMIT:
            self.cur[eng] = self._new_sem(eng)
            self.cnt[eng] = 0
        self.cnt[eng] += 1
        sem = self.cur[eng]
        ev = (sem, self.cnt[eng])
        rec = _Rec()
        fn(rec)
        assert rec.calls
        self.q[eng].append((_replayer(rec.calls), waits, sem, 1))
        self._record(ev, reads, writes, eng)
        self.n_ops += 1
        return ev

    def _slot(self, eng):
        if eng == "pool":
            s = self.n_hw + self.dma_next_sw
            self.dma_next_sw = (self.dma_next_sw + 1) % (N_DMA_SEMS - self.n_hw)
        else:
            s = self.dma_next_hw
            self.dma_next_hw = (self.dma_next_hw + 1) % self.n_hw
        return s

    def dma(self, eng, out, in_, reads=(), writes=(), **kw):
        s = self._slot(eng)
        sem = self.dma_sems[s]
        evs = self._deps(reads, writes)
        if self.dma_cnt[s] > 0:
            evs.append((sem, self.dma_cnt[s]))
        waits = self._waits(eng, evs)
        self.dma_cnt[s] += 16
        ev = (sem, self.dma_cnt[s])

        def fn(e, out=out, in_=in_, kw=kw):
            return e.dma_start(out=out, in_=in_, **kw)

        self.q[eng].append((fn, waits, sem, 16))
        self._record(ev, reads, writes, ("dma", s))
        self.n_ops += 1
        return ev

    def idma(self, out, in_, idx_ap, gather, reads=(), writes=(), bound=None):
        eng = "pool"
        s = self._slot(eng)
        sem = self.dma_sems[s]
        evs = self._deps(reads, writes)
        if self.dma_cnt[s] > 0:
            evs.append((sem, self.dma_cnt[s]))
        waits = self._waits(eng, evs)
        self.dma_cnt[s] += 16
        ev = (sem, self.dma_cnt[s])
        off = bass.IndirectOffsetOnAxis(ap=idx_ap, axis=0)

        def fn(e):
            kw = {}
            if bound is not None:
                kw = dict(bounds_check=bound, oob_is_err=False)
            if gather:
                return e.indirect_dma_start(out=out, out_offset=None, in_=in_, in_offset=off, **kw)
            return e.indirect_dma_start(out=out, out_offset=off, in_=in_, in_offset=None, **kw)

        self.q[eng].append((fn, waits, sem, 16))
        self._record(ev, reads, writes, ("dma", s))
        self.n_ops += 1
        return ev

    def barrier(self):
        evs = [(self.cur[e], self.cnt[e]) for e in ENGS if self.cnt[e] > 0]
        evs += [(self.dma_sems[s], self.dma_cnt[s]) for s in range(N_DMA_SEMS) if self.dma_cnt[s] > 0]
        for e in ENGS:
            w = self._waits(e, evs)
            if w:
                self.q[e].append((None, w, None, 0))

    def final_wait(self, eng, evs):
        waits = self._waits(eng, evs)
        self.q[eng].append((None, waits, None, 0))

    def emit(self):
        nc = self.nc
        with nc.Block() as block:
            def mk(q):
                def body(e):
                    for (fn, waits, sem, inc) in q:
                        for (ws, wv) in waits:
                            e.wait_ge(ws, wv)
                        if fn is not None:
                            fn(e).then_inc(sem, inc)
                return body
            block.tensor(mk(self.q["pe"]))
            block.scalar(mk(self.q["act"]))
            block.vector(mk(self.q["dve"]))
            block.gpsimd(mk(self.q["pool"]))
            block.sync(mk(self.q["sp"]))


def build(S, dbg=False):
    NT = S // 128
    NBLK = S // 512
    SB = min(2048, S)
    NSB = S // SB
    nc = bass.Bass("TRN2", target_bir_lowering=False)

    def din(name, shape, dt=F32):
        return nc.dram_tensor(name, list(shape), dt, kind="ExternalInput").ap()

    def dscr(name, shape, dt):
        kind = "ExternalOutput" if dbg else "Internal"
        return nc.dram_tensor(name, list(shape), dt, kind=kind).ap()

    x = din("x", [S, D]); mem = din("mem", [256, D])
    g_mix = din("g_mix", [1, D]); w_in = din("w_in", [D, 5960]); b_gate = din("b_gate", [24, 128])
    g_qa = din("g_qa", [1, 64]); g_ka = din("g_ka", [1, 64]); g_idx_k = din("g_idx_k", [1, 64])
    conv_w = din("conv_w", [31, 512]); conv_b = din("conv_b", [4, 128])
    ln_g = din("ln_g", [4, 128]); ln_b = din("ln_b", [4, 128])
    g_mem = din("g_mem", [1, D]); w_mem_kv = din("w_mem_kv", [D, 1024])
    g_qm = din("g_qm", [1, 128]); g_km = din("g_km", [1, 128])
    w_br_a = din("w_br_a", [512, D]); w_br_b = din("w_br_b", [512, D]); w_br_m = din("w_br_m", [512, D])
    w_o = din("w_o", [D, D]); g_ffn = din("g_ffn", [1, D])
    w_rg = din("w_rg", [D, 4]); b_rg = din("b_rg", [1, 4]); w_re = din("w_re", [D, 32]); b_re = din("b_re", [1, 32])
    w_up = din("w_up", [32, D, 512]); w_down = din("w_down", [32, 256, D])
    c_identb = din("c_identb", [128, 128], BF16); c_identf = din("c_identf", [128, 128])
    c_i4 = din("c_i4", [128, 512], BF16); c_cb = din("c_cb", [128, 128])
    c_cos = din("c_cos", [128, NT, 32]); c_sin = din("c_sin", [128, NT, 32])
    c_pow2 = din("c_pow2", [128, KITER + 1])
    NTS_ = (2 * S + 32 * 511 + 511) // 512
    c_ltri = din("c_ltri", [128, 128], BF16); c_thr = din("c_thr", [128, 32, 2 * S // 512]); c_tm = din("c_tm", [128, 32, 32])
    c_kc = din("c_kc", [128, NTS_, 32]); c_io8 = din("c_io8", [128, 10])
    out = nc.dram_tensor("out", [S, D], F32, kind="ExternalOutput").ap()

    qT_s = dscr("qT_s", [NT, 64, 1024], BF16)
    qiT_s = dscr("qiT_s", [NT, 64, 1024], BF16)
    kT_s = dscr("kT_s", [2, 64, S], BF16)
    kiT_s = dscr("kiT_s", [64, S], BF16)
    v_s = dscr("v_s", [S, 130], BF16)
    wi_s = dscr("wi_s", [S, 8], F32)
    mg_s = dscr("mg_s", [NBLK, 128, 8, 2, 512], BF16)
    oaT_s = dscr("oaT_s", [128, 4, S], BF16)
    NTS = (2 * S + 32 * 511 + 511) // 512
    x1_s = dscr("x1_s", [S, D], F32)
    h2_s = dscr("h2_s", [S, D], BF16)
    Hs = dscr("Hs", [NTS * 512, D], BF16)
    Ysc = dscr("Ysc", [NTS * 512, D], F32)
    wub_s = dscr("wub_s", [32 * 128, 8 * 512], BF16)
    wdb_s = dscr("wdb_s", [32 * 128, 2 * D], BF16)

    st = ExitStack()
    with st:
        P = Prog(nc, st)

        ARENA_F32 = 50688
        arena = st.enter_context(nc.sbuf_tensor("arena", [128, ARENA_F32], F32))
        top = {"v": 0, "max": 0}

        class _Scope:
            def __init__(self, s):
                self.s = s
            def __enter__(self):
                self.s.__enter__()
                self.mark = top["v"]
                return self.s
            def __exit__(self, *a):
                top["v"] = self.mark
                return self.s.__exit__(*a)

        def scope():
            return _Scope(ExitStack())

        def sb(stack, name, shape, dt):
            esz = 4 if dt in (F32, mybir.dt.int32) else 2
            n = 1
            for d_ in shape[1:]:
                n *= d_
            nwords = (n * esz + 3) // 4
            nwords = (nwords + 7) // 8 * 8
            off = top["v"]
            top["v"] = off + nwords
            top["max"] = max(top["max"], top["v"])
            assert top["v"] <= ARENA_F32, ("SBUF arena overflow", name, top["v"])
            v = arena[0:shape[0], off:off + nwords]
            if dt != F32:
                v = v.bitcast(dt)
            v = v[:, 0:n]
            if len(shape) == 3:
                v = v.rearrange("p (a b) -> p a b", a=shape[1])
            elif len(shape) == 4:
                v = v.rearrange("p (a b c) -> p a b c", a=shape[1], b=shape[2])
            return v

        def pe(fn, r, w): return P.op("pe", fn, r, w)
        def act(fn, r, w): return P.op("act", fn, r, w)
        def dve(fn, r, w): return P.op("dve", fn, r, w)
        def pool(fn, r, w): return P.op("pool", fn, r, w)

        BK = [st.enter_context(nc.psum_tensor("bk%d" % i, [128, 512], F32)) for i in range(8)]
        rr = {"i": 0}

        def nbank(n=6):
            i = rr["i"] % n
            rr["i"] += 1
            return BK[i], "bk%d" % i

        identb = sb(st, "identb", [128, 128], BF16)
        identf = sb(st, "identf", [128, 128], F32)
        onesf = sb(st, "onesf", [128, 128], F32)
        onesb = sb(st, "onesb", [128, 128], BF16)
        epsb = sb(st, "epsb", [128, 1], F32)
        P.dma("sp", identb[:], c_identb, writes=["identb"])
        P.dma("sp", identf[:], c_identf, writes=["identf"])
        pool(lambda e: e.memset(onesf[:], 1.0), [], ["onesf"])
        pool(lambda e: e.memset(onesb[:], 1.0), [], ["onesb"])
        pool(lambda e: e.memset(epsb[:], EPS), [], ["epsb"])

        joinsc = sb(st, "joinsc", [128, 1], F32)

        def bcast_load(stack, name, ap_row, n):
            t = sb(stack, name, [128, n], F32)
            P.dma("sp", t[:], ap_row.to_broadcast([128, n]), writes=[name])
            return t

        def load_T(stack, name, src, R, C):
            t = sb(stack, name, [128, C, R], F32)
            with scope() as tmp:
                raw = sb(tmp, name + "_raw", [R, C * 128], F32)
                P.dma("sp", raw[:], src, writes=[name + "_raw"])
                for c in range(C):
                    bk, bkk = nbank()
                    pe(lambda e, bk=bk, c=c: e.transpose(bk[:, 0:R], raw[:, c * 128:(c + 1) * 128], identf[0:R, 0:R]),
                       [name + "_raw", "identf"], [bkk])
                    dve(lambda e, bk=bk, c=c: e.tensor_copy(out=t[:, c, :], in_=bk[:, 0:R]), [bkk], [name])
                P.barrier()
            return t

        def rstd_from_ss(ss, rs, n, scale, keys_r, key_w):
            act(lambda e: e.activation(out=rs, in_=ss, func=AF.Sqrt, bias=epsb[:], scale=scale), keys_r + ["epsb"], [key_w])
            dve(lambda e: e.reciprocal(out=rs, in_=rs), [key_w], [key_w])

        def wload_bf16(dst3, src2, K, N, key, n0=0):
            subs = []
            c = 0
            while c < N:
                w = min(2048, N - c)
                for k in range(K):
                    sk = "%s#%d_%d" % (key, c, k)
                    subs.append(sk)
                    P.dma("pool", dst3[:, k, c:c + w], src2[k * 128:(k + 1) * 128, n0 + c:n0 + c + w], writes=[sk])
                c += w
            P.op("pool", lambda e: e.memset(joinsc[:], 0.0), subs, [key, "joinsc"])

        with scope() as p1:
            w_tm = sb(p1, "w_tm", [128, 8, N_TM], BF16)
            w_fm = sb(p1, "w_fm", [128, 8, N_FM], BF16)
            wbb = sb(p1, "wbb", [128, 4, D], BF16)
            wbm = sb(p1, "wbm", [128, 4, D], BF16)
            kmT = sb(p1, "kmT", [128, 4, 256], BF16)
            vm = sb(p1, "vm", [128, 2, 512], BF16)
            wload_bf16(w_tm, w_in, 8, N_TM, "w_tm", 0)
            wload_bf16(w_fm, w_in, 8, N_FM, "w_fm", N_TM)
            wload_bf16(wbb, w_br_b, 4, D, "wbb")
            wload_bf16(wbm, w_br_m, 4, D, "wbm")
            gmix_bc = bcast_load(p1, "gmix_bc", g_mix, D)
            gqa_bc = bcast_load(p1, "gqa_bc", g_qa, 64)
            gka_bc = bcast_load(p1, "gka_bc", g_ka, 64)
            gki_bc = bcast_load(p1, "gki_bc", g_idx_k, 64)
            bgT = load_T(p1, "bgT", b_gate, 24, 1)
            cbT = load_T(p1, "cbT", conv_b, 4, 1)
            lgT = load_T(p1, "lgT", ln_g, 4, 1)
            lbT = load_T(p1, "lbT", ln_b, 4, 1)
            cwT = load_T(p1, "cwT", conv_w, 31, 4)
            uu = sb(p1, "uu", [128, 4, 30 + 512], BF16)
            pool(lambda e: e.memset(uu[:], 0.0), [], ["uu"])

            with scope() as p0:
                wkv = sb(p0, "wkv", [128, 8, 1024], BF16)
                wload_bf16(wkv, w_mem_kv, 8, 1024, "wkv")
                gmem_bc = bcast_load(p0, "gmem_bc", g_mem, D)
                gkm_bc = bcast_load(p0, "gkm_bc", g_km, 128)
                gqm_bc = bcast_load(p0, "gqm_bc", g_qm, 128)
                dve(lambda e: e.scalar_tensor_tensor(out=gkm_bc[:], in0=gkm_bc[:], scalar=128.0 ** -0.5, in1=gqm_bc[:],
                                                     op0=ALU.mult, op1=ALU.mult), ["gkm_bc", "gqm_bc"], ["gkm_bc"])
                mt_ = sb(p0, "mt_", [128, D], F32)
                junk = sb(p0, "junk0", [128, D], BF16)
                hm = sb(p0, "hm", [128, D], BF16)
                memT = sb(p0, "memT", [128, 8, 256], BF16)
                ss = sb(p0, "ss0", [128, 4], F32)
                rs = sb(p0, "rs0", [128, 4], F32)
                kx = sb(p0, "kx", [128, 512], F32)
                kn = sb(p0, "kn", [128, 512], BF16)
                for m in range(2):
                    P.dma("sp", mt_[:], mem[m * 128:(m + 1) * 128, :], writes=["mt_"])
                    act(lambda e: e.activation(out=junk[:], in_=mt_[:], func=AF.Square, accum_out=ss[:, 0:1]), ["mt_"], ["junk0", "ss0"])
                    rstd_from_ss(ss[:, 0:1], rs[:, 0:1], 1, 1.0 / D, ["ss0"], "rs0")
                    dve(lambda e: e.scalar_tensor_tensor(out=hm[:], in0=mt_[:], scalar=rs[:, 0:1], in1=gmem_bc[:],
                                                         op0=ALU.mult, op1=ALU.mult), ["mt_", "rs0", "gmem_bc"], ["hm"])
                    tb = BK[6][:].bitcast(BF16)
                    pe(lambda e: [e.transpose(tb[:, k * 128:(k + 1) * 128], hm[:, k * 128:(k + 1) * 128], identb[:]) for k in range(8)][-1],
                       ["hm", "identb"], ["bk6"])
                    dve(lambda e, m=m: e.tensor_copy(out=memT[:, :, m * 128:(m + 1) * 128],
                                                     in_=tb.rearrange("p (k t) -> p k t", k=8)), ["bk6"], ["memT"])
                for m in range(2):
                    for half in range(2):
                        bk, bkk = nbank()
                        pe(lambda e, bk=bk, m=m, half=half: [e.matmul(bk[:], lhsT=memT[:, k, m * 128:(m + 1) * 128],
                                                                       rhs=wkv[:, k, half * 512:(half + 1) * 512],
                                                                       start=(k == 0), stop=(k == 7)) for k in range(8)][-1],
                           ["memT", "wkv"], [bkk])
                        if half == 1:
                            act(lambda e, bk=bk, m=m: e.copy(out=vm[:, m, :], in_=bk[:]), [bkk], ["vm"])
                        else:
                            act(lambda e, bk=bk: e.copy(out=kx[:], in_=bk[:]), [bkk], ["kx"])
                            dve(lambda e: e.tensor_tensor(out=junk[:, 0:512], in0=kx[:], in1=kx[:], op=ALU.mult), ["kx"], ["junk0"])
                            dve(lambda e: e.tensor_reduce(out=ss[:], in_=junk[:, 0:512].rearrange("p (h d) -> p h d", h=4),
                                                          axis=AX.X, op=ALU.add), ["junk0"], ["ss0"])
                            rstd_from_ss(ss[:], rs[:], 4, 1.0 / 128, ["ss0"], "rs0")
                            for h in range(4):
                                dve(lambda e, h=h: e.scalar_tensor_tensor(out=kn[:, h * 128:(h + 1) * 128], in0=kx[:, h * 128:(h + 1) * 128],
                                                                          scalar=rs[:, h:h + 1], in1=gkm_bc[:], op0=ALU.mult, op1=ALU.mult),
                                    ["kx", "rs0", "gkm_bc"], ["kn"])
                            tb = BK[6][:].bitcast(BF16)
                            pe(lambda e: [e.transpose(tb[:, h * 128:(h + 1) * 128], kn[:, h * 128:(h + 1) * 128], identb[:]) for h in range(4)][-1],
                               ["kn", "identb"], ["bk6"])
                            dve(lambda e, m=m: e.tensor_copy(out=kmT[:, :, m * 128:(m + 1) * 128],
                                                             in_=tb[:, 0:512].rearrange("p (h t) -> p h t", h=4)), ["bk6"], ["kmT"])
                P.barrier()

            hT = sb(p1, "hT", [128, 8, 512], BF16)
            obT = sb(p1, "obT", [128, 4, 512], BF16)
            omT = sb(p1, "omT", [128, 4, 512], BF16)
            xt = [sb(p1, "xt%d" % i, [128, D], F32) for i in range(2)]
            junk = sb(p1, "junkA", [128, D], BF16)
            hh = sb(p1, "hh", [128, D], BF16)
            ss = sb(p1, "ssA", [128, 16], F32)
            rs = sb(p1, "rsA", [128, 16], F32)
            cs = [sb(p1, "cs%d" % i, [128, 2, 32], F32) for i in range(4)]
            qx = sb(p1, "qx", [128, 512], F32)
            sq = sb(p1, "sqA", [128, 512], F32)
            qn = sb(p1, "qn", [128, 512], F32)
            t1 = sb(p1, "t1", [128, 256], F32); t2 = sb(p1, "t2", [128, 256], F32)
            t3 = sb(p1, "t3", [128, 256], F32); t4 = sb(p1, "t4", [128, 256], F32)
            qr2 = [sb(p1, "qr%d" % i, [128, 512], BF16) for i in range(2)]
            stg = [sb(p1, "stg%d" % i, [64, 1024], BF16) for i in range(2)]
            vst = sb(p1, "vst", [128, 2, 65], BF16)
            wst = sb(p1, "wst", [128, 8], F32)
            pool(lambda e: e.memset(vst[:], 1.0), [], ["vst"])
            sgB = sb(p1, "sgB", [128, 512], F32)
            yy = sb(p1, "yy", [128, 4, 512], F32)
            sq2 = [sb(p1, "sq2_%d" % i, [128, 512], F32) for i in range(2)]
            rb = sb(p1, "rbB", [128, 512], F32)
            qmn = sb(p1, "qmn", [128, 512], BF16)
            pT = [sb(p1, "pTm%d" % i, [128, 512], BF16) for i in range(2)]
            rden = sb(p1, "rden", [128, 512], F32)
            mgst = [sb(p1, "mgst%d" % i, [128, 2, 512], BF16) for i in range(2)]
            print("phase1 arena words", top["v"], "of", ARENA_F32)
            sgi = {"i": 0}
            ab = {"i": 0}
            dgi = {"i": 0}
            dg = [sb(p1, "dg%d" % i, [128, 128], BF16) for i in range(8)]

            def norm_rope(src_ap, src_keys, nh, gain_bc, gkey, ck, csk, out_dram_fn):
                n = nh * 64
                qr = qr2[sgi["i"] % 2]; qrk = "qr%d" % (sgi["i"] % 2)
                if gain_bc is not None:
                    act(lambda e: e.copy(out=qx[:, 0:n], in_=src_ap), src_keys, ["qx"])
                    dve(lambda e: e.tensor_tensor(out=sq[:, 0:n], in0=qx[:, 0:n], in1=qx[:, 0:n], op=ALU.mult), ["qx"], ["sqA"])
                    dve(lambda e: e.tensor_reduce(out=ss[:, 0:nh], in_=sq[:, 0:n].rearrange("p (h d) -> p h d", h=nh),
                                                  axis=AX.X, op=ALU.add), ["sqA"], ["ssA"])
                    rstd_from_ss(ss[:, 0:nh], rs[:, 0:nh], nh, 1.0 / 64, ["ssA"], "rsA")
                    dve(lambda e: e.tensor_tensor(out=qn[:, 0:n].rearrange("p (h d) -> p h d", h=nh),
                                                  in0=qx[:, 0:n].rearrange("p (h d) -> p h d", h=nh),
                                                  in1=rs[:, 0:nh].unsqueeze(2).to_broadcast([128, nh, 64]), op=ALU.mult),
                        ["qx", "rsA"], ["qn"])
                    dve(lambda e: e.tensor_tensor(out=qn[:, 0:n].rearrange("p (h d) -> p h d", h=nh),
                                                  in0=qn[:, 0:n].rearrange("p (h d) -> p h d", h=nh),
                                                  in1=gain_bc[:].unsqueeze(1).to_broadcast([128, nh, 64]), op=ALU.mult),
                        ["qn", gkey], ["qn"])
                else:
                    act(lambda e: e.copy(out=qn[:, 0:n], in_=src_ap), src_keys, ["qn"])
                q3 = qn[:, 0:n].rearrange("p (h d) -> p h d", h=nh)
                o3 = qr[:, 0:n].rearrange("p (h d) -> p h d", h=nh)
                cosb = ck[:, 0:1, :].to_broadcast([128, nh, 32])
                sinb = ck[:, 1:2, :].to_broadcast([128, nh, 32])
                m = nh * 32
                v1 = t1[:, 0:m].rearrange("p (h d) -> p h d", h=nh); v2 = t2[:, 0:m].rearrange("p (h d) -> p h d", h=nh)
                v3 = t3[:, 0:m].rearrange("p (h d) -> p h d", h=nh); v4 = t4[:, 0:m].rearrange("p (h d) -> p h d", h=nh)
                dve(lambda e: e.tensor_tensor(out=v1, in0=q3[:, :, 0:32], in1=cosb, op=ALU.mult), ["qn", csk], ["t1"])
                dve(lambda e: e.tensor_tensor(out=v2, in0=q3[:, :, 32:64], in1=sinb, op=ALU.mult), ["qn", csk], ["t2"])
                dve(lambda e: e.tensor_tensor(out=o3[:, :, 0:32], in0=v1, in1=v2, op=ALU.subtract), ["t1", "t2"], [qrk])
                pool(lambda e: e.tensor_tensor(out=v3, in0=q3[:, :, 32:64], in1=cosb, op=ALU.mult), ["qn", csk], ["t3"])
                pool(lambda e: e.tensor_tensor(out=v4, in0=q3[:, :, 0:32], in1=sinb, op=ALU.mult), ["qn", csk], ["t4"])
                pool(lambda e: e.tensor_tensor(out=o3[:, :, 32:64], in0=v3, in1=v4, op=ALU.add), ["t3", "t4"], [qrk])
                sg = stg[sgi["i"] % 2]; sgk = "stg%d" % (sgi["i"] % 2); sgi["i"] += 1

                def part2():
                    tb = BK[7][:].bitcast(BF16)
                    pe(lambda e: [e.transpose(tb[0:64, h * 128:(h + 1) * 128], qr[:, h * 64:(h + 1) * 64], identb[:]) for h in range(nh)][-1],
                       [qrk, "identb"], ["bk7"])
                    act(lambda e: e.copy(out=sg[:, 0:nh * 128], in_=tb[0:64, 0:nh * 128]), ["bk7"], [sgk])
                    out_dram_fn(sg, sgk)
                return part2

            for b in range(NBLK):
                for tl in range(4):
                    ti = b * 4 + tl
                    r0 = ti * 128
                    xb = xt[tl % 2]; xk = "xt%d" % (tl % 2)
                    ck = cs[tl]; csk = "cs%d" % tl
                    P.dma("sp", xb[:], x[r0:r0 + 128, :], writes=[xk])
                    P.dma("sp", ck[:, 0, :], c_cos[:, ti, :], writes=[csk])
                    P.dma("sp", ck[:, 1, :], c_sin[:, ti, :], writes=[csk])
                    act(lambda e: e.activation(out=junk[:], in_=xb[:], func=AF.Square, accum_out=ss[:, 15:16]), [xk], ["junkA", "ss15"])
                    act(lambda e: e.activation(out=rs[:, 15:16], in_=ss[:, 15:16], func=AF.Sqrt, bias=epsb[:], scale=1.0 / D), ["ss15", "epsb"], ["rs15"])
                    dve(lambda e: e.reciprocal(out=rs[:, 15:16], in_=rs[:, 15:16]), ["rs15"], ["rs15"])
                    dve(lambda e: e.scalar_tensor_tensor(out=hh[:], in0=xb[:], scalar=rs[:, 15:16], in1=gmix_bc[:],
                                                         op0=ALU.mult, op1=ALU.mult), [xk, "rs15", "gmix_bc"], ["hh"])
                    tb6 = BK[6][:].bitcast(BF16)
                    pe(lambda e: [e.transpose(tb6[:, k * 128:(k + 1) * 128], hh[:, k * 128:(k + 1) * 128], identb[:]) for k in range(8)][-1],
                       ["hh", "identb"], ["bk6"])
                    act(lambda e: e.copy(out=hT[:, :, tl * 128:(tl + 1) * 128], in_=tb6.rearrange("p (k t) -> p k t", k=8)), ["bk6"], ["hT"])

                def tm_proj(tl, c0, n):
                    ab["i"] += 1
                    bk, bkk = BK[4 + ab["i"] % 2], "bk%d" % (4 + ab["i"] % 2)
                    pe(lambda e: [e.matmul(bk[:, 0:n], lhsT=hT[:, k, tl * 128:(tl + 1) * 128], rhs=w_tm[:, k, c0:c0 + n],
                                           start=(k == 0), stop=(k == 7)) for k in range(8)][-1], ["hT", "w_tm"], [bkk])
                    return bk, bkk

                def fm_proj(cc):
                    bk, bkk = nbank(4)
                    pe(lambda e: [e.matmul(bk[:], lhsT=w_fm[:, k, cc * 128:(cc + 1) * 128], rhs=hT[:, k, :],
                                           start=(k == 0), stop=(k == 7)) for k in range(8)][-1], ["hT", "w_fm"], [bkk])
                    return bk, bkk

                A_items = []
                X_items = []

                def mk_A(tl):
                    ti = b * 4 + tl
                    r0 = ti * 128
                    ck = cs[tl]; csk = "cs%d" % tl

                    def it_q():
                        bk, bkk = tm_proj(tl, C_QA, 512)
                        return norm_rope(bk[:, 0:512], [bkk], 8, gqa_bc, "gqa_bc", ck, csk,
                                         lambda sg, sgk: P.dma("sp", qT_s[ti], sg[:], reads=[sgk], writes=["qT_s%d" % ti]))

                    def it_kv():
                        bk, bkk = tm_proj(tl, C_KA, 256)
                        act(lambda e: e.copy(out=vst[:, :, 0:64], in_=bk[:, 128:256].rearrange("p (g d) -> p g d", g=2)), [bkk], ["vst"])
                        P.dma("sp", v_s[r0:r0 + 128, :], vst[:].rearrange("p g d -> p (g d)"), reads=["vst"], writes=["v_s%d" % ti])

                        def kout(sg, sgk):
                            for g in range(2):
                                P.dma("sp", kT_s[g, :, r0:r0 + 128], sg[:, g * 128:(g + 1) * 128], reads=[sgk], writes=["kT_s%d" % ti])
                        return norm_rope(bk[:, 0:128], [bkk], 2, gka_bc, "gka_bc", ck, csk, kout)

                    def it_qi():
                        bk, bkk = tm_proj(tl, C_QI, 512)
                        return norm_rope(bk[:, 0:512], [bkk], 8, None, None, ck, csk,
                                         lambda sg, sgk: P.dma("sp", qiT_s[ti], sg[:], reads=[sgk], writes=["qiT_s%d" % ti]))

                    def it_ki():
                        bk, bkk = tm_proj(tl, C_KI, 72)
                        act(lambda e: e.activation(out=wst[:], in_=bk[:, 64:72], func=AF.Copy, scale=float(8 ** -0.5 * 64 ** -0.5)), [bkk], ["wst"])
                        P.dma("sp", wi_s[r0:r0 + 128, :], wst[:], reads=["wst"], writes=["wi_s%d" % ti])
                        return norm_rope(bk[:, 0:64], [bkk], 1, gki_bc, "gki_bc", ck, csk,
                                         lambda sg, sgk: P.dma("sp", kiT_s[:, r0:r0 + 128], sg[:, 0:128], reads=[sgk], writes=["kiT_s%d" % ti]))
                    return [it_q, it_kv, it_qi, it_ki]

                for tl in range(4):
                    A_items.extend(mk_A(tl))

                def mk_conv(cp):
                    def it():
                        for c in cp:
                            bka, bkak = fm_proj(c)
                            bkg, bkgk = fm_proj(4 + c)
                            act(lambda e: e.activation(out=sgB[:], in_=bkg[:], func=AF.Sigmoid), [bkgk], ["sgB"])
                            dve(lambda e: e.tensor_tensor(out=uu[:, c, 30:542], in0=bka[:], in1=sgB[:], op=ALU.mult), [bkak, "sgB"], ["uu%d" % c])
                            cbk, cbkk = nbank(4)
                            for j in range(31):
                                d_ = dg[dgi["i"] % 8]; dk = "dg%d" % (dgi["i"] % 8); dgi["i"] += 1
                                pool(lambda e: e.tensor_scalar(out=d_[:], in0=identb[:], scalar1=cwT[:, c, j:j + 1], scalar2=0.0, op0=ALU.mult, op1=ALU.add),
                                     ["identb", "cwT"], [dk])
                                pe(lambda e: e.matmul(cbk[:], lhsT=d_[:], rhs=uu[:, c, j:j + 512], start=(j == 0), stop=(j == 30)),
                                   [dk, "uu%d" % c], [cbkk])
                            act(lambda e: e.activation(out=yy[:, c, :], in_=cbk[:], func=AF.Identity, bias=cbT[:, 0, c:c + 1]), [cbkk, "cbT"], ["yy%d" % c])
                            pool(lambda e: e.tensor_copy(out=uu[:, c, 0:30], in_=uu[:, c, 512:542]), ["uu%d" % c], ["uu%d" % c])
                    return it

                def it_ln():
                    yk = ["yy%d" % c for c in range(4)]
                    mbk, mbkk = nbank()
                    pe(lambda e: [e.matmul(mbk[:], lhsT=onesf[:], rhs=yy[:, c, :], start=(c == 0), stop=(c == 3)) for c in range(4)][-1],
                       ["onesf"] + yk, [mbkk])
                    for c in range(4):
                        dve(lambda e: e.scalar_tensor_tensor(out=yy[:, c, :], in0=mbk[:], scalar=-1.0 / 512, in1=yy[:, c, :],
                                                             op0=ALU.mult, op1=ALU.add), [mbkk, "yy%d" % c], ["yy%d" % c])
                    vbk, vbkk = nbank()
                    for c in range(4):
                        s_ = sq2[c % 2]; sk = "sq2_%d" % (c % 2)
                        act(lambda e: e.activation(out=s_[:], in_=yy[:, c, :], func=AF.Square), ["yy%d" % c], [sk])
                        pe(lambda e: e.matmul(vbk[:], lhsT=onesf[:], rhs=s_[:], start=(c == 0), stop=(c == 3)), ["onesf", sk], [vbkk])
                    act(lambda e: e.activation(out=rb[:], in_=vbk[:], func=AF.Sqrt, bias=epsb[:], scale=1.0 / 512), [vbkk, "epsb"], ["rbB"])
                    dve(lambda e: e.reciprocal(out=rb[:], in_=rb[:]), ["rbB"], ["rbB"])
                    for c in range(4):
                        dve(lambda e: e.tensor_tensor(out=yy[:, c, :], in0=yy[:, c, :], in1=rb[:], op=ALU.mult), ["yy%d" % c, "rbB"], ["yy%d" % c])
                        act(lambda e: e.activation(out=obT[:, c, :], in_=yy[:, c, :], func=AF.Silu, bias=lbT[:, 0, c:c + 1],
                                                   scale=lgT[:, 0, c:c + 1]), ["yy%d" % c, "lbT", "lgT"], ["obT"])

                def mk_mem(h):
                    def it():
                        bq, bqk = fm_proj(8 + h)
                        act(lambda e: e.activation(out=sgB[:], in_=bq[:], func=AF.Square), [bqk], ["sgB"])
                        b2, b2k = nbank()
                        pe(lambda e: e.matmul(b2[:], lhsT=onesf[:], rhs=sgB[:], start=True, stop=True), ["onesf", "sgB"], [b2k])
                        act(lambda e: e.activation(out=rb[:], in_=b2[:], func=AF.Sqrt, bias=epsb[:], scale=1.0 / 128), [b2k, "epsb"], ["rbB"])
                        dve(lambda e: e.reciprocal(out=rb[:], in_=rb[:]), ["rbB"], ["rbB"])
                        dve(lambda e: e.tensor_tensor(out=qmn[:], in0=bq[:], in1=rb[:], op=ALU.mult), [bqk, "rbB"], ["qmn"])
                        for mc in range(2):
                            b3, b3k = nbank()
                            pe(lambda e: e.matmul(b3[:], lhsT=kmT[:, h, mc * 128:(mc + 1) * 128], rhs=qmn[:], start=True, stop=True),
                               ["kmT", "qmn"], [b3k])
                            act(lambda e: e.activation(out=pT[mc][:], in_=b3[:], func=AF.Exp), [b3k], ["pTm%d" % mc])
                        bo, bok = nbank()
                        pe(lambda e: [e.matmul(bo[:], lhsT=vm[:, mc, h * 128:(h + 1) * 128], rhs=pT[mc][:], start=(mc == 0), stop=(mc == 1))
                                      for mc in range(2)][-1], ["vm", "pTm0", "pTm1"], [bok])
                        bd, bdk = nbank()
                        pe(lambda e: [e.matmul(bd[:], lhsT=onesb[:], rhs=pT[mc][:], start=(mc == 0), stop=(mc == 1)) for mc in range(2)][-1],
                           ["onesb", "pTm0", "pTm1"], [bdk])
                        dve(lambda e: e.reciprocal(out=rden[:], in_=bd[:]), [bdk], ["rden"])
                        dve(lambda e: e.tensor_tensor(out=omT[:, h, :], in0=bo[:], in1=rden[:], op=ALU.mult), [bok, "rden"], ["omT"])
                    return it

                def mk_gate(oc):
                    def it():
                        g1 = sq2[0]; g2 = sq2[1]; t2_ = rden
                        ms = mgst[oc % 2]; msk = "mgst%d" % (oc % 2)
                        bg0, bg0k = fm_proj(12 + oc)
                        act(lambda e: e.activation(out=ms[:, 0, :], in_=bg0[:], func=AF.Sigmoid, bias=bgT[:, 0, oc:oc + 1]), [bg0k, "bgT"], [msk])
                        bg1, bg1k = fm_proj(20 + oc)
                        act(lambda e: e.activation(out=g1[:], in_=bg1[:], func=AF.Sigmoid, bias=bgT[:, 0, 8 + oc:9 + oc]), [bg1k, "bgT"], ["sq2_0"])
                        bg2, bg2k = fm_proj(28 + oc)
                        act(lambda e: e.activation(out=g2[:], in_=bg2[:], func=AF.Sigmoid, bias=bgT[:, 0, 16 + oc:17 + oc]), [bg2k, "bgT"], ["sq2_1"])
                        pb, pbk = nbank()
                        pe(lambda e: [e.matmul(pb[:], lhsT=wbb[:, k, oc * 128:(oc + 1) * 128], rhs=obT[:, k, :], start=(k == 0), stop=(k == 3))
                                      for k in range(4)][-1], ["wbb", "obT"], [pbk])
                        dve(lambda e: e.tensor_tensor(out=g1[:], in0=pb[:], in1=g1[:], op=ALU.mult), [pbk, "sq2_0"], ["sq2_0"])
                        pm, pmk = nbank()
                        pe(lambda e: [e.matmul(pm[:], lhsT=wbm[:, k, oc * 128:(oc + 1) * 128], rhs=omT[:, k, :], start=(k == 0), stop=(k == 3))
                                      for k in range(4)][-1], ["wbm", "omT"], [pmk])
                        dve(lambda e: e.tensor_tensor(out=t2_[:], in0=pm[:], in1=g2[:], op=ALU.mult), [pmk, "sq2_1"], ["rden"])
                        pool(lambda e: e.tensor_tensor(out=ms[:, 1, :], in0=g1[:], in1=t2_[:], op=ALU.add), ["sq2_0", "rden"], [msk])
                        P.dma("sp", mg_s[b, :, oc], ms[:], reads=[msk], writes=["mg_s%d" % b])
                    return it

                X_items = [mk_conv((0, 1)), mk_conv((2, 3)), it_ln] + [mk_mem(h) for h in range(4)] + [mk_gate(oc) for oc in range(8)]
                na, nx = len(A_items), len(X_items)
                prev2 = None
                for n_ in range(na):
                    if n_ < nx:
                        X_items[n_]()
                    p2 = A_items[n_]()
                    if prev2 is not None:
                        prev2()
                    prev2 = p2
                for n_ in range(na, nx):
                    X_items[n_]()
                prev2()
            P.barrier()

        with scope() as p2:
            KT = sb(p2, "KT", [128, S], BF16)
            kiT = sb(p2, "kiT", [128, S], BF16)
            V = sb(p2, "V", [128, NT, 130], BF16)
            widx = sb(p2, "widx", [128, NT, 8], F32)
            score = sb(p2, "score", [128, S], F32)
            MB = sb(p2, "MB", [128, S], BF16)
            i4 = sb(p2, "i4", [128, 512], BF16)
            cb = sb(p2, "cb", [128, 128], F32)
            pow2 = sb(p2, "pow2", [128, KITER + 1], F32)
            allk = ["kT_s%d" % t for t in range(NT)]
            for g in range(2):
                P.dma("sp", KT[g * 64:(g + 1) * 64, :], kT_s[g], reads=allk, writes=["KT"])
            pool(lambda e: e.memset(kiT[64:128, :], 0.0), [], ["kiT"])
            P.dma("sp", kiT[0:64, :], kiT_s, reads=["kiT_s%d" % t for t in range(NT)], writes=["kiT"])
            for t0 in range(0, NT, 16):
                t1_ = min(NT, t0 + 16)
                P.dma("sp", V[:, t0:t1_, :], v_s[t0 * 128:t1_ * 128, :].rearrange("(n p) c -> p n c", p=128),
                      reads=["v_s%d" % t for t in range(t0, t1_)], writes=["V"])
            for t0 in range(0, NT, 8):
                t1_ = min(NT, t0 + 8)
                P.dma("sp", widx[:, t0:t1_, :], wi_s[t0 * 128:t1_ * 128, :].rearrange("(n p) c -> p n c", p=128),
                      reads=["wi_s%d" % t for t in range(t0, t1_)], writes=["widx"])
            P.dma("sp", i4[:], c_i4, writes=["i4"])
            conv_jobs = []
            for ex_ in range(32):
                for k8 in range(8):
                    conv_jobs.append((wub_s[ex_ * 128:(ex_ + 1) * 128, k8 * 512:(k8 + 1) * 512], w_up[ex_][k8 * 128:(k8 + 1) * 128, :], "wub_s"))
                for a_ in range(2):
                    conv_jobs.append((wdb_s[ex_ * 128:(ex_ + 1) * 128, a_ * D:(a_ + 1) * D], w_down[ex_][a_ * 128:(a_ + 1) * 128, :], "wdb_s"))

            def issue_conv(n_):
                for _ in range(n_):
                    if conv_jobs:
                        o_, i_, k_ = conv_jobs.pop(0)
                        P.dma("pool", o_, i_, writes=[])

            P.dma("sp", cb[:], c_cb, writes=["cb"])
            P.dma("sp", pow2[:], c_pow2, writes=["pow2"])
            qT = [sb(p2, "qT%d" % i, [128, 1024], BF16) for i in range(2)]
            qiT = [sb(p2, "qiT%d" % i, [128, 1024], BF16) for i in range(2)]
            for i_ in range(2):
                pool(lambda e: e.memset(qT[i_][:], 0.0), [], ["qT%d" % i_])
                pool(lambda e: e.memset(qiT[i_][:], 0.0), [], ["qiT%d" % i_])
            MB2 = [MB, sb(p2, "MBb", [128, S], BF16)]
            SC2 = [score, sb(p2, "scoreb", [128, S], F32)]
            diagw = sb(p2, "diagw", [128, 8, 128], BF16)
            RR = [sb(p2, "RR%d" % i, [128, 512], BF16) for i in range(4)]
            PT = [sb(p2, "PT%d" % i, [128, 512], BF16) for i in range(4)]
            sm2 = [sb(p2, "sm%d" % i, [128, 8], F32) for i in range(2)]
            wk2 = [sb(p2, "wk%d" % i, [128, KITER + 1], F32) for i in range(2)]
            jk = sb(p2, "jk", [128, 8], BF16)
            rdn = sb(p2, "rdn", [128, 8], F32)
            oa = sb(p2, "oa", [128, 512], BF16)
            oaT = [sb(p2, "oaT%d" % i, [128, 4, 128], BF16) for i in range(2)]
            OB = [BK[4], BK[5]]
            SC = BK[3]
            print("phase2 arena words", top["v"], "of", ARENA_F32)

            def stage_A(i):
                n = (i + 1) * 128
                qib = qiT[i % 2]; qik = "qiT%d" % (i % 2)
                MBi = MB2[i % 2]; MBk = "MB%d" % (i % 2)
                score = SC2[i % 2]; sck = "score%d" % (i % 2)
                sm = sm2[i % 2]; smk = "sm%d" % (i % 2)
                wk = wk2[i % 2]; wkk = "wk%d" % (i % 2)
                P.dma("sp", qib[0:64, :], qiT_s[i], reads=["qiT_s%d" % i], writes=[qik])
                for h in range(8):
                    pool(lambda e, h=h: e.tensor_scalar(out=diagw[:, h, :], in0=identb[:], scalar1=widx[:, i, h:h + 1], scalar2=None, op0=ALU.mult),
                         ["identb", "widx"], ["diagw"])
                nch = (n + 511) // 512
                units = [(c, h) for c in range(nch) for h in range(8)]
                Lb = {}

                def emit_L(u):
                    c, h = units[u]
                    k0 = c * 512
                    ncol = min(512, n - k0)
                    bk, bkk = nbank(3)
                    pe(lambda e: e.matmul(bk[:, 0:ncol], lhsT=qib[:, h * 128:(h + 1) * 128], rhs=kiT[:, k0:k0 + ncol], start=True, stop=True),
                       [qik, "kiT"], [bkk])
                    Lb[u] = (bk, bkk)

                for u in range(min(2, len(units))):
                    emit_L(u)
                for u in range(len(units)):
                    c, h = units[u]
                    k0 = c * 512
                    ncol = min(512, n - k0)
                    bk, bkk = Lb.pop(u)
                    R = RR[u % 4]; Rk = "RR%d" % (u % 4)
                    act(lambda e: e.activation(out=R[:, 0:ncol], in_=bk[:, 0:ncol], func=AF.Relu), [bkk], [Rk])
                    if u + 2 < len(units):
                        emit_L(u + 2)
                    scb = (BK[3], "bk3") if c % 2 == 0 else (BK[7], "bk7")
                    pe(lambda e: e.matmul(scb[0][:, 0:ncol], lhsT=diagw[:, h, :], rhs=R[:, 0:ncol], start=(h == 0), stop=(h == 7)),
                       ["diagw", Rk], [scb[1]])
                    if h == 7:
                        act(lambda e: e.copy(out=score[:, k0:k0 + ncol], in_=scb[0][:, 0:ncol]), [scb[1]], [sck])
                dve(lambda e: e.tensor_tensor(out=score[:, i * 128:(i + 1) * 128], in0=score[:, i * 128:(i + 1) * 128], in1=cb[:], op=ALU.add),
                    [sck, "cb"], [sck])
                if i >= 2:
                    dve(lambda e: e.tensor_reduce(out=sm[:, 0:1], in_=score[:, 0:n], axis=AX.X, op=ALU.max), [sck], [smk])
                    dve(lambda e: e.tensor_reduce(out=sm[:, 1:2], in_=score[:, 0:256], axis=AX.X, op=ALU.min), [sck], [smk])
                    dve(lambda e: e.tensor_tensor(out=sm[:, 2:3], in0=sm[:, 0:1], in1=sm[:, 1:2], op=ALU.subtract), [smk], [smk])
                    dve(lambda e: e.tensor_scalar(out=wk[:], in0=pow2[:], scalar1=sm[:, 2:3], scalar2=None, op0=ALU.mult), [smk, "pow2"], [wkk])
                    dve(lambda e: e.tensor_tensor(out=sm[:, 3:4], in0=sm[:, 1:2], in1=wk[:, 0:1], op=ALU.add), [smk, wkk], [smk])
                    for k in range(KITER):
                        dve(lambda e: e.tensor_scalar(out=jk[:, 0:1].to_broadcast([128, n]), in0=score[:, 0:n], scalar1=sm[:, 3:4], scalar2=None,
                                                      op0=ALU.is_ge, op1=ALU.add, accum_out=sm[:, 4:5]), [sck, smk], ["jk", smk])
                        dve(lambda e: e.tensor_scalar(out=sm[:, 5:6], in0=sm[:, 4:5], scalar1=TOPK - 0.5, scalar2=0.5, op0=ALU.is_ge, op1=ALU.subtract),
                            [smk], [smk])
                        dve(lambda e: e.scalar_tensor_tensor(out=sm[:, 3:4], in0=sm[:, 5:6], scalar=wk[:, k:k + 1], in1=sm[:, 3:4],
                                                             op0=ALU.mult, op1=ALU.add), [smk, wkk], [smk])
                    dve(lambda e: e.tensor_tensor(out=sm[:, 6:7], in0=sm[:, 3:4], in1=wk[:, KITER:KITER + 1], op=ALU.subtract), [smk, wkk], [smk])

            def stage_A2(i):
                n = (i + 1) * 128
                MBi = MB2[i % 2]; MBk = "MB%d" % (i % 2)
                score = SC2[i % 2]; sck = "score%d" % (i % 2)
                sm = sm2[i % 2]; smk = "sm%d" % (i % 2)
                if i >= 2:
                    dve(lambda e: e.tensor_scalar(out=MBi[:, 0:n], in0=score[:, 0:n], scalar1=sm[:, 6:7], scalar2=NEG, op0=ALU.is_lt, op1=ALU.mult),
                        [sck, smk], [MBk])
                else:
                    dve(lambda e: e.tensor_scalar(out=MBi[:, 0:n], in0=score[:, 0:n], scalar1=-1e29, scalar2=NEG, op0=ALU.is_lt, op1=ALU.mult),
                        [sck], [MBk])

            def stage_B(i):
                qb = qT[i % 2]; qk = "qT%d" % (i % 2)
                MBi = MB2[i % 2]; MBk = "MB%d" % (i % 2)
                P.dma("sp", qb[0:64, 0:512], qT_s[i][:, 0:512], reads=["qT_s%d" % i], writes=[qk])
                P.dma("sp", qb[64:128, 512:1024], qT_s[i][:, 512:1024], reads=["qT_s%d" % i], writes=[qk])
                units = [(j, g) for j in range(i + 1) for g in range(2)]
                Sb = {}

                def emit_S(u):
                    j, g = units[u]
                    bk, bkk = nbank(3)
                    pe(lambda e: [e.matmul(bk[:], lhsT=KT[:, j * 128:(j + 1) * 128], rhs=qb[:, g * 512:(g + 1) * 512], start=True, stop=False),
                                  e.matmul(bk[:], lhsT=MBi[:, j * 128:(j + 1) * 128], rhs=i4[:], start=False, stop=True)][-1],
                       ["KT", qk, MBk, "i4"], [bkk])
                    Sb[u] = (bk, bkk)

                for u in range(min(2, len(units))):
                    emit_S(u)
                for u in range(len(units)):
                    j, g = units[u]
                    bk, bkk = Sb.pop(u)
                    pt = PT[u % 4]; ptk = "PT%d" % (u % 4)
                    act(lambda e: e.activation(out=pt[:], in_=bk[:], func=AF.Exp, scale=0.125), [bkk], [ptk])
                    if u + 2 < len(units):
                        emit_S(u + 2)
                    ob = OB[g]
                    pe(lambda e: [e.matmul(ob[:, hh * 65:(hh + 1) * 65], lhsT=pt[:, hh * 128:(hh + 1) * 128], rhs=V[:, j, g * 65:(g + 1) * 65],
                                           start=(j == 0 and hh == 0), stop=(j == i and hh == 3), skip_group_check=True) for hh in range(4)][-1],
                       [ptk, "V"], ["bk%d" % (4 + g)])
                for g in range(2):
                    ob = OB[g]
                    o3 = ob[:, 0:260].rearrange("p (h d) -> p h d", h=4)
                    act(lambda e: e.activation(out=rdn[:, g * 4:(g + 1) * 4].unsqueeze(2), in_=o3[:, :, 64:65], func=AF.Ln), ["bk%d" % (4 + g)], ["rdn"])
                    act(lambda e: e.activation(out=rdn[:, g * 4:(g + 1) * 4], in_=rdn[:, g * 4:(g + 1) * 4], func=AF.Exp, scale=-1.0), ["rdn"], ["rdn"])
                    for hh in range(4):
                        h_ = g * 4 + hh
                        act(lambda e: e.activation(out=oa[:, h_ * 64:(h_ + 1) * 64], in_=o3[:, hh, 0:64], func=AF.Copy, scale=rdn[:, h_:h_ + 1]),
                            ["bk%d" % (4 + g), "rdn"], ["oa"])
                tb = BK[6][:].bitcast(BF16)
                pe(lambda e: [e.transpose(tb[:, k * 128:(k + 1) * 128], oa[:, k * 128:(k + 1) * 128], identb[:]) for k in range(4)][-1],
                   ["oa", "identb"], ["bk6"])
                ot = oaT[i % 2]; otk = "oaT%d" % (i % 2)
                act(lambda e: e.copy(out=ot[:], in_=tb[:, 0:512].rearrange("p (k t) -> p k t", k=4)), ["bk6"], [otk])
                P.dma("sp", oaT_s[:, :, i * 128:(i + 1) * 128], ot[:], reads=[otk], writes=["oaT_s%d" % i])

            stage_A(0); stage_A2(0)
            if NT > 1:
                stage_A(1); stage_A2(1)
            per_tile = (len(conv_jobs) + NT - 1) // NT
            for i in range(NT):
                if i + 2 < NT:
                    stage_A(i + 2)
                issue_conv(per_tile)
                stage_B(i)
                if i + 2 < NT:
                    stage_A2(i + 2)
            issue_conv(len(conv_jobs))
            P.barrier()

        out_evs = []
        with scope() as p3:
            I32 = mybir.dt.int32
            NM = 2 * S // 512
            gffn_bc = bcast_load(p3, "gffn_bc", g_ffn, D)
            wr = sb(p3, "wr", [128, 8, 36], BF16)
            wba = sb(p3, "wba", [128, 4, D], BF16)
            wo = sb(p3, "wo", [128, 8, D], BF16)
            wload_bf16(wba, w_br_a, 4, D, "wba")
            wload_bf16(wo, w_o, 8, D, "wo")
            brt = sb(p3, "brt", [128, 36], F32)
            P.dma("sp", brt[:, 0:4], b_rg.to_broadcast([128, 4]), writes=["brt"])
            P.dma("sp", brt[:, 4:36], b_re.to_broadcast([128, 32]), writes=["brt"])
            for k in range(8):
                P.dma("pool", wr[:, k, 0:4], w_rg[k * 128:(k + 1) * 128, :], writes=["wr"])
                P.dma("pool", wr[:, k, 4:36], w_re[k * 128:(k + 1) * 128, :], writes=["wr"])
            ltri = sb(p3, "ltri", [128, 128], BF16)
            P.dma("sp", ltri[:], c_ltri, writes=["ltri"])
            thrC = sb(p3, "thrC", [128, 32, NM], F32)
            P.dma("sp", thrC[:], c_thr, writes=["thrC"])
            tmC = sb(p3, "tmC", [128, 32, 32], F32)
            P.dma("sp", tmC[:], c_tm, writes=["tmC"])
            kC = sb(p3, "kC", [128, NTS, 32], F32)
            P.dma("sp", kC[:], c_kc, writes=["kC"])
            io8 = sb(p3, "io8", [128, 10], F32)
            P.dma("sp", io8[:], c_io8, writes=["io8"])
            M1s = sb(p3, "M1s", [128, NT, 32], F32)
            M2s = sb(p3, "M2s", [128, NT, 32], F32)
            rank1 = sb(p3, "rank1", [128, NT], F32)
            rank2 = sb(p3, "rank2", [128, NT], F32)
            W1s = sb(p3, "W1s", [128, NT], F32)
            W2s = sb(p3, "W2s", [128, NT], F32)
            cum = sb(p3, "cum", [128, 32], F32)
            pool(lambda e: e.memset(cum[:], 0.0), [], ["cum"])
            pos1 = sb(p3, "pos1", [128, NT], I32)
            pos2 = sb(p3, "pos2", [128, NT], I32)

            with scope() as pa:
                oab = sb(pa, "oab", [128, 4, 512], BF16)
                mgo = [sb(pa, "mgo%d" % i, [128, 2, 512], BF16) for i in range(2)]
                mTb = sb(pa, "mTb", [128, 8, 512], BF16)
                tt0 = sb(pa, "tt0", [128, 512], F32)
                x1b = [sb(pa, "x1b%d" % i, [128, D], F32) for i in range(4)]
                junk = sb(pa, "junk3", [128, D], BF16)
                h2 = [sb(pa, "h2_%d" % i, [128, D], BF16) for i in range(4)]
                h2Tt = [sb(pa, "h2Tt%d" % i, [128, 8, 128], BF16) for i in range(2)]
                ss4 = sb(pa, "ss4", [128, 4], F32)
                rs4 = sb(pa, "rs4", [128, 4], F32)
                lg4 = sb(pa, "lg4", [128, 4, 36], F32)
                gmx = sb(pa, "gmx", [128, 4], F32)
                oh4 = sb(pa, "oh4", [128, 4, 4], F32)
                eg4 = sb(pa, "eg4", [128, 4, 4], F32)
                se4 = sb(pa, "se4", [128, 4], F32)
                ps4 = sb(pa, "ps4", [128, 4], F32)
                em4 = sb(pa, "em4", [128, 4, 32], F32)
                em24 = sb(pa, "em24", [128, 4, 32], F32)
                m14 = sb(pa, "m14", [128, 4], F32)
                m24 = sb(pa, "m24", [128, 4], F32)
                mk14 = sb(pa, "mk14", [128, 4, 32], F32)
                mk24 = sb(pa, "mk24", [128, 4, 32], F32)
                dm4 = sb(pa, "dm4", [128, 4], F32)
                Mb4 = sb(pa, "Mb4", [128, 4, 32], BF16)
                cumT = sb(pa, "cumT", [128, 4, 32], F32)
                rk4 = sb(pa, "rk4", [128, 4, 32], F32)
                tmp4 = sb(pa, "tmp4", [128, 4, 32], F32)
                brt4 = brt[:].unsqueeze(1).to_broadcast([128, 4, 36])
                for blk in range(NBLK):
                    c0 = blk * 512
                    tb0 = blk * 4
                    P.dma("sp", oab[:], oaT_s[:, :, c0:c0 + 512], reads=["oaT_s%d" % (c0 // 128 + q_) for q_ in range(4)], writes=["oab"])
                    for oc in range(8):
                        mo = mgo[oc % 2]; mok = "mgo%d" % (oc % 2)
                        P.dma("sp", mo[:], mg_s[blk, :, oc], reads=["mg_s%d" % blk], writes=[mok])
                        bk, bkk = nbank()
                        pe(lambda e: [e.matmul(bk[:], lhsT=wba[:, k, oc * 128:(oc + 1) * 128], rhs=oab[:, k, :], start=(k == 0), stop=(k == 3))
                                      for k in range(4)][-1], ["wba", "oab"], [bkk])
                        dve(lambda e: e.tensor_tensor(out=tt0[:], in0=bk[:], in1=mo[:, 0, :], op=ALU.mult), [bkk, mok], ["tt0"])
                        pool(lambda e: e.tensor_tensor(out=mTb[:, oc, :], in0=tt0[:], in1=mo[:, 1, :], op=ALU.add), ["tt0", mok], ["mTb"])
                    for tl in range(4):
                        t = tb0 + tl
                        xb = x1b[tl]; xk = "x1b%d" % tl
                        P.dma("sp", xb[:], x[t * 128:(t + 1) * 128, :], writes=[xk])
                        for half in range(2):
                            bk, bkk = nbank()
                            pe(lambda e: [e.matmul(bk[:], lhsT=mTb[:, k, tl * 128:(tl + 1) * 128], rhs=wo[:, k, half * 512:(half + 1) * 512],
                                                   start=(k == 0), stop=(k == 7)) for k in range(8)][-1], ["mTb", "wo"], [bkk])
                            dve(lambda e: e.tensor_tensor(out=xb[:, half * 512:(half + 1) * 512], in0=bk[:],
                                                          in1=xb[:, half * 512:(half + 1) * 512], op=ALU.add), [bkk, xk], [xk])
                        P.dma("sp", x1_s[t * 128:(t + 1) * 128, :], xb[:], reads=[xk], writes=["x1_s%d" % t])
                        act(lambda e: e.activation(out=junk[:], in_=xb[:], func=AF.Square, accum_out=ss4[:, tl:tl + 1]), [xk], ["junk3", "ss4"])
                    act(lambda e: e.activation(out=rs4[:], in_=ss4[:], func=AF.Sqrt, bias=epsb[:], scale=1.0 / D), ["ss4", "epsb"], ["rs4"])
                    dve(lambda e: e.reciprocal(out=rs4[:], in_=rs4[:]), ["rs4"], ["rs4"])
                    for tl in range(4):
                        t = tb0 + tl
                        xb = x1b[tl]; xk = "x1b%d" % tl
                        hb = h2[tl]; hk = "h2_%d" % tl
                        hT_ = h2Tt[tl % 2]; hTk = "h2Tt%d" % (tl % 2)
                        dve(lambda e: e.scalar_tensor_tensor(out=hb[:], in0=xb[:], scalar=rs4[:, tl:tl + 1], in1=gffn_bc[:], op0=ALU.mult, op1=ALU.mult),
                            [xk, "rs4", "gffn_bc"], [hk])
                        P.dma("sp", h2_s[t * 128:(t + 1) * 128, :], hb[:], reads=[hk], writes=["h2_s%d" % t])
                        tb = BK[6][:].bitcast(BF16)
                        pe(lambda e: [e.transpose(tb[:, k * 128:(k + 1) * 128], hb[:, k * 128:(k + 1) * 128], identb[:]) for k in range(8)][-1],
                           [hk, "identb"], ["bk6"])
                        act(lambda e: e.copy(out=hT_[:], in_=tb.rearrange("p (k t) -> p k t", k=8)), ["bk6"], [hTk])
                        pe(lambda e: [e.matmul(BK[7][:, tl * 64:tl * 64 + 36], lhsT=hT_[:, k, :], rhs=wr[:, k, :], start=(k == 0), stop=(k == 7))
                                      for k in range(8)][-1], [hTk, "wr"], ["bk7"])
                    lgv = BK[7][:, 0:256].rearrange("p (t c) -> p t c", t=4)[:, :, 0:36]
                    dve(lambda e: e.tensor_tensor(out=lg4[:], in0=lgv, in1=brt4, op=ALU.add), ["bk7", "brt"], ["lg4"])
                    gl = lg4[:, :, 0:4]
                    el = lg4[:, :, 4:36]
                    dve(lambda e: e.tensor_reduce(out=gmx[:], in_=gl, axis=AX.X, op=ALU.max), ["lg4"], ["gmx"])
                    dve(lambda e: e.tensor_tensor(out=oh4[:], in0=gl, in1=gmx[:].unsqueeze(2).to_broadcast([128, 4, 4]), op=ALU.is_ge), ["lg4", "gmx"], ["oh4"])
                    dve(lambda e: e.tensor_tensor(out=eg4[:], in0=gl, in1=gmx[:].unsqueeze(2).to_broadcast([128, 4, 4]), op=ALU.subtract), ["lg4", "gmx"], ["eg4"])
                    act(lambda e: e.activation(out=eg4[:], in_=eg4[:], func=AF.Exp), ["eg4"], ["eg4"])
                    dve(lambda e: e.tensor_reduce(out=se4[:], in_=eg4[:], axis=AX.X, op=ALU.add), ["eg4"], ["se4"])
                    dve(lambda e: e.reciprocal(out=ps4[:], in_=se4[:]), ["se4"], ["ps4"])
                    dve(lambda e: e.tensor_scalar(out=oh4[:], in0=oh4[:], scalar1=1.0, scalar2=1e9, op0=ALU.subtract, op1=ALU.mult), ["oh4"], ["oh4"])
                    dve(lambda e: e.tensor_tensor(out=em4[:].rearrange("p t (g x) -> p t g x", g=4), in0=el.rearrange("p t (g x) -> p t g x", g=4),
                                                  in1=oh4[:].unsqueeze(3).to_broadcast([128, 4, 4, 8]), op=ALU.add), ["lg4", "oh4"], ["em4"])
                    dve(lambda e: e.tensor_reduce(out=m14[:], in_=em4[:], axis=AX.X, op=ALU.max), ["em4"], ["m14"])
                    dve(lambda e: e.tensor_tensor(out=mk14[:], in0=em4[:], in1=m14[:].unsqueeze(2).to_broadcast([128, 4, 32]), op=ALU.is_ge), ["em4", "m14"], ["mk14"])
                    dve(lambda e: e.scalar_tensor_tensor(out=em24[:].rearrange("p t e -> p (t e)"), in0=mk14[:].rearrange("p t e -> p (t e)"), scalar=-1e9,
                                                         in1=em4[:].rearrange("p t e -> p (t e)"), op0=ALU.mult, op1=ALU.add), ["mk14", "em4"], ["em24"])
                    dve(lambda e: e.tensor_reduce(out=m24[:], in_=em24[:], axis=AX.X, op=ALU.max), ["em24"], ["m24"])
                    dve(lambda e: e.tensor_tensor(out=mk24[:], in0=em24[:], in1=m24[:].unsqueeze(2).to_broadcast([128, 4, 32]), op=ALU.is_ge), ["em24", "m24"], ["mk24"])
                    dve(lambda e: e.tensor_tensor(out=dm4[:], in0=m24[:], in1=m14[:], op=ALU.subtract), ["m24", "m14"], ["dm4"])
                    act(lambda e: e.activation(out=dm4[:], in_=dm4[:], func=AF.Exp), ["dm4"], ["dm4"])
                    dve(lambda e: e.tensor_scalar(out=dm4[:], in0=dm4[:], scalar1=1.0, scalar2=None, op0=ALU.add), ["dm4"], ["dm4"])
                    dve(lambda e: e.reciprocal(out=dm4[:], in_=dm4[:]), ["dm4"], ["dm4"])
                    dve(lambda e: e.tensor_tensor(out=W1s[:, tb0:tb0 + 4], in0=dm4[:], in1=ps4[:], op=ALU.mult), ["dm4", "ps4"], ["W1s"])
                    dve(lambda e: e.tensor_tensor(out=W2s[:, tb0:tb0 + 4], in0=ps4[:], in1=W1s[:, tb0:tb0 + 4], op=ALU.subtract), ["ps4", "W1s"], ["W2s"])
                    dve(lambda e: e.tensor_tensor(out=Mb4[:], in0=mk14[:], in1=mk24[:], op=ALU.add), ["mk14", "mk24"], ["Mb4"])
                    pe(lambda e: [[e.matmul(BK[7][:, 256 + tl * 32:256 + (tl + 1) * 32], lhsT=ltri[:], rhs=Mb4[:, tl, :], start=True, stop=True),
                                   e.matmul(BK[7][:, 384 + tl * 32:384 + (tl + 1) * 32], lhsT=onesb[:], rhs=Mb4[:, tl, :], start=True, stop=True)][-1]
                                  for tl in range(4)][-1], ["ltri", "onesb", "Mb4"], ["bk7"])
                    pool(lambda e: e.tensor_copy(out=cumT[:, 0, :], in_=cum[:]), ["cum"], ["cumT"])
                    for tl in range(1, 4):
                        dve(lambda e: e.tensor_tensor(out=cumT[:, tl, :], in0=BK[7][:, 384 + (tl - 1) * 32:384 + tl * 32], in1=cumT[:, tl - 1, :], op=ALU.add),
                            ["bk7", "cumT"], ["cumT"])
                    dve(lambda e: e.tensor_tensor(out=cum[:], in0=BK[7][:, 480:512], in1=cumT[:, 3, :], op=ALU.add), ["bk7", "cumT"], ["cum"])
                    dve(lambda e: e.tensor_tensor(out=rk4[:], in0=BK[7][:, 256:384].rearrange("p (t e) -> p t e", t=4), in1=cumT[:], op=ALU.add),
                        ["bk7", "cumT"], ["rk4"])
                    dve(lambda e: e.tensor_tensor(out=tmp4[:], in0=mk14[:], in1=rk4[:], op=ALU.mult), ["mk14", "rk4"], ["tmp4"])
                    dve(lambda e: e.tensor_reduce(out=rank1[:, tb0:tb0 + 4], in_=tmp4[:], axis=AX.X, op=ALU.add), ["tmp4"], ["rank1"])
                    dve(lambda e: e.tensor_tensor(out=tmp4[:], in0=mk24[:], in1=rk4[:], op=ALU.mult), ["mk24", "rk4"], ["tmp4"])
                    dve(lambda e: e.tensor_reduce(out=rank2[:, tb0:tb0 + 4], in_=tmp4[:], axis=AX.X, op=ALU.add), ["tmp4"], ["rank2"])
                    pool(lambda e: e.tensor_copy(out=M1s[:, tb0:tb0 + 4, :], in_=mk14[:]), ["mk14"], ["M1s"])
                    pool(lambda e: e.tensor_copy(out=M2s[:, tb0:tb0 + 4, :], in_=mk24[:]), ["mk24"], ["M2s"])
                P.barrier()

            Ek = sb(p3, "Ek", [128, NTS], F32)
            idxu = sb(p3, "idxu", [128, NTS, 8], I32)
            idxd = sb(p3, "idxd", [128, NTS, 2], I32)
            with scope() as pb:
                big = sb(pb, "bigtmp", [128, max(NT, NTS, 32) * 32], F32)
                ntl = sb(pb, "ntl", [128, 32], F32)
                offt = sb(pb, "offt", [128, 32], F32)
                offs = sb(pb, "offs", [128, 32], F32)
                pf = sb(pb, "pf", [128, NT], F32)
                ef = sb(pb, "ef", [128, NTS, 8], F32)
                b3 = big[:, 0:32 * NM].rearrange("p (e m) -> p e m", e=32)
                dve(lambda e: e.tensor_tensor(out=b3, in0=cum[:].unsqueeze(2).to_broadcast([128, 32, NM]), in1=thrC[:], op=ALU.is_gt),
                    ["cum", "thrC"], ["bigtmp"])
                dve(lambda e: e.tensor_reduce(out=ntl[:], in_=b3, axis=AX.X, op=ALU.add), ["bigtmp"], ["ntl"])
                b4 = big[:, 0:1024].rearrange("p (e f) -> p e f", e=32)
                dve(lambda e: e.tensor_tensor(out=b4, in0=ntl[:].unsqueeze(1).to_broadcast([128, 32, 32]), in1=tmC[:], op=ALU.mult),
                    ["ntl", "tmC"], ["bigtmp"])
                dve(lambda e: e.tensor_reduce(out=offt[:], in_=b4, axis=AX.X, op=ALU.add), ["bigtmp"], ["offt"])
                dve(lambda e: e.tensor_scalar(out=offs[:], in0=offt[:], scalar1=512.0, scalar2=None, op0=ALU.mult), ["offt"], ["offs"])
                b5 = big[:, 0:NTS * 32].rearrange("p (k e) -> p k e", k=NTS)
                dve(lambda e: e.tensor_tensor(out=b5, in0=offt[:].unsqueeze(1).to_broadcast([128, NTS, 32]), in1=kC[:], op=ALU.is_le),
                    ["offt", "kC"], ["bigtmp"])
                dve(lambda e: e.tensor_reduce(out=Ek[:], in_=b5, axis=AX.X, op=ALU.add), ["bigtmp"], ["Ek"])
                dve(lambda e: e.tensor_scalar(out=Ek[:], in0=Ek[:], scalar1=-1.0, scalar2=None, op0=ALU.add), ["Ek"], ["Ek"])
                dve(lambda e: e.scalar_tensor_tensor(out=ef[:, :, 0], in0=Ek[:], scalar=128.0,
                                                     in1=io8[:, 8:9].to_broadcast([128, NTS]), op0=ALU.mult, op1=ALU.add),
                    ["Ek", "io8"], ["ef"])
                dve(lambda e: e.tensor_copy(out=idxu[:, :, 0], in_=ef[:, :, 0]), ["ef"], ["idxu"])
                for (Ms, Mk, rnk, rkk, ps, pk) in ((M1s, "M1s", rank1, "rank1", pos1, "pos1"), (M2s, "M2s", rank2, "rank2", pos2, "pos2")):
                    b6 = big[:, 0:NT * 32].rearrange("p (t e) -> p t e", t=NT)
                    dve(lambda e: e.tensor_tensor(out=b6, in0=Ms[:], in1=offs[:].unsqueeze(1).to_broadcast([128, NT, 32]), op=ALU.mult),
                        [Mk, "offs"], ["bigtmp"])
                    dve(lambda e: e.tensor_reduce(out=pf[:], in_=b6, axis=AX.X, op=ALU.add), ["bigtmp"], ["pf"])
                    dve(lambda e: e.tensor_tensor(out=pf[:], in0=pf[:], in1=rnk[:], op=ALU.add), ["pf", rkk], ["pf"])
                    dve(lambda e: e.tensor_copy(out=ps[:], in_=pf[:]), ["pf"], [pk])
                hrow = [sb(pb, "hrow%d" % i, [128, D], BF16) for i in range(3)]
                for t in range(NT):
                    hb = hrow[t % 3]; hk = "hrow%d" % (t % 3)
                    P.dma("sp", hb[:], h2_s[t * 128:(t + 1) * 128, :], reads=["h2_s%d" % t], writes=[hk])
                    P.idma(Hs[:, :], hb[:, :], pos1[:, t:t + 1], False, reads=[hk, "pos1"], writes=[])
                    P.idma(Hs[:, :], hb[:, :], pos2[:, t:t + 1], False, reads=[hk, "pos2"], writes=[])
                P.barrier()

            with scope() as pc:
                wu = [sb(pc, "wu%d" % i, [128, 8, 512], BF16) for i in range(3)]
                wd = [sb(pc, "wd%d" % i, [128, 2, D], BF16) for i in range(3)]
                hs = [sb(pc, "hs%d" % i, [128, D], BF16) for i in range(4)]
                HT = [sb(pc, "HT%d" % i, [128, 8, 512], BF16) for i in range(2)]
                sa = [sb(pc, "sa%d" % i, [128, 512], F32) for i in range(2)]
                gg = [sb(pc, "gg%d" % i, [128, 2, 512], BF16) for i in range(2)]
                ysb = [sb(pc, "ysb%d" % i, [128, D], F32) for i in range(2)]
                wub_v = wub_s
                wdb_v = wdb_s

                def load_w(k):
                    P.idma(wu[k % 3][:].rearrange("p k c -> p (k c)"), wub_v[:, :], idxu[:, k, 0:1], True, reads=["idxu"], writes=["wu%d" % (k % 3)])
                    P.idma(wd[k % 3][:].rearrange("p a c -> p (a c)"), wdb_v[:, :], idxu[:, k, 0:1], True, reads=["idxu"], writes=["wd%d" % (k % 3)])

                def load_h(k):
                    HTk = HT[k % 2]; HTkk = "HT%d" % (k % 2)
                    for sub in range(4):
                        hb = hs[sub]; hk = "hs%d" % sub
                        r0 = k * 512 + sub * 128
                        P.dma("sp", hb[:], Hs[r0:r0 + 128, :], writes=[hk])
                        tb = BK[6][:].bitcast(BF16)
                        pe(lambda e: [e.transpose(tb[:, kk * 128:(kk + 1) * 128], hb[:, kk * 128:(kk + 1) * 128], identb[:]) for kk in range(8)][-1],
                           [hk, "identb"], ["bk6"])
                        act(lambda e: e.copy(out=HTk[:, :, sub * 128:(sub + 1) * 128], in_=tb.rearrange("p (k t) -> p k t", k=8)), ["bk6"], [HTkk])

                pend = None

                def emit_down(pd):
                    k_, ci_ = pd
                    wdb_ = wd[k_ % 3]; wdk_ = "wd%d" % (k_ % 3)
                    for sub in range(4):
                        yb = ysb[sub % 2]; yk = "ysb%d" % (sub % 2)
                        for half in range(2):
                            di = 4 + ((sub * 2 + half) % 2)
                            pe(lambda e: [e.matmul(BK[di][:], lhsT=gg[ci_][:, a, sub * 128:(sub + 1) * 128], rhs=wdb_[:, a, half * 512:(half + 1) * 512],
                                                   start=(a == 0), stop=(a == 1)) for a in range(2)][-1], ["gg%d" % ci_, wdk_], ["bk%d" % di])
                            if half == 0:
                                act(lambda e: e.copy(out=yb[:, 0:512], in_=BK[di][:]), ["bk%d" % di], [yk])
                            else:
                                dve(lambda e: e.tensor_copy(out=yb[:, 512:1024], in_=BK[di][:]), ["bk%d" % di], [yk])
                        r0 = k_ * 512 + sub * 128
                        P.dma("sp", Ysc[r0:r0 + 128, :], yb[:], reads=[yk], writes=[])

                load_w(0)
                if NTS > 1:
                    load_w(1)
                load_h(0)
                for k in range(NTS):
                    ci = k % 2
                    wub = wu[k % 3]; wuk = "wu%d" % (k % 3)
                    HTk = HT[k % 2]; HTkk = "HT%d" % (k % 2)
                    for a in range(2):
                        for fc in (a, 2 + a):
                            pe(lambda e: [e.matmul(BK[fc][:], lhsT=wub[:, kk, fc * 128:(fc + 1) * 128], rhs=HTk[:, kk, :],
                                                   start=(kk == 0), stop=(kk == 7)) for kk in range(8)][-1], [wuk, HTkk], ["bk%d" % fc])
                        act(lambda e: e.activation(out=sa[a][:], in_=BK[a][:], func=AF.Silu), ["bk%d" % a], ["sa%d" % a])
                        dve(lambda e: e.tensor_tensor(out=gg[ci][:, a, :], in0=BK[2 + a][:], in1=sa[a][:], op=ALU.mult),
                            ["bk%d" % (2 + a), "sa%d" % a], ["gg%d" % ci])
                    if k + 1 < NTS:
                        load_h(k + 1)
                    if pend is not None:
                        emit_down(pend)
                    pend = (k, ci)
                    if k + 2 < NTS:
                        load_w(k + 2)
                emit_down(pend)
                P.barrier()

            with scope() as pd_:
                xf = [sb(pd_, "xf%d" % i, [128, D], F32) for i in range(3)]

                def fin_load(t_):
                    P.dma("sp", xf[t_ % 3][:], x1_s[t_ * 128:(t_ + 1) * 128, :], reads=["x1_s%d" % t_], writes=["xf%d" % (t_ % 3)])
                fin_load(0)
                y1 = [sb(pd_, "y1_%d" % i, [128, D], F32) for i in range(2)]
                y2 = [sb(pd_, "y2_%d" % i, [128, D], F32) for i in range(2)]
                for t in range(NT):
                    xb = xf[t % 3]; xk = "xf%d" % (t % 3)
                    a1 = y1[t % 2]; a1k = "y1_%d" % (t % 2)
                    a2 = y2[t % 2]; a2k = "y2_%d" % (t % 2)
                    if t + 1 < NT:
                        fin_load(t + 1)
                    P.idma(a1[:, :], Ysc[:, :], pos1[:, t:t + 1], True, reads=["pos1"], writes=[a1k])
                    P.idma(a2[:, :], Ysc[:, :], pos2[:, t:t + 1], True, reads=["pos2"], writes=[a2k])
                    dve(lambda e: e.scalar_tensor_tensor(out=xb[:], in0=a1[:], scalar=W1s[:, t:t + 1], in1=xb[:], op0=ALU.mult, op1=ALU.add),
                        [a1k, "W1s", xk], [xk])
                    dve(lambda e: e.scalar_tensor_tensor(out=xb[:], in0=a2[:], scalar=W2s[:, t:t + 1], in1=xb[:], op0=ALU.mult, op1=ALU.add),
                        [a2k, "W2s", xk], [xk])
                    out_evs.append(P.dma("sp", out[t * 128:(t + 1) * 128, :], xb[:], reads=[xk], writes=["out%d" % t]))
            P.final_wait("sp", out_evs)
        P.emit()
    return nc


def _consts(S):
    NT = S // 128
    bf = ml_dtypes.bfloat16
    c = {}
    c["c_identb"] = np.eye(128, dtype=np.float32).astype(bf)
    c["c_identf"] = np.eye(128, dtype=np.float32)
    c["c_i4"] = np.tile(np.eye(128, dtype=np.float32), (1, 4)).astype(bf)
    t = np.arange(128)[:, None]; s = np.arange(128)[None, :]
    c["c_cb"] = np.where(s <= t, 0.0, -1e30).astype(np.float32)
    half = 32
    inv = (10000.0 ** (-np.arange(half, dtype=np.float32) / half)).astype(np.float32)
    pos = np.arange(S, dtype=np.float32)
    ang = (pos[:, None] * inv[None, :]).astype(np.float32)
    cos = np.cos(ang).astype(np.float32).reshape(NT, 128, 32).transpose(1, 0, 2)
    sin = np.sin(ang).astype(np.float32).reshape(NT, 128, 32).transpose(1, 0, 2)
    c["c_cos"] = np.ascontiguousarray(cos); c["c_sin"] = np.ascontiguousarray(sin)
    NTS = (2 * S + 32 * 511 + 511) // 512
    NM = 2 * S // 512
    kk = np.arange(128)
    c["c_ltri"] = (kk[:, None] < kk[None, :]).astype(np.float32).astype(bf)
    c["c_thr"] = np.ascontiguousarray(np.broadcast_to((512.0 * np.arange(NM, dtype=np.float32))[None, None, :], (128, 32, NM))).astype(np.float32)
    ee = np.arange(32)
    c["c_tm"] = np.ascontiguousarray(np.broadcast_to((ee[None, :] < ee[:, None]).astype(np.float32)[None], (128, 32, 32)))
    c["c_kc"] = np.ascontiguousarray(np.broadcast_to(np.arange(NTS, dtype=np.float32)[None, :, None], (128, NTS, 32))).astype(np.float32)
    io = np.zeros((128, 10), np.float32)
    for k8 in range(8):
        io[:, k8] = k8 * 128 + np.arange(128)
    for a in range(2):
        io[:, 8 + a] = a * 128 + np.arange(128)
    c["c_io8"] = io
    c["c_pow2"] = np.tile((2.0 ** -(np.arange(KITER + 1) + 1.0)).astype(np.float32)[None, :], (128, 1))
    return c


def _prep(inp, S):
    f = lambda a: np.ascontiguousarray(np.asarray(a, dtype=np.float32))
    w = {}
    w["g_mix"] = f(inp["g_mix"]).reshape(1, D); w["w_in"] = f(inp["w_in"]).reshape(D, 5960)
    w["b_gate"] = f(inp["b_gate"]).reshape(24, 128)
    w["g_qa"] = f(inp["g_qa"]).reshape(1, 64); w["g_ka"] = f(inp["g_ka"]).reshape(1, 64); w["g_idx_k"] = f(inp["g_idx_k"]).reshape(1, 64)
    w["conv_w"] = f(inp["conv_w"]).reshape(31, 512); w["conv_b"] = f(inp["conv_b"]).reshape(4, 128)
    w["ln_g"] = f(inp["ln_g"]).reshape(4, 128); w["ln_b"] = f(inp["ln_b"]).reshape(4, 128)
    w["g_mem"] = f(inp["g_mem"]).reshape(1, D); w["w_mem_kv"] = f(inp["w_mem_kv"]).reshape(D, 1024)
    w["g_qm"] = f(inp["g_qm"]).reshape(1, 128); w["g_km"] = f(inp["g_km"]).reshape(1, 128)
    w["w_br_a"] = f(inp["w_br_a"]).reshape(512, D); w["w_br_b"] = f(inp["w_br_b"]).reshape(512, D); w["w_br_m"] = f(inp["w_br_m"]).reshape(512, D)
    w["w_o"] = f(inp["w_o"]).reshape(D, D); w["g_ffn"] = f(inp["g_ffn"]).reshape(1, D)
    w["w_rg"] = f(inp["w_rg"]).reshape(D, 4); w["b_rg"] = f(inp["b_rg"]).reshape(1, 4)
    w["w_re"] = f(inp["w_re"]).reshape(D, 32); w["b_re"] = f(inp["b_re"]).reshape(1, 32)
    w["w_up"] = f(inp["w_up"]).reshape(32, D, 512); w["w_down"] = f(inp["w_down"]).reshape(32, 256, D)
    w.update(_consts(S))
    return w


def kernel(**inputs):
    x = np.asarray(inputs["x"], dtype=np.float32)
    mem = np.asarray(inputs["mem"], dtype=np.float32)
    B, S, _ = x.shape
    shared = _prep(inputs, S)
    nc = build(S)
    in_maps = []
    for bi in range(B):
        m = dict(shared)
        m["x"] = np.ascontiguousarray(x[bi])
        m["mem"] = np.ascontiguousarray(mem[bi])
        in_maps.append(m)
    res = run_bass_kernel_spmd(nc, in_maps, core_ids=list(range(B)))
    return np.stack([np.asarray(r["out"]).reshape(S, D) for r in res.results], axis=0).astype(np.float32)
```
